# Optimizing a Trainium2 kernel written in Bass

```python
import math
import jax
import jax.numpy as jnp
from jax import lax
import numpy as np

D_MODEL = 1024
BATCH = 4
SEQ = 4096
DEPTH = 4

HEAD_DIM = 64
ROPE_DIM = HEAD_DIM // 4
ROPE_THETA = 500000.0
NORM_EPS = 1e-6
NEG_INF = -1e30
FORCE_SCORE = 1e6

MOBA_HEADS = D_MODEL // (2 * HEAD_DIM)
MOBA_BLOCK = 256
MOBA_TOPK = 3
MOBA_QCHUNK = 32
DIFF_HEADS = D_MODEL // (4 * HEAD_DIM)
DIFF_VDIM = 2 * HEAD_DIM
DIFF_QCHUNK = 128
MOBA_W = MOBA_HEADS * HEAD_DIM
DIFF_QK_W = DIFF_HEADS * 2 * HEAD_DIM
DIFF_V_W = DIFF_HEADS * DIFF_VDIM
EVEN_IN = 3 * MOBA_W + 2 * DIFF_QK_W + DIFF_V_W
EVEN_MIX = MOBA_W + DIFF_V_W

NSA_HEADS = D_MODEL // HEAD_DIM
NSA_GROUP = 4
NSA_KV_HEADS = NSA_HEADS // NSA_GROUP
CMP_BLOCK = 32
CMP_STRIDE = 16
CMP_HIDDEN = 256
SLC_BLOCK = 64
SLC_TOPN = 16
WINDOW = 512
NSA_QCHUNK = 16
NSA_Q_W = NSA_HEADS * HEAD_DIM
NSA_KV_W = NSA_KV_HEADS * HEAD_DIM
ODD_IN = NSA_Q_W + 6 * NSA_KV_W + 3 * NSA_HEADS
ODD_MIX = NSA_Q_W

MOE_GROUPS = 4
MOE_PER_GROUP = 4
MOE_EXPERTS = MOE_GROUPS * MOE_PER_GROUP
MOE_TOPK = 2
MOE_FF = 256

N_EVEN = (DEPTH + 1) // 2
N_ODD = DEPTH // 2

kernel_name = 'hybrid_moba_diff_nsa_hmoe'


def rms_norm(x, g):
    xf = x.astype(jnp.float32)
    y = xf * lax.rsqrt(jnp.mean(xf * xf, axis=-1, keepdims=True) + NORM_EPS)
    return (y * g.astype(jnp.float32)).astype(x.dtype)


def rope_tables(seq):
    pos = jnp.arange(seq, dtype=jnp.float32)
    inv = ROPE_THETA ** (-jnp.arange(0, ROPE_DIM, 2, dtype=jnp.float32) / ROPE_DIM)
    ang = pos[:, None] * inv[None, :]
    return jnp.cos(ang), jnp.sin(ang)


def apply_rope(t, cos, sin):
    half = ROPE_DIM // 2
    c = cos.astype(t.dtype)
    s = sin.astype(t.dtype)
    t1 = t[..., :half]
    t2 = t[..., half:ROPE_DIM]
    return jnp.concatenate([t1 * c - t2 * s, t2 * c + t1 * s, t[..., ROPE_DIM:]], axis=-1)


def split_heads(t, n):
    b, s, _ = t.shape
    return t.reshape(b, s, n, -1).transpose(0, 2, 1, 3)


def merge_heads(t):
    b, h, s, d = t.shape
    return t.transpose(0, 2, 1, 3).reshape(b, s, h * d)


def gather_blocks(blocks, idx):
    return jax.vmap(jax.vmap(lambda bl, ix: bl[ix]))(blocks, idx)


def moba_attention(q, k, v):
    b, h, s, d = q.shape
    nb = -(-s // MOBA_BLOCK)
    pad = nb * MOBA_BLOCK - s
    kp = jnp.pad(k, ((0, 0), (0, 0), (0, pad), (0, 0)))
    vp = jnp.pad(v, ((0, 0), (0, 0), (0, pad), (0, 0)))
    kb = kp.reshape(b, h, nb, MOBA_BLOCK, d)
    vb = vp.reshape(b, h, nb, MOBA_BLOCK, d)
    kmean = jnp.mean(kb.astype(jnp.float32), axis=3)
    ksel = max(1, min(MOBA_TOPK, nb - 1))
    scale = d ** -0.5
    nq = s // MOBA_QCHUNK
    qc = jnp.moveaxis(q.reshape(b, h, nq, MOBA_QCHUNK, d), 2, 0)

    def chunk(args):
        qi, ci = args
        q0 = ci * MOBA_QCHUNK
        qpos = q0 + jnp.arange(MOBA_QCHUNK)
        own = q0 // MOBA_BLOCK
        gs = jnp.einsum('bhqd,bhnd->bhqn', qi.astype(jnp.float32), kmean)
        gs = jnp.where(jnp.arange(nb) < own, gs, NEG_INF)
        _, idx = lax.top_k(gs, ksel)
        valid = idx < own
        k_sel = gather_blocks(kb, idx)
        v_sel = gather_blocks(vb, idx)
        s_sel = jnp.einsum('bhqd,bhqnld->bhqnl', qi, k_sel).astype(jnp.float32) * scale
        s_sel = jnp.where(valid[..., None], s_sel, NEG_INF).reshape(b, h, MOBA_QCHUNK, ksel * MOBA_BLOCK)
        k_own = lax.dynamic_slice_in_dim(kp, own * MOBA_BLOCK, MOBA_BLOCK, axis=2)
        v_own = lax.dynamic_slice_in_dim(vp, own * MOBA_BLOCK, MOBA_BLOCK, axis=2)
        kpos = own * MOBA_BLOCK + jnp.arange(MOBA_BLOCK)
        s_own = jnp.einsum('bhqd,bhld->bhql', qi, k_own).astype(jnp.float32) * scale
        s_own = jnp.where(kpos[None, :] <= qpos[:, None], s_own, NEG_INF)
        p = jax.nn.softmax(jnp.concatenate([s_sel, s_own], axis=-1), axis=-1).astype(v.dtype)
        p_sel = p[..., :ksel * MOBA_BLOCK].reshape(b, h, MOBA_QCHUNK, ksel, MOBA_BLOCK)
        p_own = p[..., ksel * MOBA_BLOCK:]
        return (jnp.einsum('bhqnl,bhqnld->bhqd', p_sel, v_sel)
                + jnp.einsum('bhql,bhld->bhqd', p_own, v_own))

    o = lax.map(chunk, (qc, jnp.arange(nq)))
    return jnp.moveaxis(o, 0, 2).reshape(b, h, s, d)


def diff_attention(q, k, v, lam):
    b, h, _, s, d = q.shape
    nq = s // DIFF_QCHUNK
    scale = d ** -0.5
    kpos = jnp.arange(s)
    qc = jnp.moveaxis(q.reshape(b, h, 2, nq, DIFF_QCHUNK, d), 3, 0)

    def chunk(args):
        qi, ci = args
        qpos = ci * DIFF_QCHUNK + jnp.arange(DIFF_QCHUNK)
        sc = jnp.einsum('bhmqd,bhmkd->bhmqk', qi, k).astype(jnp.float32) * scale
        sc = jnp.where(kpos[None, :] <= qpos[:, None], sc, NEG_INF)
        p = jax.nn.softmax(sc, axis=-1)
        w = p[:, :, 0] - lam * p[:, :, 1]
        return jnp.einsum('bhqk,bhke->bhqe', w.astype(v.dtype), v)

    o = lax.map(chunk, (qc, jnp.arange(nq)))
    return jnp.moveaxis(o, 0, 2).reshape(b, h, s, -1)


def compress_blocks(t, pos_emb, w1, b1, w2, b2):
    b, g, s, d = t.shape
    nc = (s - CMP_BLOCK) // CMP_STRIDE + 1
    idx = jnp.arange(nc)[:, None] * CMP_STRIDE + jnp.arange(CMP_BLOCK)[None, :]
    blocks = t[:, :, idx, :] + pos_emb.astype(t.dtype)
    hid = jax.nn.gelu(jnp.einsum('bgnld,ldf->bgnf', blocks, w1.reshape(CMP_BLOCK, d, -1)) + b1)
    return jnp.einsum('bgnf,fe->bgne', hid, w2) + b2


def nsa_attention(q, kc, vc, ks, vs, kw, vw, gates):
    b, hq, s, d = q.shape
    g = NSA_KV_HEADS
    r = NSA_GROUP
    nc = kc.shape[2]
    ns = s // SLC_BLOCK
    nsel = min(SLC_TOPN, ns)
    scale = d ** -0.5
    cmp_start = jnp.arange(nc) * CMP_STRIDE
    cmp_end = cmp_start + CMP_BLOCK - 1
    slc_start = jnp.arange(ns) * SLC_BLOCK
    overlap = ((cmp_start[:, None] <= slc_start[None, :] + SLC_BLOCK - 1)
               & (cmp_end[:, None] >= slc_start[None, :])).astype(jnp.float32)
    ksb = ks.reshape(b, g, ns, SLC_BLOCK, d)
    vsb = vs.reshape(b, g, ns, SLC_BLOCK, d)
    kwp = jnp.pad(kw, ((0, 0), (0, 0), (WINDOW, 0), (0, 0)))
    vwp = jnp.pad(vw, ((0, 0), (0, 0), (WINDOW, 0), (0, 0)))
    nq = s // NSA_QCHUNK
    qc = jnp.moveaxis(q.reshape(b, hq, nq, NSA_QCHUNK, d), 2, 0)
    gc = jnp.moveaxis(gates.reshape(b, hq, nq, NSA_QCHUNK, 3), 2, 0)
    blk = jnp.arange(ns)

    def chunk(args):
        qi, gi, ci = args
        q0 = ci * NSA_QCHUNK
        qpos = q0 + jnp.arange(NSA_QCHUNK)
        qg = qi.reshape(b, g, r, NSA_QCHUNK, d)
        s_c = jnp.einsum('bgrqd,bgnd->bgrqn', qg, kc).astype(jnp.float32) * scale
        s_c = jnp.where(cmp_end[None, :] <= qpos[:, None], s_c, NEG_INF)
        p_c = jax.nn.softmax(s_c, axis=-1)
        p_c = jnp.where((qpos >= CMP_BLOCK - 1)[:, None], p_c, 0.0)
        o_c = jnp.einsum('bgrqn,bgnd->bgrqd', p_c.astype(vc.dtype), vc)
        imp = jnp.einsum('bgrqn,nj->bgqj', p_c, overlap)
        own = qpos // SLC_BLOCK
        started = slc_start[None, :] <= qpos[:, None]
        forced = (blk[None, :] == 0) | (blk[None, :] == own[:, None]) | (blk[None, :] == own[:, None] - 1)
        imp = jnp.where(started, jnp.where(forced, FORCE_SCORE, imp), NEG_INF)
        _, sidx = lax.top_k(imp, nsel)
        k_sel = gather_blocks(ksb, sidx)
        v_sel = gather_blocks(vsb, sidx)
        s_s = jnp.einsum('bgrqd,bgqnld->bgrqnl', qg, k_sel).astype(jnp.float32) * scale
        tok = sidx[..., None] * SLC_BLOCK + jnp.arange(SLC_BLOCK)
        s_s = jnp.where((tok <= qpos[:, None, None])[:, :, None], s_s, NEG_INF)
        p_s = jax.nn.softmax(s_s.reshape(b, g, r, NSA_QCHUNK, -1), axis=-1).reshape(s_s.shape)
        o_s = jnp.einsum('bgrqnl,bgqnld->bgrqd', p_s.astype(v_sel.dtype), v_sel)
        k_w = lax.dynamic_slice_in_dim(kwp, q0, WINDOW + NSA_QCHUNK, axis=2)
        v_w = lax.dynamic_slice_in_dim(vwp, q0, WINDOW + NSA_QCHUNK, axis=2)
        kpos = q0 - WINDOW + jnp.arange(WINDOW + NSA_QCHUNK)
        band = ((kpos[None, :] <= qpos[:, None]) & (kpos[None, :] > qpos[:, None] - WINDOW)
                & (kpos[None, :] >= 0))
        s_w = jnp.einsum('bgrqd,bgkd->bgrqk', qg, k_w).astype(jnp.float32) * scale
        p_w = jax.nn.softmax(jnp.where(band, s_w, NEG_INF), axis=-1)
        o_w = jnp.einsum('bgrqk,bgkd->bgrqd', p_w.astype(v_w.dtype), v_w)
        gt = gi.reshape(b, g, r, NSA_QCHUNK, 3).astype(o_c.dtype)
        o = gt[..., 0:1] * o_c + gt[..., 1:2] * o_s + gt[..., 2:3] * o_w
        return o.reshape(b, hq, NSA_QCHUNK, d)

    o = lax.map(chunk, (qc, gc, jnp.arange(nq)))
    return jnp.moveaxis(o, 0, 2).reshape(b, hq, s, d)


def even_mixer(h, w_in, w_out, lam_p, subln_g, lambda_init, cos, sin):
    b, s, _ = h.shape
    p = h @ w_in
    c1 = MOBA_W
    c2 = 2 * MOBA_W
    c3 = 3 * MOBA_W
    c4 = c3 + DIFF_QK_W
    c5 = c4 + DIFF_QK_W
    qa, ka, va, qb, kb, vb = jnp.split(p, [c1, c2, c3, c4, c5], axis=-1)
    qa = apply_rope(split_heads(qa, MOBA_HEADS), cos, sin)
    ka = apply_rope(split_heads(ka, MOBA_HEADS), cos, sin)
    oa = moba_attention(qa, ka, split_heads(va, MOBA_HEADS))
    qb = apply_rope(qb.reshape(b, s, DIFF_HEADS, 2, HEAD_DIM).transpose(0, 2, 3, 1, 4), cos, sin)
    kb = apply_rope(kb.reshape(b, s, DIFF_HEADS, 2, HEAD_DIM).transpose(0, 2, 3, 1, 4), cos, sin)
    lp = lam_p.astype(jnp.float32)
    lam = jnp.exp(jnp.sum(lp[0] * lp[1])) - jnp.exp(jnp.sum(lp[2] * lp[3])) + lambda_init
    ob = diff_attention(qb, kb, split_heads(vb, DIFF_HEADS), lam)
    ob = rms_norm(ob, subln_g) * (1.0 - lambda_init)
    o = jnp.concatenate([merge_heads(oa), merge_heads(ob)], axis=-1)
    return o @ w_out


def odd_mixer(h, w_in, w_out, cmp_pos, cmp_w1, cmp_b1, cmp_w2, cmp_b2, cos, sin):
    b, s, _ = h.shape
    p = h @ w_in
    cuts = [NSA_Q_W + i * NSA_KV_W for i in range(7)]
    q, kc, vc, ks, vs, kw, vw, gl = jnp.split(p, cuts, axis=-1)
    q = apply_rope(split_heads(q, NSA_HEADS), cos, sin)
    kc = apply_rope(split_heads(kc, NSA_KV_HEADS), cos, sin)
    ks = apply_rope(split_heads(ks, NSA_KV_HEADS), cos, sin)
    kw = apply_rope(split_heads(kw, NSA_KV_HEADS), cos, sin)
    vc = split_heads(vc, NSA_KV_HEADS)
    vs = split_heads(vs, NSA_KV_HEADS)
    vw = split_heads(vw, NSA_KV_HEADS)
    gates = jax.nn.sigmoid(gl.astype(jnp.float32)).reshape(b, s, NSA_HEADS, 3).transpose(0, 2, 1, 3)
    k_cmp = compress_blocks(kc, cmp_pos[0], cmp_w1[0], cmp_b1[0], cmp_w2[0], cmp_b2[0])
    v_cmp = compress_blocks(vc, cmp_pos[1], cmp_w1[1], cmp_b1[1], cmp_w2[1], cmp_b2[1])
    o = nsa_attention(q, k_cmp, v_cmp, ks, vs, kw, vw, gates)
    return merge_heads(o) @ w_out


def hier_moe(h, wg, bg, we, be, w_gate, w_up, w_down):
    b, s, d = h.shape
    t = h.reshape(-1, d)
    n = t.shape[0]
    gl = (t @ wg + bg).astype(jnp.float32)
    gp = jax.nn.softmax(gl, axis=-1)
    _, gsel = lax.top_k(gl, 1)
    g_oh = jax.nn.one_hot(gsel[:, 0], MOE_GROUPS, dtype=jnp.float32)
    gw = jnp.sum(gp * g_oh, axis=-1, keepdims=True)
    el = (t @ we + be).astype(jnp.float32).reshape(n, MOE_GROUPS, MOE_PER_GROUP)
    el_g = jnp.sum(el * g_oh[:, :, None], axis=1)
    tv, ti = lax.top_k(el_g, MOE_TOPK)
    tw = jax.nn.softmax(tv, axis=-1)
    w_grp = jnp.sum(jax.nn.one_hot(ti, MOE_PER_GROUP, dtype=jnp.float32) * tw[..., None], axis=1)
    comb = ((g_oh * gw)[:, :, None] * w_grp[:, None, :]).reshape(n, MOE_EXPERTS)
    a = jax.nn.silu(jnp.einsum('td,edf->tef', t, w_gate)) * jnp.einsum('td,edf->tef', t, w_up)
    y = jnp.einsum('tef,efd->td', a * comb[:, :, None].astype(a.dtype), w_down)
    return y.reshape(b, s, d)


def setup_inputs(seed: int = 0) -> dict:
    key = jax.random.key(seed)
    ks = jax.random.split(key, 28)
    D = D_MODEL

    def nrm(k, shape, std):
        return jax.random.normal(k, shape, jnp.float32) * std

    return {
        'x': nrm(ks[0], (BATCH, SEQ, D), 1.0),
        'c': nrm(ks[1], (BATCH, D), 1.0),
        'norm1_g': 1.0 + nrm(ks[2], (DEPTH, D), 0.02),
        'norm2_g': 1.0 + nrm(ks[3], (DEPTH, D), 0.02),
        'final_g': 1.0 + nrm(ks[4], (D,), 0.02),
        'ada_w': nrm(ks[5], (DEPTH, D, 6 * D), 0.5 * D ** -0.5),
        'ada_b': nrm(ks[6], (DEPTH, 6 * D), 0.02),
        'ev_w_in': nrm(ks[7], (N_EVEN, D, EVEN_IN), D ** -0.5),
        'ev_w_out': nrm(ks[8], (N_EVEN, EVEN_MIX, D), EVEN_MIX ** -0.5),
        'ev_lambda': nrm(ks[9], (N_EVEN, 4, HEAD_DIM), 0.1),
        'ev_subln_g': 1.0 + nrm(ks[10], (N_EVEN, DIFF_VDIM), 0.02),
        'od_w_in': nrm(ks[11], (N_ODD, D, ODD_IN), D ** -0.5),
        'od_w_out': nrm(ks[12], (N_ODD, ODD_MIX, D), ODD_MIX ** -0.5),
        'od_cmp_pos': nrm(ks[13], (N_ODD, 2, CMP_BLOCK, HEAD_DIM), 0.1),
        'od_cmp_w1': nrm(ks[14], (N_ODD, 2, CMP_BLOCK * HEAD_DIM, CMP_HIDDEN), (CMP_BLOCK * HEAD_DIM) ** -0.5),
        'od_cmp_b1': nrm(ks[15], (N_ODD, 2, CMP_HIDDEN), 0.01),
        'od_cmp_w2': nrm(ks[16], (N_ODD, 2, CMP_HIDDEN, HEAD_DIM), CMP_HIDDEN ** -0.5),
        'od_cmp_b2': nrm(ks[17], (N_ODD, 2, HEAD_DIM), 0.01),
        'moe_wg': nrm(ks[18], (DEPTH, D, MOE_GROUPS), D ** -0.5),
        'moe_bg': nrm(ks[19], (DEPTH, MOE_GROUPS), 0.01),
        'moe_we': nrm(ks[20], (DEPTH, D, MOE_EXPERTS), D ** -0.5),
        'moe_be': nrm(ks[21], (DEPTH, MOE_EXPERTS), 0.01),
        'moe_w_gate': nrm(ks[22], (DEPTH, MOE_EXPERTS, D, MOE_FF), D ** -0.5),
        'moe_w_up': nrm(ks[23], (DEPTH, MOE_EXPERTS, D, MOE_FF), D ** -0.5),
        'moe_w_down': nrm(ks[24], (DEPTH, MOE_EXPERTS, MOE_FF, D), MOE_FF ** -0.5),
    }


def reference(x, c, norm1_g, norm2_g, final_g, ada_w, ada_b, ev_w_in, ev_w_out, ev_lambda,
              ev_subln_g, od_w_in, od_w_out, od_cmp_pos, od_cmp_w1, od_cmp_b1, od_cmp_w2,
              od_cmp_b2, moe_wg, moe_bg, moe_we, moe_be, moe_w_gate, moe_w_up, moe_w_down):
    s = x.shape[1]
    cos, sin = rope_tables(s)
    cs = jax.nn.silu(c)
    for l in range(DEPTH):
        mod = (cs @ ada_w[l] + ada_b[l])[:, None, :]
        sh1, sc1, g1, sh2, sc2, g2 = jnp.split(mod, 6, axis=-1)
        h = rms_norm(x, norm1_g[l]) * (1.0 + sc1) + sh1
        if l % 2 == 0:
            i = l // 2
            lambda_init = 0.8 - 0.6 * math.exp(-0.3 * l)
            y = even_mixer(h, ev_w_in[i], ev_w_out[i], ev_lambda[i], ev_subln_g[i], lambda_init, cos, sin)
        else:
            i = l // 2
            y = odd_mixer(h, od_w_in[i], od_w_out[i], od_cmp_pos[i], od_cmp_w1[i], od_cmp_b1[i],
                          od_cmp_w2[i], od_cmp_b2[i], cos, sin)
        x = x + g1 * y
        h = rms_norm(x, norm2_g[l]) * (1.0 + sc2) + sh2
        x = x + g2 * hier_moe(h, moe_wg[l], moe_bg[l], moe_we[l], moe_be[l],
                              moe_w_gate[l], moe_w_up[l], moe_w_down[l])
    return rms_norm(x, final_g)
```

```python
import math, os
import numpy as np
import ml_dtypes
from contextlib import ExitStack
import concourse.bass as bass
import concourse.mybir as mybir
from concourse.bass_utils import run_bass_kernel_spmd


F32 = mybir.dt.float32
BF16 = mybir.dt.bfloat16
AF = mybir.ActivationFunctionType
ALU = mybir.AluOpType
AX = mybir.AxisListType

SEM_ROT = 8000


class Buf:
    __slots__ = ("w", "r", "name", "excl")

    def __init__(self, name="", excl=False):
        self.w = None
        self.r = []
        self.name = name
        self.excl = excl


class T:
    def __init__(self, t, name=""):
        self.t = t
        self.b = Buf(name)

    def __getitem__(self, idx):
        return self.t[idx]


class KB:
    def __init__(self):
        self.nc = bass.Bass("TRN2", target_bir_lowering=False)
        self.es = ExitStack()
        self.scopes = []
        self.drams = {}
        nc = self.nc
        self.eng = {"pe": nc.tensor, "act": nc.scalar, "dve": nc.vector, "pool": nc.gpsimd, "sp": nc.sync}
        self.sem = {}
        self.cnt = {}
        self.seen = {e: {} for e in self.eng}
        for e in self.eng:
            self._newsem(e)
        self.dsems = [self.es.enter_context(nc.semaphore(f"dq{i}")) for i in range(24)]
        self.dcnt = [0] * len(self.dsems)
        self.dnext = 0
        self.n_inst = 0
        self.n_wait = 0

    def _newsem(self, e):
        self.sem[e] = self.es.enter_context(self.nc.semaphore(f"s_{e}_{len(self.sem)}_{self.n_inst if hasattr(self,'n_inst') else 0}"))
        self.cnt[e] = 0

    def sb(self, shape, dt=F32, name=None):
        es = self.scopes[-1] if self.scopes else self.es
        p = self.nc.sbuf_base
        st = (p + 31) // 32 * 32
        padb = (-st) % 128
        if padb:
            self._npad = getattr(self, "_npad", 0) + 1
            es.enter_context(self.nc.sbuf_tensor(f"pad_{self._npad}", [128, padb // 2], BF16))
        self._nsb = getattr(self, "_nsb", 0) + 1
        t = es.enter_context(self.nc.sbuf_tensor(f"sb{self._nsb}_" + (name or "t"), list(shape), dt))
        return T(t, name or "")

    def push_scope(self):
        self.scopes.append(ExitStack())

    def pop_scope(self):
        self.barrier()
        self.scopes.pop().close()

    def dram_once(self, name, shape, dt, kind="ExternalInput"):
        if name not in self.drams:
            self.drams[name] = self.dram(name, shape, dt, kind)
        return self.drams[name]

    def barrier(self):
        toks = []
        for e in ("pe", "act", "dve", "pool"):
            if self.cnt[e] > 0:
                toks.append((self.sem[e], self.cnt[e]))
        for i, s_ in enumerate(self.dsems):
            if self.dcnt[i] > 0:
                toks.append((s_, self.dcnt[i]))
        for (s_, v) in getattr(self, "cc_toks", []):
            toks.append((s_, v))
        for e in ("pe", "act", "dve", "pool", "sp"):
            self._wait(e, toks)

    def allgather(self, src, dst, groups):
        toks = self._deps([src], [dst])
        self._wait("pool", toks)
        if not hasattr(self, "cc_sem"):
            self.cc_sem = self.es.enter_context(self.nc.semaphore("cc_sem"))
            self.cc_cnt = 0
            self.cc_toks = []
        self.nc.gpsimd.collective_compute("AllGather", mybir.AluOpType.bypass, replica_groups=groups,
                                          ins=[src.t.ap().opt()], outs=[dst.t.ap().opt()]).then_inc(self.cc_sem)
        self.cc_cnt += 1
        tok = (self.cc_sem, self.cc_cnt)
        self.cc_toks = [tok]
        self._commit(tok, [src], [dst])
        return tok

    def ps(self, shape, dt=F32, name=None):
        t = self.es.enter_context(self.nc.psum_tensor("ps_" + name, list(shape), dt) if name else self.nc.psum_tensor(list(shape), dt))
        r = T(t, name or "")
        r.b.excl = True
        return r

    def dram(self, name, shape, dt, kind):
        if kind == "Internal":
            t = self.nc.dram_tensor(name, list(shape), dt)
        else:
            t = self.nc.dram_tensor(name, list(shape), dt, kind=kind)
        return T(t, name)

    def _deps(self, reads, writes):
        toks = []
        for b in reads:
            b = b.b if isinstance(b, T) else b
            if b.w is not None:
                toks.append(b.w)
            if b.excl:
                toks.extend(b.r)
        for b in writes:
            b = b.b if isinstance(b, T) else b
            if b.w is not None:
                toks.append(b.w)
            toks.extend(b.r)
        return toks

    def _wait(self, e, toks, skip_sem=None):
        eng = self.eng[e]
        need = {}
        for (s, v) in toks:
            if skip_sem is not None and s is skip_sem:
                continue
            if self.seen[e].get(s, 0) >= v:
                continue
            if need.get(s, 0) < v:
                need[s] = v
        for s, v in need.items():
            eng.wait_ge(s, v)
            self.seen[e][s] = v
            self.n_wait += 1

    def _commit(self, tok, reads, writes):
        for b in writes:
            b = b.b if isinstance(b, T) else b
            b.w = tok
            b.r = []
        for b in reads:
            b = b.b if isinstance(b, T) else b
            b.r.append(tok)
            if len(b.r) > 64:
                m = {}
                for (s, v) in b.r:
                    if m.get(s, (None, 0))[1] < v:
                        m[s] = (s, v)
                b.r = list(m.values())

    def op(self, e, fn, reads=(), writes=(), same_ok=False):
        if self.cnt[e] >= SEM_ROT:
            self._newsem(e)
        toks = self._deps(reads, writes)
        self._wait(e, toks, skip_sem=self.sem[e] if same_ok else None)
        ins = fn(self.eng[e])
        self.cnt[e] += 1
        tok = (self.sem[e], self.cnt[e])
        ins.then_inc(self.sem[e], 1)
        self._commit(tok, reads, writes)
        self.n_inst += 1
        return tok

    def dma(self, q, out, in_, reads=(), writes=(), **kw):
        i = self.dnext
        self.dnext = (self.dnext + 1) % len(self.dsems)
        s = self.dsems[i]
        toks = self._deps(reads, writes)
        if self.dcnt[i] > 0:
            toks.append((s, self.dcnt[i]))
        self._wait(q, toks)
        self.dcnt[i] += 16
        self.eng[q].dma_start(out=out, in_=in_, **kw).then_inc(s, 16)
        tok = (s, self.dcnt[i])
        self._commit(tok, reads, writes)
        self.n_inst += 1
        return tok

    def finish(self, outs):
        toks = []
        for b in outs:
            b = b.b if isinstance(b, T) else b
            if b.w is not None:
                toks.append(b.w)
        self._wait("sp", toks)
        for i, s_ in enumerate(self.dsems):
            if self.dcnt[i] > 0:
                self._wait("sp", [(s_, self.dcnt[i])])
        self._wait("sp", getattr(self, "cc_toks", []))
        for e in ("pe", "act", "dve", "pool"):
            if self.cnt[e] > 0:
                self._wait("sp", [(self.sem[e], self.cnt[e])])

def run_streams(gens, lag):
    active = []
    nxt = 0
    g = 0
    while nxt < len(gens) or active:
        if nxt < len(gens) and g >= nxt * lag:
            active.append(gens[nxt]); nxt += 1
        for gen in list(active):
            try:
                next(gen)
            except StopIteration:
                active.remove(gen)
        g += 1

SKIP = set(os.environ.get('SKIP', '').split(','))

D = 1024
S = 4096
NTT = 32
EPS = 1e-6
NEG = -30000.0


def norm_to_hT(k, x_ap, x_buf, small, sq_t, sq_b, xn_t, xn_b, pbA, pbB, id32, a_col, sh_col, col_bufs, outs):
    k.op("dve", lambda e: e.memset(small[:, 0:2], 0.0), writes=[small])
    k.op("act", lambda e: e.activation(out=sq_t, in_=x_ap, func=AF.Square, accum_out=small[:, 0:1]),
         reads=[x_buf, small], writes=[sq_b, small])
    k.op("act", lambda e: e.activation(out=small[:, 1:2], in_=small[:, 0:1], func=AF.Sqrt, scale=1.0 / D, bias=EPS),
         reads=[small], writes=[small])
    yield
    k.op("dve", lambda e: e.reciprocal(small[:, 2:3], small[:, 1:2]), reads=[small], writes=[small])
    k.op("dve", lambda e: e.tensor_scalar(xn_t, x_ap, small[:, 2:3], None, op0=ALU.mult),
         reads=[x_buf, small], writes=[xn_b])
    yield
    for c in range(8):
        bank = pbA if c < 4 else pbB
        k.op("pe", lambda e, c=c, bank=bank: e.transpose(bank[:, (c % 4) * 128:(c % 4 + 1) * 128], xn_t[:, c * 128:(c + 1) * 128], id32[:]),
             reads=[xn_b, id32], writes=[bank], same_ok=True)
    yield
    out_ap, out_buf = outs
    for c in range(8):
        bank = pbA if c < 4 else pbB
        src = bank[:, (c % 4) * 128:(c % 4 + 1) * 128]
        k.op("act", lambda e, c=c, src=src: e.activation(out=out_ap[:, c * 128:(c + 1) * 128], in_=src, func=AF.Identity,
                                                         scale=a_col[:, c:c + 1], bias=sh_col[:, c:c + 1]),
             reads=[bank] + col_bufs, writes=[out_buf])
    yield


def emit_A_even(k, pb, l, lambda_init, x_d, o_d, modc, PH=3):
    k.push_scope()
    n1c_d = k.dram_once("n1c_%d" % l, [128, 8], F32, "ExternalInput")
    win_d = k.dram_once("winA_%d" % l, [D, 1536], F32, "ExternalInput")
    cos_d = k.dram_once("cosT", [128, NTT * 8], F32)
    sin_d = k.dram_once("sinT", [128, NTT * 8], F32)
    lam_d = k.dram_once("lam_%d" % l, [1, 256], F32, "ExternalInput")
    sg_d = k.dram_once("subg_%d" % l, [1, 128], F32, "ExternalInput")
    id32_d = k.dram_once("id32", [128, 128], F32)
    id16_d = k.dram_once("id16", [128, 128], BF16)
    eall_d = k.dram_once("eall", [128, S], BF16)
    cb_d = k.dram_once("cb", [128, 4 * 512], BF16)
    ptab_d = k.dram_once("ptab", [128, 96], F32)

    win = k.sb([128, 8, 1536], BF16, "win")
    QKT = k.sb([128, 8, S], BF16, "QKT")
    QKT_b = [[Buf(f"QKT{j}_{t}") for t in range(8)] for j in range(8)]
    Va = k.sb([128, NTT, 4 * 65], BF16, "Va")
    Vb = k.sb([128, NTT, 2 * 129], BF16, "Vb")
    V_b = [Buf(f"V{t}") for t in range(NTT)]
    oo = k.sb([128, NTT, 512], BF16, "oo")
    oo_b = [Buf(f"oo{t}") for t in range(NTT)]
    eall = k.sb([128, S], BF16, "eall")
    cb = k.sb([128, 4 * 512], BF16, "cb")
    ptab = k.sb([128, 96], F32, "ptab")
    id32 = k.sb([128, 128], F32, "id32")
    id16 = k.sb([128, 128], BF16, "id16")
    n1c = k.sb([128, 8], F32, "n1c")
    a1 = k.sb([128, 8], F32, "a1")
    cosT = k.sb([128, NTT, 8], F32, "cosT")
    sinT = k.sb([128, NTT, 8], F32, "sinT")
    kmT = k.sb([128, 2, 16], BF16, "kmT")
    kmT32 = k.sb([128, 2, 16], F32, "kmT32")
    lamr = k.sb([1, 256], F32, "lamr")
    lams = k.sb([1, 8], F32, "lams")
    ones1 = k.sb([1, 128], F32, "ones1")
    nlam = k.sb([128, 1], F32, "nlam")
    gsl = k.sb([128, 128], F32, "gsl")
    smalls = [k.sb([128, 16], F32, f"small{i}") for i in range(2)]
    xt = [k.sb([128, D], F32, f"xt{i}") for i in range(3)]
    sq = k.sb([128, 128], BF16, "sq")
    hT = [k.sb([128, 8 * 128], BF16, f"hT{i}") for i in range(2)]
    qk16 = [k.sb([128, 1024], BF16, f"qk16_{i}") for i in range(2)]
    rtmps = [k.sb([128, 4, 128], F32, f"rtmp{i}") for i in range(2)]
    PT = [k.sb([128, 512], BF16, f"PT{i}") for i in range(2)]
    gst = k.sb([128, 64], F32, "gst")
    fsc = k.sb([128, 8], F32, "fsc")
    ob32 = k.sb([128, 4, 128], F32, "ob32")
    ob32_b = [Buf(f"ob32_{i}") for i in range(4)]
    QZ = [k.sb([128, 512], BF16, f"QZ{i}") for i in range(2)]
    qzc = [0]
    fss = [k.sb([128, 4], F32, f"fss{i}") for i in range(4)]

    MBTv = [win[:, 0:3, :].rearrange("p a b -> p (a b)")[:, 0:S], win[:, 3:6, :].rearrange("p a b -> p (a b)")[:, 0:S]]
    MBT_b = [Buf("MBT0"), Buf("MBT1")]
    for (t, d) in [(eall, eall_d), (cb, cb_d), (ptab, ptab_d), (id32, id32_d), (id16, id16_d), (n1c, n1c_d), (lamr, lam_d)]:
        k.dma("sp", t[:], d.t.ap(), writes=[t])
    k.dma("sp", cosT[:], cos_d.t.ap().rearrange("p (t f) -> p t f", f=8), writes=[cosT])
    k.dma("sp", sinT[:], sin_d.t.ap().rearrange("p (t f) -> p t f", f=8), writes=[sinT])
    k.dma("sp", gsl[:], sg_d.t.ap()[0:1, :].to_broadcast([128, 128]), writes=[gsl])
    for c in range(8):
        k.dma("pool", win[:, c, :], win_d.t.ap()[c * 128:(c + 1) * 128, :], writes=[win])
    k.op("dve", lambda e: e.memset(ones1[:], 1.0), writes=[ones1])
    k.op("dve", lambda e: e.scalar_tensor_tensor(out=a1[:], in0=modc[:, 8:16], scalar=1.0, in1=n1c[:], op0=ALU.add, op1=ALU.mult),
         reads=[modc, n1c], writes=[a1])
    sh1 = modc[:, 0:8]
    k.op("dve", lambda e: e.tensor_scalar(gsl[:], gsl[:], 1.0 - lambda_init, None, op0=ALU.mult), reads=[gsl], writes=[gsl])
    if "lam" in SKIP: lamr = k.sb([1, 256], F32, "lamr2")
    k.op("dve", lambda e: e.memset(lams[:], 0.0), writes=[lams])
    k.op("dve", lambda e: e.tensor_tensor(out=lamr[:, 0:64], in0=lamr[:, 0:64], in1=lamr[:, 64:128], op=ALU.mult), reads=[lamr], writes=[lamr])
    k.op("dve", lambda e: e.tensor_tensor(out=lamr[:, 128:192], in0=lamr[:, 128:192], in1=lamr[:, 192:256], op=ALU.mult), reads=[lamr], writes=[lamr])
    k.op("dve", lambda e: e.reduce_sum(lams[:, 0:1], lamr[:, 0:64], axis=AX.X), reads=[lamr, lams], writes=[lams])
    k.op("dve", lambda e: e.reduce_sum(lams[:, 1:2], lamr[:, 128:192], axis=AX.X), reads=[lamr, lams], writes=[lams])
    k.op("act", lambda e: e.activation(out=lams[:, 2:4], in_=lams[:, 0:2], func=AF.Exp), reads=[lams], writes=[lams])
    k.op("dve", lambda e: e.tensor_tensor(out=lams[:, 4:5], in0=lams[:, 3:4], in1=lams[:, 2:3], op=ALU.subtract), reads=[lams], writes=[lams])
    k.op("dve", lambda e: e.tensor_scalar(lams[:, 5:6], lams[:, 4:5], -lambda_init, None, op0=ALU.add), reads=[lams], writes=[lams])
    if "nlam" not in SKIP:
        k.op("pe", lambda e: e.matmul(pb[7][:, 0:1], ones1[:], lams[:, 5:6], start=True, stop=True), reads=[ones1, lams], writes=[pb[7]])
        k.op("dve", lambda e: e.tensor_copy(nlam[:], pb[7][:, 0:1]), reads=[pb[7]], writes=[nlam])

    NT1e = int(os.environ.get('NT1', NTT))

    def load_x(tt):
        xb = xt[tt % 3]
        k.dma("sp" if tt % 2 == 0 else "act", xb[:], x_d(tt)[0], reads=[x_d(tt)[1]], writes=[xb])

    def tileA(tt):
        j = tt % 2
        xb = xt[tt % 3]
        small = smalls[j]
        rtmp = rtmps[j]
        if tt == 0:
            load_x(0)
        if tt + 1 < NT1e:
            load_x(tt + 1)
        yield from norm_to_hT(k, xb[:], xb, small, hT[j][:], hT[j], xb[:], xb, pb[0], pb[1], id32, a1, sh1, [a1, modc], (hT[j][:], hT[j]))
        for blk in range(3):
            bank = pb[2 + blk]
            for c in range(8):
                k.op("pe", lambda e, c=c, blk=blk, bank=bank, j=j: e.matmul(bank[:], hT[j][:, c * 128:(c + 1) * 128], win[:, c, blk * 512:(blk + 1) * 512],
                                                                         start=(c == 0), stop=(c == 7)),
                     reads=[hT[j], win], writes=[bank], same_ok=True)
        yield
        for blk in range(2):
            k.op("act", lambda e, blk=blk, j=j: e.copy(qk16[j][:, blk * 512:(blk + 1) * 512], pb[2 + blk][:]), reads=[pb[2 + blk]], writes=[qk16[j]])
        k.op("act", lambda e, tt=tt: e.copy(Va[:, tt, :].rearrange("p (h d) -> p h d", h=4)[:, :, 0:64], pb[4][:, 0:256].rearrange("p (h d) -> p h d", h=4)),
             reads=[pb[4]], writes=[V_b[tt]])
        k.op("act", lambda e, tt=tt: e.copy(Vb[:, tt, :].rearrange("p (h d) -> p h d", h=2)[:, :, 0:128], pb[4][:, 256:512].rearrange("p (h d) -> p h d", h=2)),
             reads=[pb[4]], writes=[V_b[tt]])
        k.op("pool", lambda e, tt=tt: e.memset(Va[:, tt, :].rearrange("p (h d) -> p h d", h=4)[:, :, 64:65], 1.0), writes=[V_b[tt]])
        k.op("pool", lambda e, tt=tt: e.memset(Vb[:, tt, :].rearrange("p (h d) -> p h d", h=2)[:, :, 128:129], 1.0), writes=[V_b[tt]])
        yield
        cB = cosT[:, tt, :].unsqueeze(1).to_broadcast([128, 8, 8])
        sB = sinT[:, tt, :].unsqueeze(1).to_broadcast([128, 8, 8])
        for blk in range(2):
            pv = pb[2 + blk][:].rearrange("p (h d) -> p h d", h=8)
            t1 = pv[:, :, 0:8]; t2 = pv[:, :, 8:16]
            ov = qk16[j][:, blk * 512:(blk + 1) * 512].rearrange("p (h d) -> p h d", h=8)
            r = [rtmp[:, i, 0:64].rearrange("p (h d) -> p h d", h=8) for i in range(4)]
            RB = [pb[2 + blk], cosT, sinT, rtmp, qk16[j]]
            k.op("dve", lambda e, t1=t1, r=r: e.tensor_tensor(out=r[0], in0=t1, in1=cB, op=ALU.mult), reads=RB, writes=[rtmp])
            k.op("dve", lambda e, t2=t2, r=r: e.tensor_tensor(out=r[1], in0=t2, in1=sB, op=ALU.mult), reads=RB, writes=[rtmp])
            k.op("dve", lambda e, t2=t2, r=r: e.tensor_tensor(out=r[2], in0=t2, in1=cB, op=ALU.mult), reads=RB, writes=[rtmp])
            k.op("dve", lambda e, t1=t1, r=r: e.tensor_tensor(out=r[3], in0=t1, in1=sB, op=ALU.mult), reads=RB, writes=[rtmp])
            k.op("dve", lambda e, ov=ov, r=r: e.tensor_tensor(out=ov[:, :, 0:8], in0=r[0], in1=r[1], op=ALU.subtract), reads=[rtmp, qk16[j]], writes=[qk16[j]])
            k.op("dve", lambda e, ov=ov, r=r: e.tensor_tensor(out=ov[:, :, 8:16], in0=r[2], in1=r[3], op=ALU.add), reads=[rtmp, qk16[j]], writes=[qk16[j]])
        yield
        pT = pb[5 + j][:].bitcast(BF16)
        for blk in range(8):
            k.op("pe", lambda e, blk=blk, pT=pT, j=j: e.transpose(pT[:, blk * 128:(blk + 1) * 128], qk16[j][:, blk * 128:(blk + 1) * 128], id16[:]),
                 reads=[qk16[j], id16], writes=[pb[5 + j]], same_ok=True)
        yield
        k.op("dve", lambda e, pT=pT, tt=tt: e.tensor_copy(QKT[:, :, tt * 128:(tt + 1) * 128], pT.rearrange("p (b t) -> p b t", b=8)),
             reads=[pb[5 + j]], writes=[QKT_b[jb][tt // 4] for jb in range(8)])
        yield

    run_streams([tileA(tt) for tt in range(NT1e)], int(os.environ.get('LAGA', 4)))

    for jb in range(0 if 'red' in SKIP else 2):
        k.op("dve", lambda e, jb=jb: e.tensor_reduce(out=kmT32[:, jb, :], in_=QKT[:, 2 + jb, :].rearrange("p (n l) -> p n l", l=256), axis=AX.X, op=ALU.add),
             reads=QKT_b[2 + jb], writes=[kmT32])
    k.op("dve", lambda e: e.tensor_scalar(kmT[:], kmT32[:], 1.0 / 256, None, op0=ALU.mult), reads=[kmT32], writes=[kmT])

    def run_attention(steps, bg=None):
        n = len(steps)

        def emit_qk(i):
            st = steps[i]
            if "pre" in st:
                st["pre"]()
            bank = pb[i % 2]
            mm = st["qk"]
            for ii, (lh, rh, rd) in enumerate(mm):
                k.op("pe", lambda e, lh=lh, rh=rh, ii=ii, bank=bank: e.matmul(bank[:], lh, rh, start=(ii == 0), stop=(ii == len(mm) - 1)),
                     reads=rd, writes=[bank], same_ok=True)

        pending = [None]
        emit_qk(0)
        for i in range(n):
            st = steps[i]
            if i + 1 < n:
                emit_qk(i + 1)
            if bg is not None and i % 2 == 1:
                next(bg, None)
            bank = pb[i % 2]
            pt = PT[i % 2]
            k.op("act", lambda e, bank=bank, pt=pt: e.activation(out=pt[:], in_=bank[:], func=AF.Exp, scale=0.125), reads=[bank], writes=[pt])
            if "oT" in st:
                obT, obT_buf = st["oT"]
                k.op("pe", lambda e, obT=obT, pt=pt, st=st: e.matmul(obT, st["v"], pt[:], start=st["first"], stop=st["last"], skip_group_check=True),
                     reads=[pt] + st["vr"], writes=[obT_buf], same_ok=True)
            else:
                for sub in range(4):
                    oap, obuf = st["o"](sub)
                    k.op("pe", lambda e, sub=sub, oap=oap, pt=pt, st=st: e.matmul(oap, pt[:, sub * 128:(sub + 1) * 128], st["v"], start=(st["first"] and st["sf"](sub)), stop=st["last"], skip_group_check=True),
                         reads=[pt] + st["vr"], writes=[obuf], same_ok=True)
            if pending[0] is not None:
                pending[0]()
                pending[0] = None
            if st["last"]:
                if "fin_a" in st:
                    st["fin_a"]()
                    pending[0] = st["fin"]
                else:
                    st["fin"]()
        if pending[0] is not None:
            pending[0]()
            pending[0] = None

    gsts = [gst, k.sb([128, 64], F32, "gst1")]

    def gate_gen(h):
        jq = h // 2
        r0 = (h % 2) * 64
        mbt = MBTv[h % 2]; mbt_b = MBT_b[h % 2]
        for tt in range(NTT):
            own = tt // 2
            gs_ = gsts[tt % 2]
            k.op("pe", lambda e, tt=tt: e.matmul(pb[6][:, 0:16], QKT[r0:r0 + 64, jq, tt * 128:(tt + 1) * 128], kmT[r0:r0 + 64, jq, :], start=True, stop=True),
                 reads=[QKT_b[jq][tt // 4], kmT], writes=[pb[6]])
            G = [gs_, ptab]
            gsm = gs_[:, 0:16]; m8 = gs_[:, 16:24]; okf = gs_[:, 24:40]; mb = gs_[:, 40:56]
            k.op("dve", lambda e, own=own, gsm=gsm: e.tensor_tensor(out=gsm, in0=pb[6][:, 0:16], in1=ptab[:, 16 - own:32 - own], op=ALU.add), reads=[pb[6]] + G, writes=[gs_])
            k.op("dve", lambda e, gsm=gsm, m8=m8: e.max(out=m8, in_=gsm), reads=G, writes=[gs_])
            k.op("dve", lambda e, gsm=gsm, m8=m8, okf=okf: e.tensor_scalar(okf, gsm, m8[:, 2:3], None, op0=ALU.is_ge), reads=G, writes=[gs_])
            k.op("dve", lambda e, own=own, okf=okf: e.tensor_tensor(out=okf, in0=okf, in1=ptab[:, 32 + 16 - own:32 + 32 - own], op=ALU.mult), reads=G, writes=[gs_])
            k.op("dve", lambda e, own=own, okf=okf: e.tensor_tensor(out=okf, in0=okf, in1=ptab[:, 64 + 16 - own:64 + 32 - own], op=ALU.add), reads=G, writes=[gs_])
            k.op("dve", lambda e, okf=okf, mb=mb: e.tensor_scalar(mb, okf, -NEG, NEG, op0=ALU.mult, op1=ALU.add), reads=G, writes=[gs_])
            yield
            k.op("pe", lambda e, mb=mb: e.transpose(pb[7][0:16, 0:128], mb, id32[:]), reads=[gs_, id32], writes=[pb[7]])
            k.op("act", lambda e, tt=tt, mbt=mbt: e.copy(mbt[0:16, tt * 128:(tt + 1) * 128], pb[7][0:16, 0:128]), reads=[pb[7]], writes=[mbt_b, win])
            yield

    NH_A = 4 if PH >= 2 else 0
    bg0 = gate_gen(0) if NH_A else iter(())
    bgc = [0]
    for dh in range(2 if PH >= 3 else 0):
        for Q in range(8):
            nch = 4 * Q + 4
            steps = []
            for m in range(2):
                r0 = m * 64
                ob = (pb[2 + 2 * m], pb[3 + 2 * m])
                qz = QZ[qzc[0] % 2]; qzc[0] += 1

                def pre(qz=qz, r0=r0, dh=dh, Q=Q):
                    k.op("pool", lambda e: e.memset(qz[64 - r0:128 - r0, :], 0.0), writes=[qz])
                    k.op("pool", lambda e: e.tensor_copy(qz[r0:r0 + 64, :], QKT[r0:r0 + 64, 4 + dh, Q * 512:(Q + 1) * 512]), reads=[QKT_b[4 + dh][Q]], writes=[qz])
                for c in range(nch):
                    mm = [(QKT[:, 6 + dh, c * 128:(c + 1) * 128], qz[:, :], [QKT_b[6 + dh][c // 4], qz])]
                    if c >= 4 * Q:
                        d = c - 4 * Q
                        mm.append((id16[:], cb[:, d * 512:(d + 1) * 512], [id16, cb]))

                    def fin(Q=Q, dh=dh, m=m):
                        if m == 0:
                            return
                        for sub in range(4):
                            o0 = pb[2 + sub // 2][:, (sub % 2) * 129:(sub % 2) * 129 + 129]
                            o1 = pb[4 + sub // 2][:, (sub % 2) * 129:(sub % 2) * 129 + 129]
                            B0 = pb[2 + sub // 2]; B1 = pb[4 + sub // 2]
                            f = fss[sub]
                            F = [f]
                            k.op("dve", lambda e, f=f, o0=o0: e.tensor_scalar(f[:, 0:1], o0[:, 128:129], 1e-30, None, op0=ALU.add), reads=[B0] + F, writes=F)
                            k.op("dve", lambda e, f=f, o1=o1: e.tensor_scalar(f[:, 1:2], o1[:, 128:129], 1e-30, None, op0=ALU.add), reads=[B1] + F, writes=F)
                            k.op("dve", lambda e, f=f: e.reciprocal(f[:, 0:2], f[:, 0:2]), reads=F, writes=F)
                            k.op("dve", lambda e, f=f: e.tensor_tensor(out=f[:, 1:2], in0=f[:, 1:2], in1=nlam[:], op=ALU.mult), reads=F + [nlam], writes=F)
                            k.op("act", lambda e, sub=sub, f=f, o0=o0: e.activation(out=ob32[:, sub, :], in_=o0[:, 0:128], func=AF.Copy, scale=f[:, 0:1]), reads=[B0] + F, writes=[ob32_b[sub]])
                            k.op("dve", lambda e, sub=sub, f=f, o1=o1: e.scalar_tensor_tensor(out=ob32[:, sub, :], in0=o1[:, 0:128], scalar=f[:, 1:2], in1=ob32[:, sub, :], op0=ALU.mult, op1=ALU.add),
                                 reads=[B1, ob32_b[sub]] + F, writes=[ob32_b[sub]])
                        for sub in range(4):
                            tt = Q * 4 + sub
                            f = fss[sub]
                            F = [f]
                            k.op("dve", lambda e, f=f: e.memset(f[:, 2:4], 0.0), reads=F, writes=F)
                            k.op("act", lambda e, sub=sub, f=f: e.activation(out=sq[:, 0:128], in_=ob32[:, sub, :], func=AF.Square, accum_out=f[:, 2:3]), reads=[ob32_b[sub]] + F, writes=[sq] + F)
                            k.op("act", lambda e, f=f: e.activation(out=f[:, 3:4], in_=f[:, 2:3], func=AF.Sqrt, scale=1.0 / 128, bias=EPS), reads=F, writes=F)
                            k.op("dve", lambda e, f=f: e.reciprocal(f[:, 3:4], f[:, 3:4]), reads=F, writes=F)
                            k.op("dve", lambda e, sub=sub, tt=tt, f=f: e.scalar_tensor_tensor(out=oo[:, tt, 256 + dh * 128:256 + (dh + 1) * 128], in0=ob32[:, sub, :], scalar=f[:, 3:4], in1=gsl[:],
                                                                                  op0=ALU.mult, op1=ALU.mult), reads=[ob32_b[sub], gsl] + F, writes=[oo_b[tt]])
                    steps.append(dict(qk=mm, v=Vb[:, c, dh * 129:(dh + 1) * 129], vr=[V_b[c]],
                                      o=(lambda sub, ob=ob: (ob[sub // 2][:, (sub % 2) * 129:(sub % 2) * 129 + 129], ob[sub // 2])),
                                      first=(c == 0), last=(c == nch - 1), fin=fin, sf=(lambda sub: sub % 2 == 0)))
                    if c == 0:
                        steps[-1]["pre"] = pre
            run_attention_diff(k, steps, pb, PT, bg0, bgc)

    if NH_A:
        for _ in bg0:
            pass
    for h in range(NH_A):
        jq = h // 2
        r0 = (h % 2) * 64
        mbt = MBTv[h % 2]; mbt_b = MBT_b[h % 2]
        bg = gate_gen(h + 1) if h + 1 < NH_A else None
        steps = []
        for Q in range(8):
            nch = 4 * Q + 4
            qz = QZ[qzc[0] % 2]; qzc[0] += 1

            def pre(qz=qz, r0=r0, jq=jq, Q=Q):
                k.op("pool", lambda e: e.memset(qz[64 - r0:128 - r0, :], 0.0), writes=[qz])
                k.op("pool", lambda e: e.tensor_copy(qz[r0:r0 + 64, :], QKT[r0:r0 + 64, jq, Q * 512:(Q + 1) * 512]), reads=[QKT_b[jq][Q]], writes=[qz])
            for c in range(nch):
                mm = [(QKT[:, 2 + jq, c * 128:(c + 1) * 128], qz[:, :], [QKT_b[2 + jq][c // 4], qz]),
                      (eall[:, c * 128:(c + 1) * 128], mbt[:, Q * 512:(Q + 1) * 512], [eall, mbt_b])]
                if c >= 4 * Q:
                    d = c - 4 * Q
                    mm.append((id16[:], cb[:, d * 512:(d + 1) * 512], [id16, cb]))
                obank = pb[2 + Q % 2]

                def fin(Q=Q, h=h, obank=obank):
                    ov = obank[:, 0:260].rearrange("p (s d) -> p s d", s=4)
                    k.op("dve", lambda e: e.tensor_scalar(fsc[:, 0:4], ov[:, :, 64], 1e-30, None, op0=ALU.add), reads=[obank, fsc], writes=[fsc])
                    k.op("dve", lambda e: e.reciprocal(fsc[:, 0:4], fsc[:, 0:4]), reads=[fsc], writes=[fsc])
                    for sub in range(4):
                        tt = Q * 4 + sub
                        k.op("act", lambda e, sub=sub, tt=tt: e.activation(out=oo[:, tt, h * 64:(h + 1) * 64], in_=ov[:, sub, 0:64], func=AF.Copy, scale=fsc[:, sub:sub + 1]),
                             reads=[obank, fsc], writes=[oo_b[tt]])
                steps.append(dict(qk=mm, v=Va[:, c, h * 65:(h + 1) * 65], vr=[V_b[c]],
                                  o=(lambda sub, obank=obank: (obank[:, sub * 65:(sub + 1) * 65], obank)),
                                  first=(c == 0), last=(c == nch - 1), fin=fin, sf=(lambda sub: sub == 0)))
                if c == 0:
                    steps[-1]["pre"] = pre
        run_attention(steps, bg)
        if bg is not None:
            for _ in bg:
                pass

    for tt in range(NTT):
        k.dma("sp" if tt % 2 == 0 else "act", o_d(tt)[0], oo[:, tt, :], reads=[oo_b[tt]], writes=[o_d(tt)[1]])
    k.pop_scope()


def run_attention_diff(k, steps, pb, PT, bg=None, bgc=None):
    n = len(steps)

    def emit_qk(i):
        st = steps[i]
        if "pre" in st:
            st["pre"]()
        bank = pb[i % 2]
        mm = st["qk"]
        for ii, q in enumerate(mm):
            lh, rh, rd = q[0], q[1], q[2]
            outv = q[3](bank) if len(q) > 3 else bank[:]
            k.op("pe", lambda e, lh=lh, rh=rh, ii=ii, outv=outv: e.matmul(outv, lh, rh, start=(ii == 0), stop=(ii == len(mm) - 1), skip_group_check=True),
                 reads=rd, writes=[bank], same_ok=True)

    emit_qk(0)
    for i in range(n):
        st = steps[i]
        if i + 1 < n:
            emit_qk(i + 1)
        if bg is not None:
            bgc[0] += 1
            if bgc[0] % 4 == 0:
                next(bg, None)
        bank = pb[i % 2]
        pt = PT[i % 2]
        SM = os.environ.get('STEPMODE', 'all')
        if SM == 'qk': continue
        k.op("act", lambda e, bank=bank, pt=pt: e.activation(out=pt[:], in_=bank[:], func=AF.Exp, scale=0.125), reads=[bank], writes=[pt])
        for sub in range(0 if os.environ.get('STEPMODE', 'all') == 'qkexp' else 4):
            oap, obuf = st["o"](sub)
            k.op("pe", lambda e, sub=sub, oap=oap, pt=pt, st=st: e.matmul(oap, pt[:, sub * 128:(sub + 1) * 128], st["v"], start=(st["first"] and st["sf"](sub)), stop=st["last"], skip_group_check=True),
                 reads=[pt] + st["vr"], writes=[obuf], same_ok=True)
        if st["last"]:
            st["fin"]()


D = 1024
S = 4096
NTT = 32
EPS = 1e-6
NEG = -30000.0
NCOL = 1304
GC = 2.0 * math.sqrt(2.0 / math.pi)


def run_steps2(k, steps, pb, PT):
    n = len(steps)
    first_sw = {st["mst_job"]: idx for idx, st in enumerate(steps) if "mst_job" in st}
    deferred = []

    def emit_qk(i):
        st = steps[i]
        if "pre" in st:
            st["pre"]()
        bank = pb[i % 2]
        first = True
        for (lh, rh, rd, ofn) in st["qk"]:
            k.op("pe", lambda e, lh=lh, rh=rh, bank=bank, ofn=ofn, s0=first: e.matmul(ofn(bank), lh, rh, start=s0, stop=False, skip_group_check=True),
                 reads=rd, writes=[bank], same_ok=True)
            first = False

    emit_qk(0)
    for i in range(n):
        st = steps[i]
        if i + 1 < n:
            emit_qk(i + 1)
        bank = pb[i % 2]
        pt = PT[i % 2]
        k.op("act", lambda e, bank=bank, pt=pt: e.activation(out=pt[:], in_=bank[:], func=AF.Exp, scale=0.125), reads=[bank], writes=[pt])
        for sub in range(4):
            oap, obuf = st["o"](sub)
            k.op("pe", lambda e, sub=sub, oap=oap, pt=pt, st=st: e.matmul(oap, pt[:, sub * 128:(sub + 1) * 128], st["v"], start=(st["first"] and st["sf"](sub)), stop=st["last"], skip_group_check=True),
                 reads=[pt] + st["vr"], writes=[obuf], same_ok=True)
        if st["last"]:
            r = st["fin"]()
            if r is not None:
                job, tail = r
                due = min(i + 3, first_sw.get(job, i) - 2)
                if due <= i:
                    tail()
                else:
                    deferred.append((due, tail))
        while deferred and deferred[0][0] <= i:
            deferred.pop(0)[1]()
    for _, tail in deferred:
        tail()


def emit_A_odd(k, pb, l, x_d, o_d, modc, NT1=NTT, PH=3):
    k.push_scope()
    n1c_d = k.dram_once("n1c_%d" % l, [128, 8], F32, "ExternalInput")
    win_d = k.dram_once("winC_%d" % l, [D, NCOL], F32, "ExternalInput")
    cos_d = k.dram_once("cosT", [128, NTT * 8], F32)
    sin_d = k.dram_once("sinT", [128, NTT * 8], F32)
    id32_d = k.dram_once("id32", [128, 128], F32)
    id16_d = k.dram_once("id16", [128, 128], BF16)
    w1_d = k.dram_once("w1_%d" % l, [2, 2048, 256], F32, "ExternalInput")
    posT_d = k.dram_once("posT_%d" % l, [64, 2 * 32], F32, "ExternalInput")
    b1c_d = k.dram_once("b1c_%d" % l, [128, 4], F32, "ExternalInput")
    w2d_d = k.dram_once("w2d_%d" % l, [2, 256, 128], F32, "ExternalInput")
    b2c_d = k.dram_once("b2c_%d" % l, [128, 1], F32, "ExternalInput")
    b2r_d = k.dram_once("b2r_%d" % l, [1, 64], F32, "ExternalInput")
    ovl_d = k.dram_once("ovl", [128, 2 * 64], BF16)
    bc_d = k.dram_once("bc", [128, S], BF16)
    oh_d = k.dram_once("oh", [128, S], BF16)
    cbt_d = k.dram_once("cbt", [128, 256], BF16)
    tft_d = k.dram_once("tft", [128, 256], F32)

    QT = k.sb([128, 4, S], BF16, "QT")
    QT_b = [Buf(f"QT{t}") for t in range(NTT)]
    KT = k.sb([128, 4, S], BF16, "KT")
    KT_b = [Buf(f"KT{t}") for t in range(NTT)]
    R1 = k.sb([128, 4, S], BF16, "R1")
    R1_b = Buf("R1")
    oo = R1[:, :, :].rearrange("p a b -> p (a b)").rearrange("p (t c) -> p t c", c=512)
    oo_b = [Buf(f"oo{t}") for t in range(NTT)]
    Vs = k.sb([128, NTT, 2 * 65], BF16, "Vs")
    Vw = k.sb([128, NTT, 2 * 65], BF16, "Vw")
    V_b = [Buf(f"V{t}") for t in range(NTT)]
    gates = k.sb([128, NTT, 24], F32, "gates")
    R2 = k.sb([128, 8 * NCOL], BF16, "R2")
    R2_b = Buf("R2")
    win = R2[:, :].rearrange("p (c n) -> p c n", c=8)
    w1s = R2[0:64, 0:32 * 256].rearrange("p (l f) -> p l f", l=32)
    oh = k.sb([128, S], BF16, "oh")
    bc = k.sb([128, S], BF16, "bc")
    cbt = k.sb([128, 256], BF16, "cbt")
    tft = k.sb([128, 256], F32, "tft")
    id32 = k.sb([128, 128], F32, "id32")
    id16 = k.sb([128, 128], BF16, "id16")
    n1c = k.sb([128, 8], F32, "n1c")
    a1 = k.sb([128, 8], F32, "a1")
    cosT = k.sb([128, NTT, 8], F32, "cosT")
    sinT = k.sb([128, NTT, 8], F32, "sinT")
    ones1 = k.sb([1, 128], F32, "ones1")
    smalls = [k.sb([128, 16], F32, f"small{i}") for i in range(2)]
    xt = [k.sb([128, D], F32, f"xt{i}") for i in range(3)]
    hT = [k.sb([128, 8 * 128], BF16, f"hT{i}") for i in range(2)]
    qk32s = [k.sb([128, 896], F32, f"qk32_{i}") for i in range(2)]
    qk16 = [k.sb([128, 1408], BF16, f"qk16_{i}") for i in range(2)]
    rtmps = [k.sb([128, 4, 128], F32, f"rtmp{i}") for i in range(2)]
    PT = [k.sb([128, 512], BF16, f"PT{i}") for i in range(2)]
    posT = k.sb([64, 64], F32, "posT")
    posT16 = k.sb([64, 64], BF16, "posT16")
    b1c = k.sb([128, 4], F32, "b1c")
    w2d = k.sb([128, 2, 2, 128], BF16, "w2d")
    b2c = k.sb([128, 1], F32, "b2c")
    b2r = k.sb([1, 64], F32, "b2r")
    b2B = k.sb([128, 64], F32, "b2B")
    cb1 = k.sb([128, 2], F32, "cb1")
    gel = [k.sb([128, 256], F32, f"gel{i}") for i in range(3)]
    hidT = k.sb([128, 2, 256], BF16, "hidT")
    KCMP = k.sb([128, 2, 256], BF16, "KCMP")
    VCX = k.sb([128, 2, 2, 129], BF16, "VCX")
    imp = k.sb([128, 64], F32, "imp")
    selw = k.sb([128, 160], F32, "selw")
    MST = [k.sb([128, 128], BF16, f"MST{i}") for i in range(2)]
    oacc = [k.sb([128, 4, 64], F32, f"oacc{i}") for i in range(2)]
    fsc = k.sb([128, 32], F32, "fsc")

    for (t, d) in [(oh, oh_d), (bc, bc_d), (cbt, cbt_d), (tft, tft_d), (id32, id32_d), (id16, id16_d), (n1c, n1c_d),
                   (posT, posT_d), (b1c, b1c_d), (b2c, b2c_d), (b2r, b2r_d)]:
        k.dma("sp", t[:], d.t.ap(), writes=[t])
    k.dma("sp", b2B[:], b2r_d.t.ap()[0:1, :].to_broadcast([128, 64]), writes=[b2B])
    k.dma("sp", cosT[:], cos_d.t.ap().rearrange("p (t f) -> p t f", f=8), writes=[cosT])
    k.dma("sp", sinT[:], sin_d.t.ap().rearrange("p (t f) -> p t f", f=8), writes=[sinT])
    for c in range(8):
        k.dma("pool", win[:, c, :], win_d.t.ap()[c * 128:(c + 1) * 128, :], writes=[R2_b])
    for kind in range(2):
        k.dma("pool", w2d[:, kind, :, :], w2d_d.t.ap()[kind].rearrange("(c p) e -> p c e", p=128), writes=[w2d])
    for c in range(2):
        for g in range(2):
            k.dma("sp", VCX[:, c, g, 64:128], ovl_d.t.ap()[:, c * 64:(c + 1) * 64], writes=[VCX])
    k.op("pool", lambda e: e.memset(VCX[:, :, :, 128:129], 1.0), writes=[VCX])
    k.op("pool", lambda e: e.memset(KCMP[:], 0.0), writes=[KCMP])
    for m_ in MST:
        k.op("pool", lambda e, m_=m_: e.memset(m_[:], 0.0), writes=[m_])
    k.op("pool", lambda e: e.memset(hidT[:], 0.0), writes=[hidT])
    k.op("dve", lambda e: e.memset(ones1[:], 1.0), writes=[ones1])
    k.op("dve", lambda e: e.tensor_copy(posT16[:], posT[:]), reads=[posT], writes=[posT16])
    k.op("dve", lambda e: e.scalar_tensor_tensor(out=a1[:], in0=modc[:, 8:16], scalar=1.0, in1=n1c[:], op0=ALU.add, op1=ALU.mult),
         reads=[modc, n1c], writes=[a1])
    sh1 = modc[:, 0:8]

    def load_x(tt):
        xb = xt[tt % 3]
        k.dma("sp" if tt % 2 == 0 else "act", xb[:], x_d(tt)[0], reads=[x_d(tt)[1]], writes=[xb])

    def tileC(tt):
        j = tt % 2
        xb = xt[tt % 3]
        small = smalls[j]
        rtmp = rtmps[j]
        qk32 = qk32s[j]
        if tt == 0:
            load_x(0)
        if tt + 1 < NT1:
            load_x(tt + 1)
        yield from norm_to_hT(k, xb[:], xb, small, hT[j][:], hT[j], xb[:], xb, pb[0], pb[1], id32, a1, sh1, [a1, modc], (hT[j][:], hT[j]))
        widths = [512, 512, NCOL - 1024]
        for blk in range(3):
            bank = pb[2 + blk]
            wd = widths[blk]
            for c in range(8):
                k.op("pe", lambda e, c=c, blk=blk, bank=bank, j=j, wd=wd: e.matmul(bank[:, 0:wd], hT[j][:, c * 128:(c + 1) * 128], win[:, c, blk * 512:blk * 512 + wd],
                                                                                start=(c == 0), stop=(c == 7)),
                     reads=[hT[j], R2_b], writes=[bank], same_ok=True)
        yield
        k.op("act", lambda e: e.copy(qk32[:, 0:512], pb[2][:]), reads=[pb[2]], writes=[qk32])
        k.op("act", lambda e: e.copy(qk32[:, 512:896], pb[3][:, 0:384]), reads=[pb[3]], writes=[qk32])
        k.op("act", lambda e, j=j: e.copy(qk16[j][:, 1280:1408], pb[3][:, 384:512]), reads=[pb[3]], writes=[qk16[j]])
        k.op("act", lambda e, tt=tt: e.copy(Vs[:, tt, :].rearrange("p (g d) -> p g d", g=2)[:, :, 0:64], pb[4][:, 0:128].rearrange("p (g d) -> p g d", g=2)),
             reads=[pb[4]], writes=[V_b[tt]])
        k.op("act", lambda e, tt=tt: e.copy(Vw[:, tt, :].rearrange("p (g d) -> p g d", g=2)[:, :, 0:64], pb[4][:, 128:256].rearrange("p (g d) -> p g d", g=2)),
             reads=[pb[4]], writes=[V_b[tt]])
        k.op("act", lambda e, tt=tt: e.copy(gates[:, tt, :], pb[4][:, 256:280]), reads=[pb[4]], writes=[gates])
        k.op("pool", lambda e, tt=tt: e.memset(Vs[:, tt, :].rearrange("p (g d) -> p g d", g=2)[:, :, 64:65], 1.0), writes=[V_b[tt]])
        k.op("pool", lambda e, tt=tt: e.memset(Vw[:, tt, :].rearrange("p (g d) -> p g d", g=2)[:, :, 64:65], 1.0), writes=[V_b[tt]])
        yield
        cB = cosT[:, tt, :].unsqueeze(1).to_broadcast([128, 14, 8])
        sB = sinT[:, tt, :].unsqueeze(1).to_broadcast([128, 14, 8])
        qv = qk32[:].rearrange("p (h d) -> p h d", h=14)
        t1 = qv[:, :, 0:8]; t2 = qv[:, :, 8:16]
        r = [rtmp[:, i, 0:112].rearrange("p (h d) -> p h d", h=14) for i in range(4)]
        RB = [qk32, cosT, sinT, rtmp]
        k.op("dve", lambda e: e.tensor_tensor(out=r[0], in0=t1, in1=cB, op=ALU.mult), reads=RB, writes=[rtmp])
        k.op("dve", lambda e: e.tensor_tensor(out=r[1], in0=t2, in1=sB, op=ALU.mult), reads=RB, writes=[rtmp])
        k.op("dve", lambda e: e.tensor_tensor(out=r[2], in0=t2, in1=cB, op=ALU.mult), reads=RB, writes=[rtmp])
        k.op("dve", lambda e: e.tensor_tensor(out=r[3], in0=t1, in1=sB, op=ALU.mult), reads=RB, writes=[rtmp])
        k.op("dve", lambda e: e.tensor_tensor(out=t1, in0=r[0], in1=r[1], op=ALU.subtract), reads=[rtmp, qk32], writes=[qk32])
        k.op("dve", lambda e: e.tensor_tensor(out=t2, in0=r[2], in1=r[3], op=ALU.add), reads=[rtmp, qk32], writes=[qk32])
        yield
        k.op("pool", lambda e, j=j: e.tensor_copy(qk16[j][:, 0:512], qk32[:, 0:512]), reads=[qk32], writes=[qk16[j]])
        kdst = qk16[j][:, 512:1280].rearrange("p (k two d) -> p k two d", k=6, two=2)
        ksrc = qk32[:, 512:896].rearrange("p (k d) -> p k d", k=6)
        k.op("pool", lambda e, kdst=kdst, ksrc=ksrc, j=j: e.tensor_copy(kdst[:, :, 0, :], ksrc), reads=[qk32], writes=[qk16[j]])
        k.op("dve", lambda e, kdst=kdst, ksrc=ksrc, j=j: e.tensor_copy(kdst[:, :, 1, :], ksrc), reads=[qk32], writes=[qk16[j]])
        yield
        pA = pb[5][:].bitcast(BF16); pB = pb[6][:].bitcast(BF16)
        for blk in range(8):
            k.op("pe", lambda e, blk=blk, j=j: e.transpose(pA[:, blk * 128:(blk + 1) * 128], qk16[j][:, blk * 128:(blk + 1) * 128], id16[:]),
                 reads=[qk16[j], id16], writes=[pb[5]], same_ok=True)
        for blk in range(2):
            k.op("pe", lambda e, blk=blk, j=j: e.transpose(pB[:, blk * 128:(blk + 1) * 128], qk16[j][:, 1024 + blk * 128:1024 + (blk + 1) * 128], id16[:]),
                 reads=[qk16[j], id16], writes=[pb[6]], same_ok=True)
        for g in range(2):
            k.op("pe", lambda e, g=g, j=j: e.transpose(pB[0:64, 256 + g * 128:256 + (g + 1) * 128], qk16[j][:, 1280 + g * 64:1280 + (g + 1) * 64], id16[:]),
                 reads=[qk16[j], id16], writes=[pb[6]], same_ok=True)
        yield
        tsl = slice(tt * 128, (tt + 1) * 128)
        k.op("dve", lambda e, tsl=tsl: e.tensor_copy(QT[:, :, tsl], pA[:, 0:512].rearrange("p (b t) -> p b t", b=4)), reads=[pb[5]], writes=[QT_b[tt]])
        k.op("dve", lambda e, tsl=tsl: e.tensor_copy(R1[0:64, 0:2, tsl], pA[0:64, 512:768].rearrange("p (b t) -> p b t", b=2)), reads=[pb[5]], writes=[R1_b])
        k.op("dve", lambda e, tsl=tsl: e.tensor_copy(KT[:, 0:2, tsl], pA[:, 768:1024].rearrange("p (b t) -> p b t", b=2)), reads=[pb[5]], writes=[KT_b[tt]])
        k.op("act", lambda e, tsl=tsl: e.copy(KT[:, 2:4, tsl], pB[:, 0:256].rearrange("p (b t) -> p b t", b=2)), reads=[pb[6]], writes=[KT_b[tt]])
        k.op("act", lambda e, tsl=tsl: e.copy(R1[0:64, 2:4, tsl], pB[0:64, 256:512].rearrange("p (b t) -> p b t", b=2)), reads=[pb[6]], writes=[R1_b])
        yield

    run_streams([tileC(tt) for tt in range(NT1)], int(os.environ.get('LAGC', 4)))
    k.op("act", lambda e: e.activation(out=gates[:, :, :], in_=gates[:, :, :], func=AF.Sigmoid), reads=[gates], writes=[gates])

    if PH < 2:
        k.dma("sp", o_d.t.ap()[0:128, :], QT[:, 0, 0:512], reads=QT_b, writes=[o_d])
        k.dma("sp", o_d.t.ap()[128:256, :], KT[:, 0, 0:512], reads=KT_b, writes=[o_d])
        k.dma("sp", o_d.t.ap()[256:384, :], KT[:, 2, 0:512], reads=KT_b, writes=[o_d])
        k.dma("sp", o_d.t.ap()[384:448, :], R1[0:64, 0, 0:512], reads=[R1_b], writes=[o_d])
        k.dma("sp", o_d.t.ap()[448:512, :], R1[0:64, 2, 0:512], reads=[R1_b], writes=[o_d])
        k.finish([o_d])
        return k

    for kind in range(2):
        k.dma("pool", w1s, w1_d.t.ap()[kind].rearrange("(l d) f -> d l f", d=64), reads=[], writes=[R2_b])
        for g in range(2):
            XT = R1[0:64, kind * 2 + g, :]
            for fc in range(2):
                bank = pb[fc]
                for l in range(32):
                    k.op("pe", lambda e, l=l, fc=fc, bank=bank, XT=XT: e.matmul(bank[:, 0:255], w1s[:, l, fc * 128:(fc + 1) * 128], XT[:, l:l + 16 * 254 + 1:16],
                                                                            start=(l == 0), stop=False, skip_group_check=True),
                         reads=[R2_b, R1_b], writes=[bank], same_ok=True)
                for l in range(32):
                    k.op("pe", lambda e, l=l, fc=fc, bank=bank: e.matmul(bank[:, 255:256], w1s[:, l, fc * 128:(fc + 1) * 128], posT16[:, kind * 32 + l:kind * 32 + l + 1],
                                                                     start=False, stop=(l == 31), skip_group_check=True),
                         reads=[R2_b, posT16], writes=[bank], same_ok=True)
                u, t2_, t3_ = gel[0], gel[1], gel[2]
                k.op("dve", lambda e, fc=fc, bank=bank: e.tensor_tensor(out=cb1[:, fc:fc + 1], in0=bank[:, 255:256], in1=b1c[:, kind * 2 + fc:kind * 2 + fc + 1], op=ALU.add),
                     reads=[bank, b1c], writes=[cb1])
                k.op("act", lambda e, fc=fc, bank=bank: e.activation(out=u[:, 0:255], in_=bank[:, 0:255], func=AF.Identity, bias=cb1[:, fc:fc + 1], scale=1.0),
                     reads=[bank, cb1], writes=[u])
                k.op("dve", lambda e: e.tensor_tensor(out=t2_[:, 0:255], in0=u[:, 0:255], in1=u[:, 0:255], op=ALU.mult), reads=[u], writes=[t2_])
                k.op("dve", lambda e: e.tensor_scalar(t2_[:, 0:255], t2_[:, 0:255], 0.044715, 1.0, op0=ALU.mult, op1=ALU.add), reads=[t2_], writes=[t2_])
                k.op("dve", lambda e: e.tensor_tensor(out=t2_[:, 0:255], in0=t2_[:, 0:255], in1=u[:, 0:255], op=ALU.mult), reads=[t2_, u], writes=[t2_])
                k.op("act", lambda e: e.activation(out=t3_[:, 0:255], in_=t2_[:, 0:255], func=AF.Sigmoid, scale=GC), reads=[t2_], writes=[t3_])
                k.op("dve", lambda e, fc=fc: e.tensor_tensor(out=hidT[:, fc, 0:255], in0=t3_[:, 0:255], in1=u[:, 0:255], op=ALU.mult), reads=[t3_, u], writes=[hidT])
            if kind == 0:
                for fc in range(2):
                    k.op("pe", lambda e, fc=fc: e.matmul(pb[2][:, 0:255], w2d[:, 0, fc, :], hidT[:, fc, 0:255], start=(fc == 0), stop=(fc == 1)),
                         reads=[w2d, hidT], writes=[pb[2]], same_ok=True)
                k.op("act", lambda e, g=g: e.activation(out=KCMP[:, g, 0:255], in_=pb[2][:, 0:255], func=AF.Identity, bias=b2c[:, 0:1], scale=1.0),
                     reads=[pb[2], b2c], writes=[KCMP])
            else:
                for c in range(2):
                    nn = 128
                    for fc in range(2):
                        k.op("pe", lambda e, fc=fc, c=c, nn=nn: e.matmul(pb[3][0:nn, c * 64:(c + 1) * 64], hidT[:, fc, c * 128:c * 128 + nn], w2d[:, 1, fc, 0:64],
                                                                        start=(fc == 0 and c == 0), stop=(fc == 1), skip_group_check=True),
                             reads=[w2d, hidT], writes=[pb[3]], same_ok=True)
                k.op("pool", lambda e, g=g: e.memset(VCX[:, 1, g, 0:64], 0.0), writes=[VCX])
                k.op("dve", lambda e, g=g: e.tensor_tensor(out=VCX[:, 0, g, 0:64], in0=pb[3][:, 0:64], in1=b2B[:], op=ALU.add), reads=[pb[3], b2B], writes=[VCX])
                k.op("dve", lambda e, g=g: e.tensor_tensor(out=VCX[0:127, 1, g, 0:64], in0=pb[3][0:127, 64:128], in1=b2B[0:127, :], op=ALU.add), reads=[pb[3], b2B], writes=[VCX])

    if PH < 3:
        k.dma("sp", o_d.t.ap()[0:128, 0:256], KCMP[:, 0, :], reads=[KCMP], writes=[o_d])
        k.dma("sp", o_d.t.ap()[128:256, 0:256], KCMP[:, 1, :], reads=[KCMP], writes=[o_d])
        k.dma("sp", o_d.t.ap()[256:384, 0:258], VCX[:, 0, :, :].rearrange("p b c -> p (b c)"), reads=[VCX], writes=[o_d])
        k.dma("sp", o_d.t.ap()[384:512, 0:258], VCX[:, 1, :, :].rearrange("p b c -> p (b c)"), reads=[VCX], writes=[o_d])
        k.finish([o_d])
        return k

    QZn = [k.sb([128, 512], BF16, f"QZn{i_}") for i_ in range(2)]
    for qz_ in QZn:
        k.op("pool", lambda e, qz_=qz_: e.memset(qz_[:], 0.0), writes=[qz_])

    def qz_pre(g, i, n):
        qz_ = QZn[n % 2]

        def pre():
            for hp in range(2):
                rows = slice(hp * 64, (hp + 1) * 64)
                dst = qz_[rows, :].rearrange("p (b h q) -> p b h q", b=2, h=2)[:, :, hp, :]
                k.op("pool", lambda e, dst=dst, rows=rows: e.tensor_copy(dst, QT[rows, 2 * g:2 * g + 2, i * 128:(i + 1) * 128]), reads=[QT_b[i]], writes=[qz_])
        return pre

    def qk_mms(ktype, g, c, i):
        n = g * NTT + i
        qz_ = QZn[n % 2]
        if ktype == "c":
            lh = KCMP[:, g, c * 128:(c + 1) * 128]; rd = [KCMP, qz_]
        else:
            kb = (0 if ktype == "s" else 2) + g
            lh = KT[:, kb, c * 128:(c + 1) * 128]; rd = [KT_b[c], qz_]
        return [(lh, qz_[:, :], rd, (lambda bank: bank[:, :]))]

    def full(bank):
        return bank[:, :].rearrange("p (r q) -> p r q", r=4)

    def bcast(ap2d, parts):
        return ap2d.unsqueeze(1).to_broadcast([parts, 4, 128])

    def addbias(mm, lh, rh2d, parts, rd):
        mm.append((lh, bcast(rh2d, parts), rd, full))

    steps = []

    def cmp_steps(g, i):
        n = g * NTT + i
        nch = 2 if i >= 16 else 1
        out = []
        for c in range(nch):
            mm = qk_mms("c", g, c, i)
            off = 128 * i - 2048 * c
            addbias(mm, id16[:], bc[:, off:off + 128], 128, [id16, bc])

            def fin(g=g, i=i, n=n):
                A, B = pb[2], pb[3]
                cnt = [0]
                lim = int(os.environ.get('FINOPS', 1000))
                def kop(*a_, **kw_):
                    cnt[0] += 1
                    if cnt[0] <= lim: k.op(*a_, **kw_)
                oc = [A[:, 0:129], A[:, 129:258], B[:, 0:129], B[:, 129:258]]
                bk = [A, A, B, B]
                F = [fsc]
                for r_ in range(4):
                    kop("dve", lambda e, r_=r_: e.tensor_scalar(fsc[:, r_:r_ + 1], oc[r_][:, 128:129], 1e-30, None, op0=ALU.add), reads=[bk[r_]] + F, writes=F)
                kop("dve", lambda e: e.reciprocal(fsc[:, 0:4], fsc[:, 0:4]), reads=F, writes=F)
                kop("dve", lambda e: e.tensor_scalar(imp[:], oc[0][:, 64:128], fsc[:, 0:1], None, op0=ALU.mult), reads=[A] + F, writes=[imp])
                for r_ in range(1, 4):
                    kop("dve", lambda e, r_=r_: e.scalar_tensor_tensor(out=imp[:], in0=oc[r_][:, 64:128], scalar=fsc[:, r_:r_ + 1], in1=imp[:], op0=ALU.mult, op1=ALU.add),
                         reads=[bk[r_], imp] + F, writes=[imp])
                gv = gates[:, i, g * 12:(g + 1) * 12].rearrange("p (r t) -> p r t", t=3)
                kop("dve", lambda e: e.tensor_tensor(out=fsc[:, 4:8], in0=fsc[:, 0:4], in1=gv[:, :, 0], op=ALU.mult), reads=F + [gates], writes=F)
                for r_ in range(4):
                    kop("act", lambda e, r_=r_: e.activation(out=oacc[n % 2][:, r_, :], in_=oc[r_][:, 0:64], func=AF.Copy, scale=fsc[:, 4 + r_:5 + r_]),
                         reads=[bk[r_]] + F, writes=[oacc[n % 2]])
                W = [selw, imp, tft]
                val = selw[:, 0:64]; m8a = selw[:, 64:72]; val2 = selw[:, 72:136]; m8b = selw[:, 136:144]
                kop("dve", lambda e: e.tensor_tensor(out=val, in0=imp[:], in1=tft[:, 64 - 2 * i:128 - 2 * i], op=ALU.max), reads=W, writes=[selw])
                kop("dve", lambda e: e.tensor_tensor(out=val, in0=val, in1=tft[:, 128 + 64 - 2 * i:128 + 128 - 2 * i], op=ALU.min), reads=W, writes=[selw])
                kop("dve", lambda e: e.memset(val[:, 0:1], 1e6), reads=W, writes=[selw])
                if 'NOSEL' in os.environ:
                    kop("dve", lambda e: e.memset(val2, 1.0), reads=W, writes=[selw])
                else:
                    kop("dve", lambda e: e.max(out=m8a, in_=val), reads=W, writes=[selw])
                    kop("dve", lambda e: e.match_replace(out=val2, in_to_replace=m8a, in_values=val, imm_value=-1e30), reads=W, writes=[selw])
                    kop("dve", lambda e: e.max(out=m8b, in_=val2), reads=W, writes=[selw])
                    kop("dve", lambda e: e.tensor_scalar(val2, val, m8b[:, 7:8], None, op0=ALU.is_ge), reads=W, writes=[selw])
                kop("dve", lambda e: e.tensor_scalar(val2, val2, -NEG, NEG, op0=ALU.mult, op1=ALU.add), reads=W, writes=[selw])
                def tail(n=n):
                    k.op("pe", lambda e: e.transpose(pb[3][0:64, 384:512], val2, id32[:]), reads=[selw, id32], writes=[pb[3]])
                    k.op("act", lambda e: e.copy(MST[n % 2][0:64, :], pb[3][0:64, 384:512]), reads=[pb[3]], writes=[MST[n % 2]])
                return (n, tail)

            ob = (pb[2], pb[3])
            out.append(dict(qk=mm, v=VCX[:, c, g, :], vr=[VCX], **({"pre": qz_pre(g, i, n)} if c == 0 else {}),
                            o=(lambda sub, ob=ob: (ob[sub // 2][:, (sub % 2) * 129:(sub % 2) * 129 + 129], ob[sub // 2])),
                            first=(c == 0), last=(c == nch - 1), fin=fin, sf=(lambda sub: sub % 2 == 0)))
        return out

    def sw_steps(g, i):
        n = g * NTT + i
        out = []
        for c in range(i + 1):
            mm = qk_mms("s", g, c, i)
            addbias(mm, oh[:, c * 128:(c + 1) * 128], MST[n % 2][:], 128, [oh, MST[n % 2]])
            if c == i:
                addbias(mm, id16[:], cbt[:, 0:128], 128, [id16, cbt])
            out.append(dict(qk=mm, v=Vs[:, c, g * 65:(g + 1) * 65], vr=[V_b[c]], **({"mst_job": n} if c == 0 else {}),
                            o=(lambda sub: (pb[4][:, sub * 65:(sub + 1) * 65], pb[4])),
                            first=(c == 0), last=(c == i), fin=(lambda: None), sf=(lambda sub: sub == 0)))
        c0 = max(0, i - 4)
        for c in range(c0, i + 1):
            mm = qk_mms("w", g, c, i)
            if c == i:
                addbias(mm, id16[:], cbt[:, 0:128], 128, [id16, cbt])
            if c == i - 4:
                addbias(mm, id16[:], cbt[:, 128:256], 128, [id16, cbt])

            def fin(g=g, i=i, n=n):
                F = [fsc]
                osv = pb[4][:, 0:260].rearrange("p (r d) -> p r d", r=4)
                owv = pb[5][:, 0:260].rearrange("p (r d) -> p r d", r=4)
                gv = gates[:, i, g * 12:(g + 1) * 12].rearrange("p (r t) -> p r t", t=3)
                k.op("dve", lambda e: e.tensor_scalar(fsc[:, 8:12], osv[:, :, 64], 1e-30, None, op0=ALU.add), reads=[pb[4]] + F, writes=F)
                k.op("dve", lambda e: e.tensor_scalar(fsc[:, 12:16], owv[:, :, 64], 1e-30, None, op0=ALU.add), reads=[pb[5]] + F, writes=F)
                k.op("dve", lambda e: e.reciprocal(fsc[:, 8:16], fsc[:, 8:16]), reads=F, writes=F)
                k.op("dve", lambda e: e.tensor_tensor(out=fsc[:, 8:12], in0=fsc[:, 8:12], in1=gv[:, :, 1], op=ALU.mult), reads=F + [gates], writes=F)
                k.op("dve", lambda e: e.tensor_tensor(out=fsc[:, 12:16], in0=fsc[:, 12:16], in1=gv[:, :, 2], op=ALU.mult), reads=F + [gates], writes=F)
                oa = oacc[n % 2]
                for r_ in range(4):
                    h = 4 * g + r_
                    k.op("dve", lambda e, r_=r_: e.scalar_tensor_tensor(out=oa[:, r_, :], in0=osv[:, r_, 0:64], scalar=fsc[:, 8 + r_:9 + r_], in1=oa[:, r_, :], op0=ALU.mult, op1=ALU.add),
                         reads=[pb[4], oa] + F, writes=[oa])
                    k.op("dve", lambda e, r_=r_, h=h: e.scalar_tensor_tensor(out=oo[:, i, h * 64:(h + 1) * 64], in0=owv[:, r_, 0:64], scalar=fsc[:, 12 + r_:13 + r_], in1=oa[:, r_, :], op0=ALU.mult, op1=ALU.add),
                         reads=[pb[5], oa] + F, writes=[oo_b[i], R1_b])

            out.append(dict(qk=mm, v=Vw[:, c, g * 65:(g + 1) * 65], vr=[V_b[c]],
                            o=(lambda sub: (pb[5][:, sub * 65:(sub + 1) * 65], pb[5])),
                            first=(c == c0), last=(c == i), fin=(fin if c == i else (lambda: None)), sf=(lambda sub: sub == 0)))
        return out

    jobs = [(g, i) for g in range(2) for i in range(NTT)][:int(os.environ.get('NJ', 64))]
    ONLY = os.environ.get('ONLY', '')
    if jobs: steps += cmp_steps(*jobs[0])
    for n in range(len(jobs)):
        if n + 1 < len(jobs):
            steps += cmp_steps(*jobs[n + 1])
        if ONLY != 'cmp': steps += sw_steps(*jobs[n])
    if steps: run_steps2(k, steps, pb, PT)

    for tt in range(NTT):
        k.dma("sp" if tt % 2 == 0 else "act", o_d(tt)[0], oo[:, tt, :], reads=[oo_b[tt], R1_b], writes=[o_d(tt)[1]])
    k.pop_scope()
    return


D = 1024
NT = 16
TOK = NT * 128
EPS = 1e-6


def emit_B(k, pb, l, last_layer, even, x_d, G_o, out_d, modc, gsc, selp):
    k.push_scope()
    n2c_d = k.dram_once("n2c_%d" % l, [128, 8], F32, "ExternalInput")
    wout_d = k.dram_once("wout_%d" % l, [D, D], F32, "ExternalInput")
    wr_d = k.dram_once("wr_%d" % l, [D, 20], F32, "ExternalInput")
    br_d = k.dram_once("br_%d" % l, [1, 20], F32, "ExternalInput")
    wgate_d = k.dram_once("wgate_%d" % l, [16, D, 256], F32, "ExternalInput")
    wup_d = k.dram_once("wup_%d" % l, [16, D, 256], F32, "ExternalInput")
    wdown_d = k.dram_once("wdown_%d" % l, [16, 256, D], F32, "ExternalInput")
    sel_d = k.dram_once("sel", [32, 16 * 128], BF16)
    id32_d = k.dram_once("id32", [128, 128], F32)
    id16_d = k.dram_once("id16", [128, 128], BF16)
    if last_layer:
        fg_d = k.dram_once("fg", [1, D], F32)

    xa = k.sb([128, NT, D], F32, "xa")
    xa_b = [Buf(f"xa{i}") for i in range(NT)]
    h2T = k.sb([128, 8, TOK], BF16, "h2T")
    h2T_b = [Buf(f"h2T{i}") for i in range(NT)]
    aT = k.sb([128, 4, TOK], BF16, "aT")
    aT_b = [Buf(f"aT{i}") for i in range(4)]
    wbig = k.sb([128, 8, D], BF16, "wbig")
    wbig_b = [Buf("wbigA"), Buf("wbigB")]
    wgu = [k.sb([128, 8, 512], BF16, f"wgu{i}") for i in range(2)]
    stage = [k.sb([128, D], F32, f"stage{i}") for i in range(2)]
    scr = k.sb([128, 24 * 1024 // 4], F32, "scr")
    gB = k.sb([128, D], F32, "gB")
    g1B = g2B = gB
    ocands = [[k.sb([128, D], BF16, f"ocand{jj}_{i}") for i in range(2)] for jj in range(2)]
    combT = k.sb([32, TOK], BF16, "combT")
    combT_b = [Buf(f"combT{i}") for i in range(4)]
    sel = k.sb([32, 16 * 128], BF16, "sel")
    id32 = k.sb([128, 128], F32, "id32")
    id16 = k.sb([128, 128], BF16, "id16")
    n2c = k.sb([128, 8], F32, "n2c")
    a2 = k.sb([128, 8], F32, "a2")
    wr = k.sb([128, 8, 20], F32, "wr")
    br = k.sb([1, 20], F32, "br")
    ones1 = k.sb([1, 128], F32, "ones1")
    smalls = [k.sb([128, 64], F32, f"small{i}") for i in range(2)]
    lgs = [k.sb([128, 20], F32, f"lg{i}") for i in range(2)]
    rts = [k.sb([128, 80], F32, f"rt{i}") for i in range(2)]
    chis = [k.sb([128, 16], BF16, f"chi{i}") for i in range(2)]


    k.dma("sp", sel[:], sel_d.t.ap(), writes=[sel])
    k.dma("sp", id32[:], id32_d.t.ap(), writes=[id32])
    k.dma("sp", id16[:], id16_d.t.ap(), writes=[id16])
    k.dma("sp", n2c[:], n2c_d.t.ap(), writes=[n2c])
    k.dma("sp", wr[:], wr_d.t.ap().rearrange("(c p) n -> p c n", p=128), writes=[wr])
    k.dma("sp", br[:], br_d.t.ap(), writes=[br])
    k.op("dve", lambda e: e.memset(ones1[:], 1.0), writes=[ones1])
    k.op("dve", lambda e: e.scalar_tensor_tensor(out=a2[:], in0=modc[:, 32:40], scalar=1.0, in1=n2c[:], op0=ALU.add, op1=ALU.mult),
         reads=[modc, n2c], writes=[a2])
    sh2 = modc[:, 24:32]

    k.dma("sp", gB[:], gsc.t.ap()[0], reads=[gsc], writes=[gB])
    for c in range(8):
        st = stage[c % 2]
        if even:
            r_, cc_ = c // 4, c % 4
            row0 = (256 * r_ + 128 * cc_) if cc_ < 2 else (512 + 256 * r_ + 128 * (cc_ - 2))
        else:
            row0 = c * 128
        k.dma("sp" if c % 2 == 0 else "act", st[:], wout_d.t.ap()[row0:row0 + 128, :], writes=[st])
        k.op("pool", lambda e, c=c, st=st: e.tensor_tensor(out=wbig[:, c, :], in0=st[:], in1=g1B[:], op=ALU.mult),
             reads=[st, g1B], writes=[wbig_b[0], wbig_b[1]])

    def scr_view(off_bytes, nbytes, dt, shape_tail):
        n32 = nbytes // 4
        ap = scr[:, off_bytes // 4: off_bytes // 4 + n32]
        if dt is not F32:
            ap = ap.bitcast(dt)
        return ap

    o_t = [scr_view(i * 2048, 2048, BF16, None) for i in range(2)]
    o_tb = [Buf("o0"), Buf("o1")]
    oT_t = [scr_view(4096 + i * 2048, 2048, BF16, None) for i in range(2)]
    oT_tb = [Buf("oT0"), Buf("oT1")]
    xn_ts = [scr_view(8192 + i * 4096, 4096, F32, None) for i in range(2)]
    xn_bs = [Buf("xn0"), Buf("xn1")]
    hT32_t = [scr_view(16384 + i * 4096, 4096, F32, None) for i in range(2)]
    hT32_b = [Buf("hT32_0"), Buf("hT32_1")]
    sg_t = [scr_view(i * 2048, 2048, F32, None) for i in range(2)]
    t1_t = [scr_view(4096 + i * 2048, 2048, F32, None) for i in range(2)]

    RBATCH = os.environ.get('RBATCH', '1') == '1'
    LGg = k.sb([128, NT, 4], F32, "LGg")
    LGe = k.sb([128, NT, 16], F32, "LGe")

    def load_B(tt):
        oc = ocands[tt % 2]
        k.dma("sp", xa[:, tt, :], x_d(tt)[0], reads=[x_d(tt)[1]], writes=[xa_b[tt]])
        for h_ in range(2):
            for r_ in range(2):
                row0 = r_ * 2048 + tt * 128
                k.dma("act" if r_ == 0 else "sp", oc[h_][:, r_ * 512:(r_ + 1) * 512], G_o[h_].t.ap()[row0:row0 + 128, :], reads=[G_o[h_]], writes=[oc[h_]])

    def tileB(tt):
        j = tt % 2
        small = smalls[j]; lg = lgs[j]; rt = rts[j]; chi = chis[j]
        xn_t = xn_ts[j]; xn_b = xn_bs[j]
        ocand = ocands[j]
        if tt == 0:
            load_B(0)
        if tt + 1 < NT:
            load_B(tt + 1)
        k.op("dve", lambda e, j=j: e.tensor_scalar(o_t[j], ocand[0][:], selp[:, 0:1], None, op0=ALU.mult), reads=[ocand[0], selp], writes=[o_tb[j]])
        k.op("dve", lambda e, j=j: e.scalar_tensor_tensor(out=o_t[j], in0=ocand[1][:], scalar=selp[:, 1:2], in1=o_t[j], op0=ALU.mult, op1=ALU.add),
             reads=[ocand[1], selp, o_tb[j]], writes=[o_tb[j]])
        yield
        pT = pb[j][:].bitcast(BF16)
        for c in range(8):
            k.op("pe", lambda e, c=c, pT=pT, j=j: e.transpose(pT[:, c * 128:(c + 1) * 128], o_t[j][:, c * 128:(c + 1) * 128], id16[:]),
                 reads=[o_tb[j], id16], writes=[pb[j]], same_ok=True)
        k.op("act", lambda e, pT=pT, j=j: e.copy(oT_t[j], pT), reads=[pb[j]], writes=[oT_tb[j]])
        yield
        for dh in range(2):
            for c in range(8):
                k.op("pe", lambda e, c=c, dh=dh, j=j: e.matmul(pb[2 + dh][:], oT_t[j][:, c * 128:(c + 1) * 128], wbig[:, c, dh * 512:(dh + 1) * 512],
                                                               start=(c == 0), stop=(c == 7)),
                     reads=[oT_tb[j], wbig_b[0], wbig_b[1]], writes=[pb[2 + dh]], same_ok=True)
            k.op("dve", lambda e, dh=dh, tt=tt: e.tensor_tensor(out=xa[:, tt, dh * 512:(dh + 1) * 512], in0=pb[2 + dh][:], in1=xa[:, tt, dh * 512:(dh + 1) * 512], op=ALU.add),
                 reads=[pb[2 + dh], xa_b[tt]], writes=[xa_b[tt]])
        yield
        k.op("dve", lambda e: e.memset(small[:, 0:2], 0.0), writes=[small])
        k.op("act", lambda e, tt=tt, j=j: e.activation(out=oT_t[j], in_=xa[:, tt, :], func=AF.Square, accum_out=small[:, 0:1]),
             reads=[xa_b[tt], small], writes=[oT_tb[j], small])
        k.op("act", lambda e: e.activation(out=small[:, 1:2], in_=small[:, 0:1], func=AF.Sqrt, scale=1.0 / D, bias=EPS),
             reads=[small], writes=[small])
        yield
        k.op("dve", lambda e: e.reciprocal(small[:, 2:3], small[:, 1:2]), reads=[small], writes=[small])
        k.op("dve", lambda e, tt=tt: e.tensor_scalar(xn_t, xa[:, tt, :], small[:, 2:3], None, op0=ALU.mult),
             reads=[xa_b[tt], small], writes=[xn_b])
        yield
        for c in range(8):
            bank = pb[4 + c // 4]
            k.op("pe", lambda e, c=c, bank=bank: e.transpose(bank[:, (c % 4) * 128:(c % 4 + 1) * 128], xn_t[:, c * 128:(c + 1) * 128], id32[:]),
                 reads=[xn_b, id32], writes=[bank], same_ok=True)
        yield
        for c in range(8):
            bank = pb[4 + c // 4]
            src = bank[:, (c % 4) * 128:(c % 4 + 1) * 128]
            k.op("act", lambda e, c=c, src=src, j=j: e.activation(out=hT32_t[j][:, c * 128:(c + 1) * 128], in_=src, func=AF.Identity,
                                                                 scale=a2[:, c:c + 1], bias=sh2[:, c:c + 1]),
                 reads=[bank, a2, modc], writes=[hT32_b[j]])
        yield
        k.op("dve", lambda e, tt=tt, j=j: e.tensor_copy(h2T[:, :, tt * 128:(tt + 1) * 128], hT32_t[j].rearrange("p (c t) -> p c t", c=8)),
             reads=[hT32_b[j]], writes=[h2T_b[tt]])
        for c in range(8):
            k.op("pe", lambda e, c=c, j=j: e.matmul(pb[6][:, 0:20], hT32_t[j][:, c * 128:(c + 1) * 128], wr[:, c, :], start=(c == 0), stop=False),
                 reads=[hT32_b[j], wr], writes=[pb[6]], same_ok=True)
        k.op("pe", lambda e: e.matmul(pb[6][:, 0:20], ones1[:], br[:], start=False, stop=True), reads=[ones1, br], writes=[pb[6]], same_ok=True)
        yield
        if RBATCH:
            k.op("dve", lambda e, tt=tt: e.tensor_copy(LGg[:, tt, :], pb[6][:, 0:4]), reads=[pb[6]], writes=[LGg])
            k.op("dve", lambda e, tt=tt: e.tensor_copy(LGe[:, tt, :], pb[6][:, 4:20]), reads=[pb[6]], writes=[LGe])
            yield
            return
        k.op("dve", lambda e: e.tensor_copy(lg[:], pb[6][:, 0:20]), reads=[pb[6]], writes=[lg])
        gmax = rt[:, 0:1]; ngmax = rt[:, 1:2]; gsum = rt[:, 2:3]; gw = rt[:, 3:4]
        goh = rt[:, 4:8]; pen = rt[:, 8:12]; gexp = rt[:, 12:16]
        elm = rt[:, 16:32]; eq1 = rt[:, 32:48]; eq2 = rt[:, 48:64]
        v1 = rt[:, 64:65]; v2 = rt[:, 65:66]; dd = rt[:, 66:67]; e2 = rt[:, 67:68]; w1 = rt[:, 68:69]; c1 = rt[:, 69:70]; c2 = rt[:, 70:71]
        comb = small[:, 16:32]; chi32 = small[:, 32:48]; c2x = small[:, 32:64]
        R = [lg, rt]
        k.op("dve", lambda e: e.reduce_max(gmax, lg[:, 0:4], axis=AX.X), reads=R, writes=[rt])
        k.op("dve", lambda e: e.tensor_scalar(ngmax, gmax, -1.0, None, op0=ALU.mult), reads=R, writes=[rt])
        k.op("dve", lambda e: e.memset(gsum, 0.0), reads=R, writes=[rt])
        k.op("act", lambda e: e.activation(out=gexp, in_=lg[:, 0:4], func=AF.Exp, bias=ngmax, scale=1.0, accum_out=gsum), reads=R, writes=[rt])
        k.op("dve", lambda e: e.tensor_scalar(goh, lg[:, 0:4], gmax, None, op0=ALU.is_ge), reads=R, writes=[rt])
        k.op("dve", lambda e: e.tensor_scalar(pen, goh, 1e30, -1e30, op0=ALU.mult, op1=ALU.add), reads=R, writes=[rt])
        k.op("dve", lambda e: e.tensor_tensor(out=elm.rearrange("p (g j) -> p g j", g=4), in0=lg[:, 4:20].rearrange("p (g j) -> p g j", g=4),
                                              in1=pen.unsqueeze(2).to_broadcast([128, 4, 4]), op=ALU.add), reads=R, writes=[rt])
        k.op("dve", lambda e: e.reduce_max(v1, elm, axis=AX.X), reads=R, writes=[rt])
        k.op("dve", lambda e: e.tensor_scalar(eq1, elm, v1, None, op0=ALU.is_ge), reads=R, writes=[rt])
        k.op("dve", lambda e: e.scalar_tensor_tensor(out=eq2, in0=eq1, scalar=-2e30, in1=elm, op0=ALU.mult, op1=ALU.add), reads=R, writes=[rt])
        k.op("dve", lambda e: e.reduce_max(v2, eq2, axis=AX.X), reads=R, writes=[rt])
        k.op("dve", lambda e: e.tensor_scalar(eq2, eq2, v2, None, op0=ALU.is_ge), reads=R, writes=[rt])
        k.op("dve", lambda e: e.tensor_tensor(out=dd, in0=v2, in1=v1, op=ALU.subtract), reads=R, writes=[rt])
        k.op("act", lambda e: e.activation(out=e2, in_=dd, func=AF.Exp), reads=R, writes=[rt])
        yield
        k.op("dve", lambda e: e.reciprocal(gw, gsum), reads=R, writes=[rt])
        k.op("dve", lambda e: e.tensor_scalar(w1, e2, 1.0, None, op0=ALU.add), reads=R, writes=[rt])
        k.op("dve", lambda e: e.reciprocal(w1, w1), reads=R, writes=[rt])
        k.op("dve", lambda e: e.tensor_tensor(out=c1, in0=w1, in1=gw, op=ALU.mult), reads=R, writes=[rt])
        k.op("dve", lambda e: e.tensor_tensor(out=c2, in0=c1, in1=e2, op=ALU.mult), reads=R, writes=[rt])
        k.op("dve", lambda e: e.tensor_scalar(comb, eq1, c1, None, op0=ALU.mult), reads=R + [small], writes=[small])
        k.op("dve", lambda e: e.scalar_tensor_tensor(out=comb, in0=eq2, scalar=c2, in1=comb, op0=ALU.mult, op1=ALU.add), reads=R + [small], writes=[small])
        k.op("dve", lambda e: e.tensor_copy(chi[:], comb), reads=[small], writes=[chi])
        k.op("dve", lambda e: e.tensor_copy(chi32, chi[:]), reads=[chi, small], writes=[small])
        k.op("dve", lambda e: e.tensor_tensor(out=small[:, 48:64], in0=comb, in1=chi32, op=ALU.subtract), reads=[small], writes=[small])
        k.op("dve", lambda e: e.tensor_copy(chi[:], small[:, 48:64]), reads=[small], writes=[chi])
        k.op("dve", lambda e: e.tensor_copy(small[:, 48:64], chi[:]), reads=[chi, small], writes=[small])
        k.op("pe", lambda e: e.transpose(pb[7][0:32, 0:128], c2x, id32[:]), reads=[small, id32], writes=[pb[7]])
        k.op("act", lambda e, tt=tt: e.copy(combT[:, tt * 128:(tt + 1) * 128], pb[7][0:32, 0:128]), reads=[pb[7]], writes=[combT_b[tt // 4]])
        yield

    run_streams([tileB(tt) for tt in range(NT)], int(os.environ.get('LAGB', 3)))

    if RBATCH:
        k.barrier()
        RS = Buf("RS")

        def rs(off, n):
            return scr[:, 2048 + off:2048 + off + n]
        gmax = rs(0, 16); gsum = rs(16, 16); gw = rs(32, 16); v1 = rs(48, 16); v2 = rs(64, 16); dd = rs(80, 16)
        e2 = rs(96, 16); w1 = rs(112, 16); c1 = rs(128, 16); c2 = rs(144, 16)
        Gs = rs(256, 64); goh = rs(320, 64); pen = rs(384, 64)
        elm = rs(512, 256); eq1 = rs(768, 256); eq2 = rs(1024, 256); comb = rs(1280, 256); tmp = rs(1536, 256)
        C2 = rs(2048, 512)
        chi = scr[:, 2048 + 2560:2048 + 2560 + 128].bitcast(BF16)

        def v3(ap, m):
            return ap.rearrange("p (a b) -> p a b", b=m)

        def bc(ap, n, m):
            return ap.unsqueeze(2).to_broadcast([128, n, m])

        def dv(fn, reads=()):
            k.op("dve", fn, reads=list(reads) + [RS], writes=[RS])
        dv(lambda e: e.tensor_reduce(out=gmax, in_=LGg[:, :, :], axis=AX.X, op=ALU.max), [LGg])
        dv(lambda e: e.tensor_tensor(out=v3(Gs, 4), in0=LGg[:, :, :], in1=bc(gmax, NT, 4), op=ALU.subtract), [LGg])
        k.op("act", lambda e: e.activation(out=Gs, in_=Gs, func=AF.Exp), reads=[RS], writes=[RS])
        dv(lambda e: e.tensor_reduce(out=gsum, in_=v3(Gs, 4), axis=AX.X, op=ALU.add))
        dv(lambda e: e.reciprocal(gw, gsum))
        dv(lambda e: e.tensor_tensor(out=v3(goh, 4), in0=LGg[:, :, :], in1=bc(gmax, NT, 4), op=ALU.is_ge), [LGg])
        dv(lambda e: e.tensor_scalar(pen, goh, 1e30, -1e30, op0=ALU.mult, op1=ALU.add))
        dv(lambda e: e.tensor_tensor(out=v3(elm, 4), in0=LGe[:, :, :].rearrange("p t (g j) -> p (t g) j", j=4), in1=bc(pen, NT * 4, 4), op=ALU.add), [LGe])
        dv(lambda e: e.tensor_reduce(out=v1, in_=v3(elm, 16), axis=AX.X, op=ALU.max))
        dv(lambda e: e.tensor_tensor(out=v3(eq1, 16), in0=v3(elm, 16), in1=bc(v1, NT, 16), op=ALU.is_ge))
        dv(lambda e: e.scalar_tensor_tensor(out=eq2, in0=eq1, scalar=-2e30, in1=elm, op0=ALU.mult, op1=ALU.add))
        dv(lambda e: e.tensor_reduce(out=v2, in_=v3(eq2, 16), axis=AX.X, op=ALU.max))
        dv(lambda e: e.tensor_tensor(out=v3(eq2, 16), in0=v3(eq2, 16), in1=bc(v2, NT, 16), op=ALU.is_ge))
        dv(lambda e: e.tensor_tensor(out=dd, in0=v2, in1=v1, op=ALU.subtract))
        k.op("act", lambda e: e.activation(out=e2, in_=dd, func=AF.Exp), reads=[RS], writes=[RS])
        dv(lambda e: e.tensor_scalar(w1, e2, 1.0, None, op0=ALU.add))
        dv(lambda e: e.reciprocal(w1, w1))
        dv(lambda e: e.tensor_tensor(out=c1, in0=w1, in1=gw, op=ALU.mult))
        dv(lambda e: e.tensor_tensor(out=c2, in0=c1, in1=e2, op=ALU.mult))
        dv(lambda e: e.tensor_tensor(out=v3(comb, 16), in0=v3(eq1, 16), in1=bc(c1, NT, 16), op=ALU.mult))
        dv(lambda e: e.tensor_tensor(out=v3(tmp, 16), in0=v3(eq2, 16), in1=bc(c2, NT, 16), op=ALU.mult))
        dv(lambda e: e.tensor_tensor(out=comb, in0=comb, in1=tmp, op=ALU.add))
        C3 = v3(C2, 32)
        dv(lambda e: e.tensor_copy(chi, comb))
        dv(lambda e: e.tensor_copy(C3[:, :, 0:16], v3(chi, 16)))
        dv(lambda e: e.tensor_tensor(out=v3(tmp, 16), in0=v3(comb, 16), in1=C3[:, :, 0:16], op=ALU.subtract))
        dv(lambda e: e.tensor_copy(chi, tmp))
        dv(lambda e: e.tensor_copy(C3[:, :, 16:32], v3(chi, 16)))
        for tt in range(NT):
            bank = pb[6 + tt % 2]
            k.op("pe", lambda e, tt=tt, bank=bank: e.transpose(bank[0:32, 0:128], C3[:, tt, :], id32[:]), reads=[RS, id32], writes=[bank])
            k.op("act", lambda e, tt=tt, bank=bank: e.copy(combT[:, tt * 128:(tt + 1) * 128], bank[0:32, 0:128]), reads=[bank], writes=[combT_b[tt // 4]])

    k.dma("sp", gB[:], gsc.t.ap()[1], reads=[gsc], writes=[gB])
    cbs = [k.sb([128, 512], F32, f"cbs{i}") for i in range(2)]
    for pr in range(8):
        wd_buf = wbig_b[pr % 2]
        wd_off = (pr % 2) * 4
        for el_ in range(2):
            ex = pr * 2 + el_
            for fc in range(2):
                st = stage[(el_ * 2 + fc) % 2]
                k.dma("sp" if fc == 0 else "act", st[:], wdown_d.t.ap()[ex, fc * 128:(fc + 1) * 128, :], writes=[st])
                k.op("pool", lambda e, st=st, el_=el_, fc=fc: e.tensor_tensor(out=wbig[:, wd_off + el_ * 2 + fc, :], in0=st[:], in1=g2B[:], op=ALU.mult),
                     reads=[st, g2B], writes=[wd_buf])
        for el_ in range(2):
            ex = pr * 2 + el_
            wb = wgu[ex % 2]
            k.dma("pool", wb[:, :, 0:256], wgate_d.t.ap()[ex].rearrange("(c p) f -> p c f", p=128), writes=[wb])
            k.dma("pool", wb[:, :, 256:512], wup_d.t.ap()[ex].rearrange("(c p) f -> p c f", p=128), writes=[wb])
            for tb in range(4):
                tsl = slice(tb * 512, (tb + 1) * 512)
                hb = h2T_b[tb * 4:(tb + 1) * 4]
                cb = cbs[(ex * 4 + tb) % 2]
                k.op("pe", lambda e, ex=ex, tsl=tsl: e.matmul(pb[4][:], sel[:, ex * 128:(ex + 1) * 128], combT[:, tsl], start=True, stop=True),
                     reads=[sel, combT_b[tb]], writes=[pb[4]], same_ok=True)
                k.op("act", lambda e, cb=cb: e.copy(cb[:], pb[4][:]), reads=[pb[4]], writes=[cb])
                for fc in range(2):
                    jj = (tb * 2 + fc) % 2
                    pg = pb[0 + jj]; pu = pb[2 + jj]
                    for c in range(8):
                        k.op("pe", lambda e, c=c, fc=fc, pg=pg, wb=wb, tsl=tsl: e.matmul(pg[:], wb[:, c, fc * 128:(fc + 1) * 128], h2T[:, c, tsl], start=(c == 0), stop=(c == 7)),
                             reads=[wb] + hb, writes=[pg], same_ok=True)
                    for c in range(8):
                        k.op("pe", lambda e, c=c, fc=fc, pu=pu, wb=wb, tsl=tsl: e.matmul(pu[:], wb[:, c, 256 + fc * 128:256 + (fc + 1) * 128], h2T[:, c, tsl], start=(c == 0), stop=(c == 7)),
                             reads=[wb] + hb, writes=[pu], same_ok=True)
                    k.op("act", lambda e, jj=jj, pg=pg: e.activation(out=sg_t[jj], in_=pg[:], func=AF.Silu), reads=[pg], writes=[o_tb[jj]])
                    k.op("dve", lambda e, jj=jj, pu=pu: e.tensor_tensor(out=t1_t[jj], in0=pu[:], in1=sg_t[jj], op=ALU.mult), reads=[pu, o_tb[jj]], writes=[oT_tb[jj]])
                    k.op("dve", lambda e, jj=jj, el_=el_, fc=fc, tsl=tsl, cb=cb: e.tensor_tensor(out=aT[:, el_ * 2 + fc, tsl], in0=cb[:], in1=t1_t[jj], op=ALU.mult),
                         reads=[cb, oT_tb[jj]], writes=[aT_b[tb]])
        for tt in range(NT):
            for dh in range(2):
                py = pb[5 + (tt * 2 + dh) % 3]
                for jx in range(4):
                    k.op("pe", lambda e, jx=jx, tt=tt, dh=dh, py=py: e.matmul(py[:], aT[:, jx, tt * 128:(tt + 1) * 128], wbig[:, wd_off + jx, dh * 512:(dh + 1) * 512],
                                                                         start=(jx == 0), stop=(jx == 3)),
                         reads=[aT_b[tt // 4], wd_buf], writes=[py], same_ok=True)
                k.op("dve", lambda e, tt=tt, dh=dh, py=py: e.tensor_tensor(out=xa[:, tt, dh * 512:(dh + 1) * 512], in0=py[:], in1=xa[:, tt, dh * 512:(dh + 1) * 512], op=ALU.add),
                     reads=[py, xa_b[tt]], writes=[xa_b[tt]])

    if last_layer:
        k.dma("sp", gB[:], fg_d.t.ap()[0:1, :].to_broadcast([128, D]), writes=[gB])
    for tt in range(NT):
        if last_layer:
            small = smalls[tt % 2]
            k.op("dve", lambda e, small=small: e.memset(small[:, 0:2], 0.0), writes=[small])
            k.op("act", lambda e, tt=tt, small=small: e.activation(out=oT_t[tt % 2], in_=xa[:, tt, :], func=AF.Square, accum_out=small[:, 0:1]),
                 reads=[xa_b[tt], small], writes=[oT_tb[tt % 2], small])
            k.op("act", lambda e, small=small: e.activation(out=small[:, 1:2], in_=small[:, 0:1], func=AF.Sqrt, scale=1.0 / D, bias=EPS), reads=[small], writes=[small])
            k.op("dve", lambda e, small=small: e.reciprocal(small[:, 2:3], small[:, 1:2]), reads=[small], writes=[small])
            k.op("dve", lambda e, tt=tt, small=small: e.scalar_tensor_tensor(out=xa[:, tt, :], in0=xa[:, tt, :], scalar=small[:, 2:3], in1=gB[:], op0=ALU.mult, op1=ALU.mult),
                 reads=[xa_b[tt], small, gB], writes=[xa_b[tt]])
        k.dma("sp" if tt % 2 == 0 else "act", out_d(tt)[0], xa[:, tt, :], reads=[xa_b[tt]], writes=[out_d(tt)[1]])
    k.pop_scope()


def emit_M(k, pb, l, cs, ones4, bselB, bselT, modc, gsc):
    k.push_scope()
    w_d = k.dram_once("adaw_%d" % l, [D, 6144], F32)
    b_d = k.dram_once("adab_%d" % l, [1, 6144], F32)
    wb = [k.sb([128, 3072], F32, f"mw{i}") for i in range(3)]
    bb = k.sb([1, 6144], F32, "mbb")
    modG = k.sb([4, 6144], F32, "modG")
    k.dma("sp", bb[:], b_d.t.ap(), writes=[bb])
    n = 0
    for rnd in range(2):
        for c in range(8):
            w = wb[n % 3]; n += 1
            k.dma("sp" if c % 2 == 0 else "act", w[:], w_d.t.ap()[c * 128:(c + 1) * 128, rnd * 3072:(rnd + 1) * 3072], writes=[w])
            for blk in range(6):
                k.op("pe", lambda e, c=c, blk=blk, w=w: e.matmul(pb[blk][0:4, :], cs[:, c * 4:(c + 1) * 4], w[:, blk * 512:(blk + 1) * 512], start=(c == 0), stop=False),
                     reads=[cs, w], writes=[pb[blk]], same_ok=True)
        for blk in range(6):
            c0 = rnd * 3072 + blk * 512
            k.op("pe", lambda e, blk=blk, c0=c0: e.matmul(pb[blk][0:4, :], ones4[:], bb[:, c0:c0 + 512], start=False, stop=True),
                 reads=[ones4, bb], writes=[pb[blk]], same_ok=True)
            k.op("act", lambda e, blk=blk, c0=c0: e.copy(modG[:, c0:c0 + 512], pb[blk][0:4, :]), reads=[pb[blk]], writes=[modG])
    for idx in range(48):
        j, c = idx // 8, idx % 8
        k.op("pe", lambda e, idx=idx, j=j, c=c: e.matmul(pb[6][:, idx:idx + 1], modG[:, j * 1024 + c * 128:j * 1024 + (c + 1) * 128], bselT[:, 0:1],
                                                       start=(idx == 0), stop=(idx == 47), skip_group_check=True),
             reads=[modG, bselT], writes=[pb[6]], same_ok=True)
    k.op("dve", lambda e: e.tensor_copy(modc[:], pb[6][:, 0:48]), reads=[pb[6]], writes=[modc])
    gt = k.sb([128, 2, D], F32, "gt")
    for (gi, j, b0) in [(0, 2, 0), (1, 5, 2)]:
        for h in range(2):
            bank = pb[b0 + h]
            k.op("pe", lambda e, j=j, h=h, bank=bank: e.matmul(bank[:], bselB[:], modG[:, j * 1024 + h * 512:j * 1024 + (h + 1) * 512], start=True, stop=True),
                 reads=[bselB, modG], writes=[bank])
            k.op("act", lambda e, gi=gi, h=h, bank=bank: e.copy(gt[:, gi, h * 512:(h + 1) * 512], bank[:]), reads=[bank], writes=[gt])
    k.dma("sp", gsc.t.ap().rearrange("g p d -> p g d"), gt[:], reads=[gt], writes=[gsc])
    k.pop_scope()


def build_fused():
    k = KB()
    pb = [k.ps([128, 512], F32, f"pb{i}") for i in range(8)]
    cT_d = k.dram("cT", [128, 32], F32, "ExternalInput")
    bselB_d = k.dram("bselB", [4, 128], F32, "ExternalInput")
    bselT_d = k.dram("bselT", [4, 1], F32, "ExternalInput")
    selp_d = k.dram("selp", [128, 2], F32, "ExternalInput")
    x_in = k.dram("x", [S, D], F32, "ExternalInput")
    xown_in = k.dram("x_own", [2048, D], F32, "ExternalInput")
    xo = k.dram("xo", [2048, D], F32, "ExternalOutput")
    src_o = [k.dram(f"src_o{h}", [2048, 512], BF16, "Internal") for h in range(2)]
    G_o = [k.dram(f"G_o{h}", [4096, 512], BF16, "Internal") for h in range(2)]
    src_x = [k.dram(f"src_x{q}", [512, D], F32, "Internal") for q in range(4)]
    G_x = [k.dram(f"G_x{q}", [1024, D], F32, "Internal") for q in range(4)]

    def x_first(tt):
        return (x_in.t.ap()[tt * 128:(tt + 1) * 128, :], x_in)

    def x_gath(tt):
        r_, q_, w_ = tt // 16, (tt % 16) // 4, (tt % 4) * 128
        return (G_x[q_].t.ap()[r_ * 512 + w_:r_ * 512 + w_ + 128, :], G_x[q_])

    def o_dst(tt):
        return (src_o[tt // 16].t.ap()[(tt % 16) * 128:(tt % 16 + 1) * 128, :], src_o[tt // 16])

    def xown_first(tt):
        return (xown_in.t.ap()[tt * 128:(tt + 1) * 128, :], xown_in)

    def xown_src(tt):
        return (src_x[tt // 4].t.ap()[(tt % 4) * 128:(tt % 4 + 1) * 128, :], src_x[tt // 4])

    def xo_dst(tt):
        return (xo.t.ap()[tt * 128:(tt + 1) * 128, :], xo)

    cT = k.sb([128, 32], F32, "cT"); cs = k.sb([128, 32], F32, "cs")
    ones4 = k.sb([1, 4], F32, "ones4")
    bselB = k.sb([4, 128], F32, "bselB"); bselT = k.sb([4, 1], F32, "bselT")
    selp = k.sb([128, 2], F32, "selp")
    modc = k.sb([128, 48], F32, "modc")
    gsc = k.dram("gsc", [2, 128, D], F32, "Internal")
    for (t, d) in [(cT, cT_d), (bselB, bselB_d), (bselT, bselT_d), (selp, selp_d)]:
        k.dma("sp", t[:], d.t.ap(), writes=[t])
    k.op("dve", lambda e: e.memset(ones4[:], 1.0), writes=[ones4])
    k.op("act", lambda e: e.activation(out=cs[:], in_=cT[:], func=AF.Silu), reads=[cT], writes=[cs])
    groups = [[0, 1], [2, 3], [4, 5], [6, 7]]
    NL = int(os.environ.get("NLAYERS", 4))
    for l in range(NL):
        last = (l == NL - 1)
        STOP = os.environ.get("STOP", "")
        emit_M(k, pb, l, cs, ones4, bselB, bselT, modc, gsc)
        if STOP == "M": break
        x_src = x_first if l == 0 else x_gath
        if l % 2 == 0:
            emit_A_even(k, pb, l, 0.8 - 0.6 * math.exp(-0.3 * l), x_src, o_dst, modc)
        else:
            emit_A_odd(k, pb, l, x_src, o_dst, modc)
        if STOP == "A": break
        for h in range(2):
            k.allgather(src_o[h], G_o[h], groups)
        if STOP == "AG": break
        emit_B(k, pb, l, last and NL == 4, l % 2 == 0, xown_first if l == 0 else xown_src, G_o, xo_dst if last else xown_src, modc, gsc, selp)
        if not last:
            for q in range(4):
                k.allgather(src_x[q], G_x[q], groups)
    k.finish([xo])
    return k


_bf = ml_dtypes.bfloat16
_KF = {}


def kernel(x, c, norm1_g, norm2_g, final_g, ada_w, ada_b, ev_w_in, ev_w_out, ev_lambda, ev_subln_g,
           od_w_in, od_w_out, od_cmp_pos, od_cmp_w1, od_cmp_b1, od_cmp_w2, od_cmp_b2,
           moe_wg, moe_bg, moe_we, moe_be, moe_w_gate, moe_w_up, moe_w_down):
    f32 = lambda a: np.ascontiguousarray(np.asarray(a, dtype=np.float32))
    x = f32(x); c = f32(c)
    NL = int(os.environ.get("NLAYERS", 4))
    if "k" not in _KF:
        _KF["k"] = build_fused()
    kf = _KF["k"]
    pos = np.arange(4096, dtype=np.float32)
    inv = (np.float32(500000.0) ** (-np.arange(0, 16, 2, dtype=np.float32) / np.float32(16))).astype(np.float32)
    ang = pos[:, None] * inv[None, :]
    cosT = np.cos(ang).astype(np.float32); sinT = np.sin(ang).astype(np.float32)
    cosT = np.ascontiguousarray(cosT.reshape(32, 128, 8).transpose(1, 0, 2).reshape(128, 256))
    sinT = np.ascontiguousarray(sinT.reshape(32, 128, 8).transpose(1, 0, 2).reshape(128, 256))
    eall = np.zeros((128, 4096), np.float32)
    for n in range(16):
        eall[n, n * 256:(n + 1) * 256] = 1
    kk = np.arange(128)[:, None]; qq = np.arange(512)[None, :]
    cb = np.concatenate([np.where(128 * d + kk > qq, NEG, 0.0) for d in range(4)], axis=1).astype(np.float32)
    ptab = np.zeros((128, 96), np.float32); ptab[:, 16:32] = -1e30; ptab[:, 32:48] = 1.0; ptab[:, 64 + 16] = 1.0
    cs_ = np.arange(255) * 16; ce = cs_ + 31; ss = np.arange(64) * 64
    overlap = ((cs_[:, None] <= ss[None, :] + 63) & (ce[:, None] >= ss[None, :])).astype(np.float32)
    ov = np.zeros((256, 64), np.float32); ov[:255] = overlap
    ovl = np.concatenate([ov[0:128], ov[128:256]], axis=1)
    npp = np.arange(128)[:, None]; u = np.arange(4096)[None, :]
    bc = np.where(16 * npp + 31 <= u, 0.0, NEG).astype(np.float32)
    oh = (np.arange(4096)[None, :] // 64 == np.arange(128)[:, None]).astype(np.float32)
    k2 = np.arange(128)[:, None]; q2 = np.arange(128)[None, :]
    cbt = np.concatenate([np.where(k2 > q2, NEG, 0.0), np.where(k2 <= q2, NEG, 0.0)], axis=1).astype(np.float32)
    qp = np.arange(128)[:, None]; xx = np.arange(128)[None, :] - 64; own = qp // 64
    TF = np.where((xx == own) | (xx == own - 1), 1e6, -1e30).astype(np.float32)
    TN = np.where(xx <= own, 1e30, -1e30).astype(np.float32)
    sel = np.zeros((32, 16, 128), np.float32)
    for e in range(16):
        sel[e, e, :] = 1.0
        sel[16 + e, e, :] = 1.0
    shared = dict(cosT=cosT, sinT=sinT, id32=np.eye(128, dtype=np.float32), id16=np.eye(128, dtype=np.float32).astype(_bf),
                  eall=eall.astype(_bf), cb=cb.astype(_bf), ptab=ptab, sel=sel.reshape(32, 2048).astype(_bf),
                  cT=np.ascontiguousarray(c.T.reshape(8, 128, 4).transpose(1, 0, 2).reshape(128, 32)))
    if NL > 1:
        shared.update(ovl=ovl.astype(_bf), bc=bc.astype(_bf), oh=oh.astype(_bf), cbt=cbt.astype(_bf), tft=np.concatenate([TF, TN], axis=1))
    if NL == 4:
        shared["fg"] = f32(final_g)[None, :].copy()
    for l in range(NL):
        i = l // 2
        shared["adaw_%d" % l] = f32(ada_w[l]); shared["adab_%d" % l] = f32(ada_b[l])[None, :].copy()
        shared["n1c_%d" % l] = np.ascontiguousarray(f32(norm1_g)[l].reshape(8, 128).T)
        shared["n2c_%d" % l] = np.ascontiguousarray(f32(norm2_g)[l].reshape(8, 128).T)
        shared["wout_%d" % l] = f32(ev_w_out[i]) if l % 2 == 0 else f32(od_w_out[i])
        shared["wr_%d" % l] = np.ascontiguousarray(np.concatenate([f32(moe_wg[l]), f32(moe_we[l])], axis=1))
        shared["br_%d" % l] = np.concatenate([f32(moe_bg[l]), f32(moe_be[l])])[None, :].copy()
        shared["wgate_%d" % l] = f32(moe_w_gate[l]); shared["wup_%d" % l] = f32(moe_w_up[l]); shared["wdown_%d" % l] = f32(moe_w_down[l])
        if l % 2 == 0:
            shared["lam_%d" % l] = f32(ev_lambda[i]).reshape(1, 256).copy()
            shared["subg_%d" % l] = f32(ev_subln_g[i]).reshape(1, 128).copy()
        else:
            pos_ = f32(od_cmp_pos[i]); b1 = f32(od_cmp_b1[i]); w2 = f32(od_cmp_w2[i]); b2 = f32(od_cmp_b2[i])
            shared["w1_%d" % l] = f32(od_cmp_w1[i])
            shared["posT_%d" % l] = np.ascontiguousarray(np.concatenate([pos_[0].T, pos_[1].T], axis=1))
            shared["b1c_%d" % l] = np.ascontiguousarray(b1.reshape(2, 2, 128).transpose(2, 0, 1).reshape(128, 4))
            shared["w2d_%d" % l] = np.ascontiguousarray(np.concatenate([w2, w2], axis=-1))
            shared["b2c_%d" % l] = np.concatenate([b2[0], b2[0]])[:, None].copy()
            shared["b2r_%d" % l] = b2[1][None, :].copy()
    in_maps = []
    for core in range(8):
        b, p = core // 2, core % 2
        d = dict(shared)
        d["x"] = np.ascontiguousarray(x[b]); d["x_own"] = np.ascontiguousarray(x[b, p * 2048:(p + 1) * 2048])
        selp = np.zeros((128, 2), np.float32); selp[:, p] = 1.0
        bselB = np.zeros((4, 128), np.float32); bselB[b, :] = 1.0
        d["selp"] = selp; d["bselB"] = bselB; d["bselT"] = np.ascontiguousarray(bselB[:, 0:1])
        for l in range(NL):
            i = l // 2
            if l % 2 == 0:
                w = f32(ev_w_in[i]); sl = slice(256 * p, 256 * p + 256)
                d["winA_%d" % l] = np.ascontiguousarray(np.concatenate([w[:, 0:512][:, sl], w[:, 512:1024][:, sl], w[:, 1536:2048][:, sl], w[:, 2048:2560][:, sl],
                                                                       w[:, 1024:1536][:, sl], w[:, 2560:3072][:, sl]], axis=1))
            else:
                w = f32(od_w_in[i]); g = slice(128 * p, 128 * p + 128)
                d["winC_%d" % l] = np.ascontiguousarray(np.concatenate([w[:, 512 * p:512 * p + 512], w[:, 1024:1280][:, g], w[:, 1536:1792][:, g], w[:, 2048:2304][:, g],
                                                                       w[:, 1280:1536][:, g], w[:, 1792:2048][:, g], w[:, 2304:2560][:, g],
                                                                       w[:, 2560 + 24 * p:2560 + 24 * p + 24]], axis=1))
        in_maps.append(d)
    res = run_bass_kernel_spmd(kf.nc, in_maps, core_ids=list(range(8))).results
    out = np.zeros_like(x)
    for core in range(8):
        b, p = core // 2, core % 2
        out[b, p * 2048:(p + 1) * 2048] = res[core]["xo"]
    return out
```

```python
import math, os
import numpy as np
import ml_dtypes
from contextlib import ExitStack
import concourse.bass as bass
import concourse.mybir as mybir
from concourse.bass_utils import run_bass_kernel_spmd


F32 = mybir.dt.float32
BF16 = mybir.dt.bfloat16
AF = mybir.ActivationFunctionType
ALU = mybir.AluOpType
AX = mybir.AxisListType

SEM_ROT = 8000


class Buf:
    __slots__ = ("w", "r", "name", "excl")

    def __init__(self, name="", excl=False):
        self.w = None
        self.r = []
        self.name = name
        self.excl = excl


class T:
    def __init__(self, t, name=""):
        self.t = t
        self.b = Buf(name)

    def __getitem__(self, idx):
        return self.t[idx]


class KB:
    def __init__(self):
        self.nc = bass.Bass("TRN2", target_bir_lowering=False)
        self.es = ExitStack()
        self.scopes = []
        self.drams = {}
        nc = self.nc
        self.eng = {"pe": nc.tensor, "act": nc.scalar, "dve": nc.vector, "pool": nc.gpsimd, "sp": nc.sync}
        self.sem = {}
        self.cnt = {}
        self.seen = {e: {} for e in self.eng}
        for e in self.eng:
            self._newsem(e)
        self.dsems = [self.es.enter_context(nc.semaphore(f"dq{i}")) for i in range(24)]
        self.dcnt = [0] * len(self.dsems)
        self.dnext = 0
        self.n_inst = 0
        self.n_wait = 0

    def _newsem(self, e):
        self.sem[e] = self.es.enter_context(self.nc.semaphore(f"s_{e}_{len(self.sem)}_{self.n_inst if hasattr(self,'n_inst') else 0}"))
        self.cnt[e] = 0

    def sb(self, shape, dt=F32, name=None):
        es = self.scopes[-1] if self.scopes else self.es
        p = self.nc.sbuf_base
        st = (p + 31) // 32 * 32
        padb = (-st) % 128
        if padb:
            self._npad = getattr(self, "_npad", 0) + 1
            es.enter_context(self.nc.sbuf_tensor(f"pad_{self._npad}", [128, padb // 2], BF16))
        self._nsb = getattr(self, "_nsb", 0) + 1
        t = es.enter_context(self.nc.sbuf_tensor(f"sb{self._nsb}_" + (name or "t"), list(shape), dt))
        return T(t, name or "")

    def push_scope(self):
        self.scopes.append(ExitStack())

    def pop_scope(self):
        self.barrier()
        self.scopes.pop().close()

    def dram_once(self, name, shape, dt, kind="ExternalInput"):
        if name not in self.drams:
            self.drams[name] = self.dram(name, shape, dt, kind)
        return self.drams[name]

    def barrier(self):
        toks = []
        for e in ("pe", "act", "dve", "pool"):
            if self.cnt[e] > 0:
                toks.append((self.sem[e], self.cnt[e]))
        for i, s_ in enumerate(self.dsems):
            if self.dcnt[i] > 0:
                toks.append((s_, self.dcnt[i]))
        for (s_, v) in getattr(self, "cc_toks", []):
            toks.append((s_, v))
        for e in ("pe", "act", "dve", "pool", "sp"):
            self._wait(e, toks)

    def allgather(self, src, dst, groups):
        toks = self._deps([src], [dst])
        self._wait("pool", toks)
        if not hasattr(self, "cc_sem"):
            self.cc_sem = self.es.enter_context(self.nc.semaphore("cc_sem"))
            self.cc_cnt = 0
            self.cc_toks = []
        self.nc.gpsimd.collective_compute("AllGather", mybir.AluOpType.bypass, replica_groups=groups,
                                          ins=[src.t.ap().opt()], outs=[dst.t.ap().opt()]).then_inc(self.cc_sem)
        self.cc_cnt += 1
        tok = (self.cc_sem, self.cc_cnt)
        self.cc_toks = [tok]
        self._commit(tok, [src], [dst])
        return tok

    def ps(self, shape, dt=F32, name=None):
        t = self.es.enter_context(self.nc.psum_tensor("ps_" + name, list(shape), dt) if name else self.nc.psum_tensor(list(shape), dt))
        r = T(t, name or "")
        r.b.excl = True
        return r

    def dram(self, name, shape, dt, kind):
        if kind == "Internal":
            t = self.nc.dram_tensor(name, list(shape), dt)
        else:
            t = self.nc.dram_tensor(name, list(shape), dt, kind=kind)
        return T(t, name)

    def _deps(self, reads, writes):
        toks = []
        for b in reads:
            b = b.b if isinstance(b, T) else b
            if b.w is not None:
                toks.append(b.w)
            if b.excl:
                toks.extend(b.r)
        for b in writes:
            b = b.b if isinstance(b, T) else b
            if b.w is not None:
                toks.append(b.w)
            toks.extend(b.r)
        return toks

    def _wait(self, e, toks, skip_sem=None):
        eng = self.eng[e]
        need = {}
        for (s, v) in toks:
            if skip_sem is not None and s is skip_sem:
                continue
            if self.seen[e].get(s, 0) >= v:
                continue
            if need.get(s, 0) < v:
                need[s] = v
        for s, v in need.items():
            eng.wait_ge(s, v)
            self.seen[e][s] = v
            self.n_wait += 1

    def _commit(self, tok, reads, writes):
        for b in writes:
            b = b.b if isinstance(b, T) else b
            b.w = tok
            b.r = []
        for b in reads:
            b = b.b if isinstance(b, T) else b
            b.r.append(tok)
            if len(b.r) > 64:
                m = {}
                for (s, v) in b.r:
                    if m.get(s, (None, 0))[1] < v:
                        m[s] = (s, v)
                b.r = list(m.values())

    def op(self, e, fn, reads=(), writes=(), same_ok=False):
        if self.cnt[e] >= SEM_ROT:
            self._newsem(e)
        toks = self._deps(reads, writes)
        self._wait(e, toks, skip_sem=self.sem[e] if same_ok else None)
        ins = fn(self.eng[e])
        self.cnt[e] += 1
        tok = (self.sem[e], self.cnt[e])
        ins.then_inc(self.sem[e], 1)
        self._commit(tok, reads, writes)
        self.n_inst += 1
        return tok

    def dma(self, q, out, in_, reads=(), writes=(), **kw):
        i = self.dnext
        self.dnext = (self.dnext + 1) % len(self.dsems)
        s = self.dsems[i]
        toks = self._deps(reads, writes)
        if self.dcnt[i] > 0:
            toks.append((s, self.dcnt[i]))
        self._wait(q, toks)
        self.dcnt[i] += 16
        self.eng[q].dma_start(out=out, in_=in_, **kw).then_inc(s, 16)
        tok = (s, self.dcnt[i])
        self._commit(tok, reads, writes)
        self.n_inst += 1
        return tok

    def finish(self, outs):
        toks = []
        for b in outs:
            b = b.b if isinstance(b, T) else b
            if b.w is not None:
                toks.append(b.w)
        self._wait("sp", toks)
        for i, s_ in enumerate(self.dsems):
            if self.dcnt[i] > 0:
                self._wait("sp", [(s_, self.dcnt[i])])
        self._wait("sp", getattr(self, "cc_toks", []))
        for e in ("pe", "act", "dve", "pool"):
            if self.cnt[e] > 0:
                self._wait("sp", [(self.sem[e], self.cnt[e])])

def run_streams(gens, lag):
    active = []
    nxt = 0
    g = 0
    while nxt < len(gens) or active:
        if nxt < len(gens) and g >= nxt * lag:
            active.append(gens[nxt]); nxt += 1
        for gen in list(active):
            try:
                next(gen)
            except StopIteration:
                active.remove(gen)
        g += 1

SKIP = set(os.environ.get('SKIP', '').split(','))

D = 1024
S = 4096
NTT = 32
EPS = 1e-6
NEG = -30000.0


def norm_to_hT(k, x_ap, x_buf, small, sq_t, sq_b, xn_t, xn_b, pbA, pbB, id32, a_col, sh_col, col_bufs, outs):
    k.op("dve", lambda e: e.memset(small[:, 0:2], 0.0), writes=[small])
    k.op("act", lambda e: e.activation(out=sq_t, in_=x_ap, func=AF.Square, accum_out=small[:, 0:1]),
         reads=[x_buf, small], writes=[sq_b, small])
    k.op("act", lambda e: e.activation(out=small[:, 1:2], in_=small[:, 0:1], func=AF.Sqrt, scale=1.0 / D, bias=EPS),
         reads=[small], writes=[small])
    yield
    k.op("dve", lambda e: e.reciprocal(small[:, 2:3], small[:, 1:2]), reads=[small], writes=[small])
    k.op("dve", lambda e: e.tensor_scalar(xn_t, x_ap, small[:, 2:3], None, op0=ALU.mult),
         reads=[x_buf, small], writes=[xn_b])
    yield
    for c in range(8):
        bank = pbA if c < 4 else pbB
        k.op("pe", lambda e, c=c, bank=bank: e.transpose(bank[:, (c % 4) * 128:(c % 4 + 1) * 128], xn_t[:, c * 128:(c + 1) * 128], id32[:]),
             reads=[xn_b, id32], writes=[bank], same_ok=True)
    yield
    out_ap, out_buf = outs
    for c in range(8):
        bank = pbA if c < 4 else pbB
        src = bank[:, (c % 4) * 128:(c % 4 + 1) * 128]
        k.op("act", lambda e, c=c, src=src: e.activation(out=out_ap[:, c * 128:(c + 1) * 128], in_=src, func=AF.Identity,
                                                         scale=a_col[:, c:c + 1], bias=sh_col[:, c:c + 1]),
             reads=[bank] + col_bufs, writes=[out_buf])
    yield


def emit_A_even(k, pb, l, lambda_init, x_d, o_d, modc, PH=3):
    k.push_scope()
    n1c_d = k.dram_once("n1c_%d" % l, [128, 8], F32, "ExternalInput")
    win_d = k.dram_once("winA_%d" % l, [D, 1536], F32, "ExternalInput")
    cos_d = k.dram_once("cosT", [128, NTT * 8], F32)
    sin_d = k.dram_once("sinT", [128, NTT * 8], F32)
    lam_d = k.dram_once("lam_%d" % l, [1, 256], F32, "ExternalInput")
    sg_d = k.dram_once("subg_%d" % l, [1, 128], F32, "ExternalInput")
    id32_d = k.dram_once("id32", [128, 128], F32)
    id16_d = k.dram_once("id16", [128, 128], BF16)
    eall_d = k.dram_once("eall", [128, S], BF16)
    cb_d = k.dram_once("cb", [128, 4 * 512], BF16)
    ptab_d = k.dram_once("ptab", [128, 96], F32)

    win = k.sb([128, 8, 1536], BF16, "win")
    QKT = k.sb([128, 8, S], BF16, "QKT")
    QKT_b = [[Buf(f"QKT{j}_{t}") for t in range(8)] for j in range(8)]
    Va = k.sb([128, NTT, 4 * 65], BF16, "Va")
    Vb = k.sb([128, NTT, 2 * 129], BF16, "Vb")
    V_b = [Buf(f"V{t}") for t in range(NTT)]
    oo = k.sb([128, NTT, 512], BF16, "oo")
    oo_b = [Buf(f"oo{t}") for t in range(NTT)]
    eall = k.sb([128, S], BF16, "eall")
    cb = k.sb([128, 4 * 512], BF16, "cb")
    ptab = k.sb([128, 96], F32, "ptab")
    id32 = k.sb([128, 128], F32, "id32")
    id16 = k.sb([128, 128], BF16, "id16")
    n1c = k.sb([128, 8], F32, "n1c")
    a1 = k.sb([128, 8], F32, "a1")
    cosT = k.sb([128, NTT, 8], F32, "cosT")
    sinT = k.sb([128, NTT, 8], F32, "sinT")
    kmT = k.sb([128, 2, 16], BF16, "kmT")
    kmT32 = k.sb([128, 2, 16], F32, "kmT32")
    lamr = k.sb([1, 256], F32, "lamr")
    lams = k.sb([1, 8], F32, "lams")
    ones1 = k.sb([1, 128], F32, "ones1")
    nlam = k.sb([128, 1], F32, "nlam")
    gsl = k.sb([128, 128], F32, "gsl")
    smalls = [k.sb([128, 16], F32, f"small{i}") for i in range(2)]
    xt = [k.sb([128, D], F32, f"xt{i}") for i in range(3)]
    sq = k.sb([128, 128], BF16, "sq")
    hT = [k.sb([128, 8 * 128], BF16, f"hT{i}") for i in range(2)]
    qk16 = [k.sb([128, 1024], BF16, f"qk16_{i}") for i in range(2)]
    rtmps = [k.sb([128, 4, 128], F32, f"rtmp{i}") for i in range(2)]
    PT = [k.sb([128, 512], BF16, f"PT{i}") for i in range(2)]
    gst = k.sb([128, 64], F32, "gst")
    fsc = k.sb([128, 8], F32, "fsc")
    ob32 = k.sb([128, 4, 128], F32, "ob32")
    ob32_b = [Buf(f"ob32_{i}") for i in range(4)]
    QZ = [k.sb([128, 512], BF16, f"QZ{i}") for i in range(2)]
    qzc = [0]
    fss = [k.sb([128, 4], F32, f"fss{i}") for i in range(4)]

    MBTv = [win[:, 0:3, :].rearrange("p a b -> p (a b)")[:, 0:S], win[:, 3:6, :].rearrange("p a b -> p (a b)")[:, 0:S]]
    MBT_b = [Buf("MBT0"), Buf("MBT1")]
    for (t, d) in [(eall, eall_d), (cb, cb_d), (ptab, ptab_d), (id32, id32_d), (id16, id16_d), (n1c, n1c_d), (lamr, lam_d)]:
        k.dma("sp", t[:], d.t.ap(), writes=[t])
    k.dma("sp", cosT[:], cos_d.t.ap().rearrange("p (t f) -> p t f", f=8), writes=[cosT])
    k.dma("sp", sinT[:], sin_d.t.ap().rearrange("p (t f) -> p t f", f=8), writes=[sinT])
    k.dma("sp", gsl[:], sg_d.t.ap()[0:1, :].to_broadcast([128, 128]), writes=[gsl])
    for c in range(8):
        k.dma("pool", win[:, c, :], win_d.t.ap()[c * 128:(c + 1) * 128, :], writes=[win])
    k.op("dve", lambda e: e.memset(ones1[:], 1.0), writes=[ones1])
    k.op("dve", lambda e: e.scalar_tensor_tensor(out=a1[:], in0=modc[:, 8:16], scalar=1.0, in1=n1c[:], op0=ALU.add, op1=ALU.mult),
         reads=[modc, n1c], writes=[a1])
    sh1 = modc[:, 0:8]
    k.op("dve", lambda e: e.tensor_scalar(gsl[:], gsl[:], 1.0 - lambda_init, None, op0=ALU.mult), reads=[gsl], writes=[gsl])
    if "lam" in SKIP: lamr = k.sb([1, 256], F32, "lamr2")
    k.op("dve", lambda e: e.memset(lams[:], 0.0), writes=[lams])
    k.op("dve", lambda e: e.tensor_tensor(out=lamr[:, 0:64], in0=lamr[:, 0:64], in1=lamr[:, 64:128], op=ALU.mult), reads=[lamr], writes=[lamr])
    k.op("dve", lambda e: e.tensor_tensor(out=lamr[:, 128:192], in0=lamr[:, 128:192], in1=lamr[:, 192:256], op=ALU.mult), reads=[lamr], writes=[lamr])
    k.op("dve", lambda e: e.reduce_sum(lams[:, 0:1], lamr[:, 0:64], axis=AX.X), reads=[lamr, lams], writes=[lams])
    k.op("dve", lambda e: e.reduce_sum(lams[:, 1:2], lamr[:, 128:192], axis=AX.X), reads=[lamr, lams], writes=[lams])
    k.op("act", lambda e: e.activation(out=lams[:, 2:4], in_=lams[:, 0:2], func=AF.Exp), reads=[lams], writes=[lams])
    k.op("dve", lambda e: e.tensor_tensor(out=lams[:, 4:5], in0=lams[:, 3:4], in1=lams[:, 2:3], op=ALU.subtract), reads=[lams], writes=[lams])
    k.op("dve", lambda e: e.tensor_scalar(lams[:, 5:6], lams[:, 4:5], -lambda_init, None, op0=ALU.add), reads=[lams], writes=[lams])
    if "nlam" not in SKIP:
        k.op("pe", lambda e: e.matmul(pb[7][:, 0:1], ones1[:], lams[:, 5:6], start=True, stop=True), reads=[ones1, lams], writes=[pb[7]])
        k.op("dve", lambda e: e.tensor_copy(nlam[:], pb[7][:, 0:1]), reads=[pb[7]], writes=[nlam])

    NT1e = int(os.environ.get('NT1', NTT))

    def load_x(tt):
        xb = xt[tt % 3]
        k.dma("sp" if tt % 2 == 0 else "act", xb[:], x_d(tt)[0], reads=[x_d(tt)[1]], writes=[xb])

    def tileA(tt):
        j = tt % 2
        xb = xt[tt % 3]
        small = smalls[j]
        rtmp = rtmps[j]
        if tt == 0:
            load_x(0)
        if tt + 1 < NT1e:
            load_x(tt + 1)
        yield from norm_to_hT(k, xb[:], xb, small, hT[j][:], hT[j], xb[:], xb, pb[0], pb[1], id32, a1, sh1, [a1, modc], (hT[j][:], hT[j]))
        for blk in range(3):
            bank = pb[2 + blk]
            for c in range(8):
                k.op("pe", lambda e, c=c, blk=blk, bank=bank, j=j: e.matmul(bank[:], hT[j][:, c * 128:(c + 1) * 128], win[:, c, blk * 512:(blk + 1) * 512],
                                                                         start=(c == 0), stop=(c == 7)),
                     reads=[hT[j], win], writes=[bank], same_ok=True)
        yield
        for blk in range(2):
            k.op("act", lambda e, blk=blk, j=j: e.copy(qk16[j][:, blk * 512:(blk + 1) * 512], pb[2 + blk][:]), reads=[pb[2 + blk]], writes=[qk16[j]])
        k.op("act", lambda e, tt=tt: e.copy(Va[:, tt, :].rearrange("p (h d) -> p h d", h=4)[:, :, 0:64], pb[4][:, 0:256].rearrange("p (h d) -> p h d", h=4)),
             reads=[pb[4]], writes=[V_b[tt]])
        k.op("act", lambda e, tt=tt: e.copy(Vb[:, tt, :].rearrange("p (h d) -> p h d", h=2)[:, :, 0:128], pb[4][:, 256:512].rearrange("p (h d) -> p h d", h=2)),
             reads=[pb[4]], writes=[V_b[tt]])
        k.op("pool", lambda e, tt=tt: e.memset(Va[:, tt, :].rearrange("p (h d) -> p h d", h=4)[:, :, 64:65], 1.0), writes=[V_b[tt]])
        k.op("pool", lambda e, tt=tt: e.memset(Vb[:, tt, :].rearrange("p (h d) -> p h d", h=2)[:, :, 128:129], 1.0), writes=[V_b[tt]])
        yield
        cB = cosT[:, tt, :].unsqueeze(1).to_broadcast([128, 8, 8])
        sB = sinT[:, tt, :].unsqueeze(1).to_broadcast([128, 8, 8])
        for blk in range(2):
            pv = pb[2 + blk][:].rearrange("p (h d) -> p h d", h=8)
            t1 = pv[:, :, 0:8]; t2 = pv[:, :, 8:16]
            ov = qk16[j][:, blk * 512:(blk + 1) * 512].rearrange("p (h d) -> p h d", h=8)
            r = [rtmp[:, i, 0:64].rearrange("p (h d) -> p h d", h=8) for i in range(4)]
            RB = [pb[2 + blk], cosT, sinT, rtmp, qk16[j]]
            k.op("dve", lambda e, t1=t1, r=r: e.tensor_tensor(out=r[0], in0=t1, in1=cB, op=ALU.mult), reads=RB, writes=[rtmp])
            k.op("dve", lambda e, t2=t2, r=r: e.tensor_tensor(out=r[1], in0=t2, in1=sB, op=ALU.mult), reads=RB, writes=[rtmp])
            k.op("dve", lambda e, t2=t2, r=r: e.tensor_tensor(out=r[2], in0=t2, in1=cB, op=ALU.mult), reads=RB, writes=[rtmp])
            k.op("dve", lambda e, t1=t1, r=r: e.tensor_tensor(out=r[3], in0=t1, in1=sB, op=ALU.mult), reads=RB, writes=[rtmp])
            k.op("dve", lambda e, ov=ov, r=r: e.tensor_tensor(out=ov[:, :, 0:8], in0=r[0], in1=r[1], op=ALU.subtract), reads=[rtmp, qk16[j]], writes=[qk16[j]])
            k.op("dve", lambda e, ov=ov, r=r: e.tensor_tensor(out=ov[:, :, 8:16], in0=r[2], in1=r[3], op=ALU.add), reads=[rtmp, qk16[j]], writes=[qk16[j]])
        yield
        pT = pb[5 + j][:].bitcast(BF16)
        for blk in range(8):
            k.op("pe", lambda e, blk=blk, pT=pT, j=j: e.transpose(pT[:, blk * 128:(blk + 1) * 128], qk16[j][:, blk * 128:(blk + 1) * 128], id16[:]),
                 reads=[qk16[j], id16], writes=[pb[5 + j]], same_ok=True)
        yield
        k.op("dve", lambda e, pT=pT, tt=tt: e.tensor_copy(QKT[:, :, tt * 128:(tt + 1) * 128], pT.rearrange("p (b t) -> p b t", b=8)),
             reads=[pb[5 + j]], writes=[QKT_b[jb][tt // 4] for jb in range(8)])
        yield

    run_streams([tileA(tt) for tt in range(NT1e)], int(os.environ.get('LAGA', 4)))

    for jb in range(0 if 'red' in SKIP else 2):
        k.op("dve", lambda e, jb=jb: e.tensor_reduce(out=kmT32[:, jb, :], in_=QKT[:, 2 + jb, :].rearrange("p (n l) -> p n l", l=256), axis=AX.X, op=ALU.add),
             reads=QKT_b[2 + jb], writes=[kmT32])
    k.op("dve", lambda e: e.tensor_scalar(kmT[:], kmT32[:], 1.0 / 256, None, op0=ALU.mult), reads=[kmT32], writes=[kmT])

    def run_attention(steps, bg=None):
        n = len(steps)

        def emit_qk(i):
            st = steps[i]
            if "pre" in st:
                st["pre"]()
            bank = pb[i % 2]
            mm = st["qk"]
            for ii, (lh, rh, rd) in enumerate(mm):
                k.op("pe", lambda e, lh=lh, rh=rh, ii=ii, bank=bank: e.matmul(bank[:], lh, rh, start=(ii == 0), stop=(ii == len(mm) - 1)),
                     reads=rd, writes=[bank], same_ok=True)

        pending = [None]
        emit_qk(0)
        for i in range(n):
            st = steps[i]
            if i + 1 < n:
                emit_qk(i + 1)
            if bg is not None and i % 2 == 1:
                next(bg, None)
            bank = pb[i % 2]
            pt = PT[i % 2]
            k.op("act", lambda e, bank=bank, pt=pt: e.activation(out=pt[:], in_=bank[:], func=AF.Exp, scale=0.125), reads=[bank], writes=[pt])
            if "oT" in st:
                obT, obT_buf = st["oT"]
                k.op("pe", lambda e, obT=obT, pt=pt, st=st: e.matmul(obT, st["v"], pt[:], start=st["first"], stop=st["last"], skip_group_check=True),
                     reads=[pt] + st["vr"], writes=[obT_buf], same_ok=True)
            else:
                for sub in range(4):
                    oap, obuf = st["o"](sub)
                    k.op("pe", lambda e, sub=sub, oap=oap, pt=pt, st=st: e.matmul(oap, pt[:, sub * 128:(sub + 1) * 128], st["v"], start=(st["first"] and st["sf"](sub)), stop=st["last"], skip_group_check=True),
                         reads=[pt] + st["vr"], writes=[obuf], same_ok=True)
            if pending[0] is not None:
                pending[0]()
                pending[0] = None
            if st["last"]:
                if "fin_a" in st:
                    st["fin_a"]()
                    pending[0] = st["fin"]
                else:
                    st["fin"]()
        if pending[0] is not None:
            pending[0]()
            pending[0] = None

    gsts = [gst, k.sb([128, 64], F32, "gst1")]

    def gate_gen(h):
        jq = h // 2
        r0 = (h % 2) * 64
        mbt = MBTv[h % 2]; mbt_b = MBT_b[h % 2]
        for tt in range(NTT):
            own = tt // 2
            gs_ = gsts[tt % 2]
            k.op("pe", lambda e, tt=tt: e.matmul(pb[6][:, 0:16], QKT[r0:r0 + 64, jq, tt * 128:(tt + 1) * 128], kmT[r0:r0 + 64, jq, :], start=True, stop=True),
                 reads=[QKT_b[jq][tt // 4], kmT], writes=[pb[6]])
            G = [gs_, ptab]
            gsm = gs_[:, 0:16]; m8 = gs_[:, 16:24]; okf = gs_[:, 24:40]; mb = gs_[:, 40:56]
            k.op("dve", lambda e, own=own, gsm=gsm: e.tensor_tensor(out=gsm, in0=pb[6][:, 0:16], in1=ptab[:, 16 - own:32 - own], op=ALU.add), reads=[pb[6]] + G, writes=[gs_])
            k.op("dve", lambda e, gsm=gsm, m8=m8: e.max(out=m8, in_=gsm), reads=G, writes=[gs_])
            k.op("dve", lambda e, gsm=gsm, m8=m8, okf=okf: e.tensor_scalar(okf, gsm, m8[:, 2:3], None, op0=ALU.is_ge), reads=G, writes=[gs_])
            k.op("dve", lambda e, own=own, okf=okf: e.tensor_tensor(out=okf, in0=okf, in1=ptab[:, 32 + 16 - own:32 + 32 - own], op=ALU.mult), reads=G, writes=[gs_])
            k.op("dve", lambda e, own=own, okf=okf: e.tensor_tensor(out=okf, in0=okf, in1=ptab[:, 64 + 16 - own:64 + 32 - own], op=ALU.add), reads=G, writes=[gs_])
            k.op("dve", lambda e, okf=okf, mb=mb: e.tensor_scalar(mb, okf, -NEG, NEG, op0=ALU.mult, op1=ALU.add), reads=G, writes=[gs_])
            yield
            k.op("pe", lambda e, mb=mb: e.transpose(pb[7][0:16, 0:128], mb, id32[:]), reads=[gs_, id32], writes=[pb[7]])
            k.op("act", lambda e, tt=tt, mbt=mbt: e.copy(mbt[0:16, tt * 128:(tt + 1) * 128], pb[7][0:16, 0:128]), reads=[pb[7]], writes=[mbt_b, win])
            yield

    NH_A = 4 if PH >= 2 else 0
    bg0 = gate_gen(0) if NH_A else iter(())
    bgc = [0]
    for dh in range(2 if PH >= 3 else 0):
        for Q in range(8):
            nch = 4 * Q + 4
            steps = []
            for m in range(2):
                r0 = m * 64
                ob = (pb[2 + 2 * m], pb[3 + 2 * m])
                qz = QZ[qzc[0] % 2]; qzc[0] += 1

                def pre(qz=qz, r0=r0, dh=dh, Q=Q):
                    k.op("pool", lambda e: e.memset(qz[64 - r0:128 - r0, :], 0.0), writes=[qz])
                    k.op("pool", lambda e: e.tensor_copy(qz[r0:r0 + 64, :], QKT[r0:r0 + 64, 4 + dh, Q * 512:(Q + 1) * 512]), reads=[QKT_b[4 + dh][Q]], writes=[qz])
                for c in range(nch):
                    mm = [(QKT[:, 6 + dh, c * 128:(c + 1) * 128], qz[:, :], [QKT_b[6 + dh][c // 4], qz])]
                    if c >= 4 * Q:
                        d = c - 4 * Q
                        mm.append((id16[:], cb[:, d * 512:(d + 1) * 512], [id16, cb]))

                    def fin(Q=Q, dh=dh, m=m):
                        if m == 0:
                            return
                        for sub in range(4):
                            o0 = pb[2 + sub // 2][:, (sub % 2) * 129:(sub % 2) * 129 + 129]
                            o1 = pb[4 + sub // 2][:, (sub % 2) * 129:(sub % 2) * 129 + 129]
                            B0 = pb[2 + sub // 2]; B1 = pb[4 + sub // 2]
                            f = fss[sub]
                            F = [f]
                            k.op("dve", lambda e, f=f, o0=o0: e.tensor_scalar(f[:, 0:1], o0[:, 128:129], 1e-30, None, op0=ALU.add), reads=[B0] + F, writes=F)
                            k.op("dve", lambda e, f=f, o1=o1: e.tensor_scalar(f[:, 1:2], o1[:, 128:129], 1e-30, None, op0=ALU.add), reads=[B1] + F, writes=F)
                            k.op("dve", lambda e, f=f: e.reciprocal(f[:, 0:2], f[:, 0:2]), reads=F, writes=F)
                            k.op("dve", lambda e, f=f: e.tensor_tensor(out=f[:, 1:2], in0=f[:, 1:2], in1=nlam[:], op=ALU.mult), reads=F + [nlam], writes=F)
                            k.op("act", lambda e, sub=sub, f=f, o0=o0: e.activation(out=ob32[:, sub, :], in_=o0[:, 0:128], func=AF.Copy, scale=f[:, 0:1]), reads=[B0] + F, writes=[ob32_b[sub]])
                            k.op("dve", lambda e, sub=sub, f=f, o1=o1: e.scalar_tensor_tensor(out=ob32[:, sub, :], in0=o1[:, 0:128], scalar=f[:, 1:2], in1=ob32[:, sub, :], op0=ALU.mult, op1=ALU.add),
                                 reads=[B1, ob32_b[sub]] + F, writes=[ob32_b[sub]])
                        for sub in range(4):
                            tt = Q * 4 + sub
                            f = fss[sub]
                            F = [f]
                            k.op("dve", lambda e, f=f: e.memset(f[:, 2:4], 0.0), reads=F, writes=F)
                            k.op("act", lambda e, sub=sub, f=f: e.activation(out=sq[:, 0:128], in_=ob32[:, sub, :], func=AF.Square, accum_out=f[:, 2:3]), reads=[ob32_b[sub]] + F, writes=[sq] + F)
                            k.op("act", lambda e, f=f: e.activation(out=f[:, 3:4], in_=f[:, 2:3], func=AF.Sqrt, scale=1.0 / 128, bias=EPS), reads=F, writes=F)
                            k.op("dve", lambda e, f=f: e.reciprocal(f[:, 3:4], f[:, 3:4]), reads=F, writes=F)
                            k.op("dve", lambda e, sub=sub, tt=tt, f=f: e.scalar_tensor_tensor(out=oo[:, tt, 256 + dh * 128:256 + (dh + 1) * 128], in0=ob32[:, sub, :], scalar=f[:, 3:4], in1=gsl[:],
                                                                                  op0=ALU.mult, op1=ALU.mult), reads=[ob32_b[sub], gsl] + F, writes=[oo_b[tt]])
                    steps.append(dict(qk=mm, v=Vb[:, c, dh * 129:(dh + 1) * 129], vr=[V_b[c]],
                                      o=(lambda sub, ob=ob: (ob[sub // 2][:, (sub % 2) * 129:(sub % 2) * 129 + 129], ob[sub // 2])),
                                      first=(c == 0), last=(c == nch - 1), fin=fin, sf=(lambda sub: sub % 2 == 0)))
                    if c == 0:
                        steps[-1]["pre"] = pre
            run_attention_diff(k, steps, pb, PT, bg0, bgc)

    if NH_A:
        for _ in bg0:
            pass
    for h in range(NH_A):
        jq = h // 2
        r0 = (h % 2) * 64
        mbt = MBTv[h % 2]; mbt_b = MBT_b[h % 2]
        bg = gate_gen(h + 1) if h + 1 < NH_A else None
        steps = []
        for Q in range(8):
            nch = 4 * Q + 4
            qz = QZ[qzc[0] % 2]; qzc[0] += 1

            def pre(qz=qz, r0=r0, jq=jq, Q=Q):
                k.op("pool", lambda e: e.memset(qz[64 - r0:128 - r0, :], 0.0), writes=[qz])
                k.op("pool", lambda e: e.tensor_copy(qz[r0:r0 + 64, :], QKT[r0:r0 + 64, jq, Q * 512:(Q + 1) * 512]), reads=[QKT_b[jq][Q]], writes=[qz])
            for c in range(nch):
                mm = [(QKT[:, 2 + jq, c * 128:(c + 1) * 128], qz[:, :], [QKT_b[2 + jq][c // 4], qz]),
                      (eall[:, c * 128:(c + 1) * 128], mbt[:, Q * 512:(Q + 1) * 512], [eall, mbt_b])]
                if c >= 4 * Q:
                    d = c - 4 * Q
                    mm.append((id16[:], cb[:, d * 512:(d + 1) * 512], [id16, cb]))
                obank = pb[2 + Q % 2]

                def fin(Q=Q, h=h, obank=obank):
                    ov = obank[:, 0:260].rearrange("p (s d) -> p s d", s=4)
                    k.op("dve", lambda e: e.tensor_scalar(fsc[:, 0:4], ov[:, :, 64], 1e-30, None, op0=ALU.add), reads=[obank, fsc], writes=[fsc])
                    k.op("dve", lambda e: e.reciprocal(fsc[:, 0:4], fsc[:, 0:4]), reads=[fsc], writes=[fsc])
                    for sub in range(4):
                        tt = Q * 4 + sub
                        k.op("act", lambda e, sub=sub, tt=tt: e.activation(out=oo[:, tt, h * 64:(h + 1) * 64], in_=ov[:, sub, 0:64], func=AF.Copy, scale=fsc[:, sub:sub + 1]),
                             reads=[obank, fsc], writes=[oo_b[tt]])
                steps.append(dict(qk=mm, v=Va[:, c, h * 65:(h + 1) * 65], vr=[V_b[c]],
                                  o=(lambda sub, obank=obank: (obank[:, sub * 65:(sub + 1) * 65], obank)),
                                  first=(c == 0), last=(c == nch - 1), fin=fin, sf=(lambda sub: sub == 0)))
                if c == 0:
                    steps[-1]["pre"] = pre
        run_attention(steps, bg)
        if bg is not None:
            for _ in bg:
                pass

    for tt in range(NTT):
        k.dma("sp" if tt % 2 == 0 else "act", o_d(tt)[0], oo[:, tt, :], reads=[oo_b[tt]], writes=[o_d(tt)[1]])
    k.pop_scope()


def run_attention_diff(k, steps, pb, PT, bg=None, bgc=None):
    n = len(steps)

    def emit_qk(i):
        st = steps[i]
        if "pre" in st:
            st["pre"]()
        bank = pb[i % 2]
        mm = st["qk"]
        for ii, q in enumerate(mm):
            lh, rh, rd = q[0], q[1], q[2]
            outv = q[3](bank) if len(q) > 3 else bank[:]
            k.op("pe", lambda e, lh=lh, rh=rh, ii=ii, outv=outv: e.matmul(outv, lh, rh, start=(ii == 0), stop=(ii == len(mm) - 1), skip_group_check=True),
                 reads=rd, writes=[bank], same_ok=True)

    emit_qk(0)
    for i in range(n):
        st = steps[i]
        if i + 1 < n:
            emit_qk(i + 1)
        if bg is not None:
            bgc[0] += 1
            if bgc[0] % 4 == 0:
                next(bg, None)
        bank = pb[i % 2]
        pt = PT[i % 2]
        SM = os.environ.get('STEPMODE', 'all')
        if SM == 'qk': continue
        k.op("act", lambda e, bank=bank, pt=pt: e.activation(out=pt[:], in_=bank[:], func=AF.Exp, scale=0.125), reads=[bank], writes=[pt])
        for sub in range(0 if os.environ.get('STEPMODE', 'all') == 'qkexp' else 4):
            oap, obuf = st["o"](sub)
            k.op("pe", lambda e, sub=sub, oap=oap, pt=pt, st=st: e.matmul(oap, pt[:, sub * 128:(sub + 1) * 128], st["v"], start=(st["first"] and st["sf"](sub)), stop=st["last"], skip_group_check=True),
                 reads=[pt] + st["vr"], writes=[obuf], same_ok=True)
        if st["last"]:
            st["fin"]()


D = 1024
S = 4096
NTT = 32
EPS = 1e-6
NEG = -30000.0
NCOL = 1304
GC = 2.0 * math.sqrt(2.0 / math.pi)


def run_steps2(k, steps, pb, PT):
    n = len(steps)
    first_sw = {st["mst_job"]: idx for idx, st in enumerate(steps) if "mst_job" in st}
    deferred = []

    def emit_qk(i):
        st = steps[i]
        if "pre" in st:
            st["pre"]()
        bank = pb[i % 2]
        first = True
        for (lh, rh, rd, ofn) in st["qk"]:
            k.op("pe", lambda e, lh=lh, rh=rh, bank=bank, ofn=ofn, s0=first: e.matmul(ofn(bank), lh, rh, start=s0, stop=False, skip_group_check=True),
                 reads=rd, writes=[bank], same_ok=True)
            first = False

    emit_qk(0)
    for i in range(n):
        st = steps[i]
        if i + 1 < n:
            emit_qk(i + 1)
        bank = pb[i % 2]
        pt = PT[i % 2]
        k.op("act", lambda e, bank=bank, pt=pt: e.activation(out=pt[:], in_=bank[:], func=AF.Exp, scale=0.125), reads=[bank], writes=[pt])
        for sub in range(4):
            oap, obuf = st["o"](sub)
            k.op("pe", lambda e, sub=sub, oap=oap, pt=pt, st=st: e.matmul(oap, pt[:, sub * 128:(sub + 1) * 128], st["v"], start=(st["first"] and st["sf"](sub)), stop=st["last"], skip_group_check=True),
                 reads=[pt] + st["vr"], writes=[obuf], same_ok=True)
        if st["last"]:
            r = st["fin"]()
            if r is not None:
                job, tail = r
                due = min(i + 5, first_sw.get(job, i) - 2)
                if due <= i:
                    tail()
                else:
                    deferred.append((due, tail))
        while deferred and deferred[0][0] <= i:
            deferred.pop(0)[1]()
    for _, tail in deferred:
        tail()


def emit_A_odd(k, pb, l, x_d, o_d, modc, NT1=NTT, PH=3):
    k.push_scope()
    n1c_d = k.dram_once("n1c_%d" % l, [128, 8], F32, "ExternalInput")
    win_d = k.dram_once("winC_%d" % l, [D, NCOL], F32, "ExternalInput")
    cos_d = k.dram_once("cosT", [128, NTT * 8], F32)
    sin_d = k.dram_once("sinT", [128, NTT * 8], F32)
    id32_d = k.dram_once("id32", [128, 128], F32)
    id16_d = k.dram_once("id16", [128, 128], BF16)
    w1_d = k.dram_once("w1_%d" % l, [2, 2048, 256], F32, "ExternalInput")
    posT_d = k.dram_once("posT_%d" % l, [64, 2 * 32], F32, "ExternalInput")
    b1c_d = k.dram_once("b1c_%d" % l, [128, 4], F32, "ExternalInput")
    w2d_d = k.dram_once("w2d_%d" % l, [2, 256, 128], F32, "ExternalInput")
    b2c_d = k.dram_once("b2c_%d" % l, [128, 1], F32, "ExternalInput")
    b2r_d = k.dram_once("b2r_%d" % l, [1, 64], F32, "ExternalInput")
    ovl_d = k.dram_once("ovl", [128, 2 * 64], BF16)
    bc_d = k.dram_once("bc", [128, S], BF16)
    oh_d = k.dram_once("oh", [128, S], BF16)
    cbt_d = k.dram_once("cbt", [128, 256], BF16)
    tft_d = k.dram_once("tft", [128, 256], F32)

    QT = k.sb([128, 4, S], BF16, "QT")
    QT_b = [Buf(f"QT{t}") for t in range(NTT)]
    KT = k.sb([128, 4, S], BF16, "KT")
    KT_b = [Buf(f"KT{t}") for t in range(NTT)]
    R1 = k.sb([128, 4, S], BF16, "R1")
    R1_b = Buf("R1")
    oo = R1[:, :, :].rearrange("p a b -> p (a b)").rearrange("p (t c) -> p t c", c=512)
    oo_b = [Buf(f"oo{t}") for t in range(NTT)]
    Vs = k.sb([128, NTT, 2 * 65], BF16, "Vs")
    Vw = k.sb([128, NTT, 2 * 65], BF16, "Vw")
    V_b = [Buf(f"V{t}") for t in range(NTT)]
    gates = k.sb([128, NTT, 24], F32, "gates")
    R2 = k.sb([128, 8 * NCOL], BF16, "R2")
    R2_b = Buf("R2")
    win = R2[:, :].rearrange("p (c n) -> p c n", c=8)
    w1s = R2[0:64, 0:32 * 256].rearrange("p (l f) -> p l f", l=32)
    oh = k.sb([128, S], BF16, "oh")
    bc = k.sb([128, S], BF16, "bc")
    cbt = k.sb([128, 256], BF16, "cbt")
    tft = k.sb([128, 256], F32, "tft")
    id32 = k.sb([128, 128], F32, "id32")
    id16 = k.sb([128, 128], BF16, "id16")
    n1c = k.sb([128, 8], F32, "n1c")
    a1 = k.sb([128, 8], F32, "a1")
    cosT = k.sb([128, NTT, 8], F32, "cosT")
    sinT = k.sb([128, NTT, 8], F32, "sinT")
    ones1 = k.sb([1, 128], F32, "ones1")
    smalls = [k.sb([128, 16], F32, f"small{i}") for i in range(2)]
    xt = [k.sb([128, D], F32, f"xt{i}") for i in range(3)]
    hT = [k.sb([128, 8 * 128], BF16, f"hT{i}") for i in range(2)]
    qk32s = [k.sb([128, 896], F32, f"qk32_{i}") for i in range(2)]
    qk16 = [k.sb([128, 1408], BF16, f"qk16_{i}") for i in range(2)]
    rtmps = [k.sb([128, 4, 128], F32, f"rtmp{i}") for i in range(2)]
    PT = [k.sb([128, 512], BF16, f"PT{i}") for i in range(2)]
    posT = k.sb([64, 64], F32, "posT")
    posT16 = k.sb([64, 64], BF16, "posT16")
    b1c = k.sb([128, 4], F32, "b1c")
    w2d = k.sb([128, 2, 2, 128], BF16, "w2d")
    b2c = k.sb([128, 1], F32, "b2c")
    b2r = k.sb([1, 64], F32, "b2r")
    b2B = k.sb([128, 64], F32, "b2B")
    cb1 = k.sb([128, 2], F32, "cb1")
    gel = [k.sb([128, 256], F32, f"gel{i}") for i in range(3)]
    hidT = k.sb([128, 2, 256], BF16, "hidT")
    KCMP = k.sb([128, 2, 256], BF16, "KCMP")
    VCX = k.sb([128, 2, 2, 129], BF16, "VCX")
    imp = k.sb([128, 64], F32, "imp")
    selw = k.sb([128, 160], F32, "selw")
    MST = [k.sb([128, 128], BF16, f"MST{i}") for i in range(2)]
    oacc = [k.sb([128, 4, 64], F32, f"oacc{i}") for i in range(2)]
    fsc = k.sb([128, 32], F32, "fsc")

    for (t, d) in [(oh, oh_d), (bc, bc_d), (cbt, cbt_d), (tft, tft_d), (id32, id32_d), (id16, id16_d), (n1c, n1c_d),
                   (posT, posT_d), (b1c, b1c_d), (b2c, b2c_d), (b2r, b2r_d)]:
        k.dma("sp", t[:], d.t.ap(), writes=[t])
    k.dma("sp", b2B[:], b2r_d.t.ap()[0:1, :].to_broadcast([128, 64]), writes=[b2B])
    k.dma("sp", cosT[:], cos_d.t.ap().rearrange("p (t f) -> p t f", f=8), writes=[cosT])
    k.dma("sp", sinT[:], sin_d.t.ap().rearrange("p (t f) -> p t f", f=8), writes=[sinT])
    for c in range(8):
        k.dma("pool", win[:, c, :], win_d.t.ap()[c * 128:(c + 1) * 128, :], writes=[R2_b])
    for kind in range(2):
        k.dma("pool", w2d[:, kind, :, :], w2d_d.t.ap()[kind].rearrange("(c p) e -> p c e", p=128), writes=[w2d])
    for c in range(2):
        for g in range(2):
            k.dma("sp", VCX[:, c, g, 64:128], ovl_d.t.ap()[:, c * 64:(c + 1) * 64], writes=[VCX])
    k.op("pool", lambda e: e.memset(VCX[:, :, :, 128:129], 1.0), writes=[VCX])
    k.op("pool", lambda e: e.memset(KCMP[:], 0.0), writes=[KCMP])
    for m_ in MST:
        k.op("pool", lambda e, m_=m_: e.memset(m_[:], 0.0), writes=[m_])
    k.op("pool", lambda e: e.memset(hidT[:], 0.0), writes=[hidT])
    k.op("dve", lambda e: e.memset(ones1[:], 1.0), writes=[ones1])
    k.op("dve", lambda e: e.tensor_copy(posT16[:], posT[:]), reads=[posT], writes=[posT16])
    k.op("dve", lambda e: e.scalar_tensor_tensor(out=a1[:], in0=modc[:, 8:16], scalar=1.0, in1=n1c[:], op0=ALU.add, op1=ALU.mult),
         reads=[modc, n1c], writes=[a1])
    sh1 = modc[:, 0:8]

    def load_x(tt):
        xb = xt[tt % 3]
        k.dma("sp" if tt % 2 == 0 else "act", xb[:], x_d(tt)[0], reads=[x_d(tt)[1]], writes=[xb])

    def tileC(tt):
        j = tt % 2
        xb = xt[tt % 3]
        small = smalls[j]
        rtmp = rtmps[j]
        qk32 = qk32s[j]
        if tt == 0:
            load_x(0)
        if tt + 1 < NT1:
            load_x(tt + 1)
        yield from norm_to_hT(k, xb[:], xb, small, hT[j][:], hT[j], xb[:], xb, pb[0], pb[1], id32, a1, sh1, [a1, modc], (hT[j][:], hT[j]))
        widths = [512, 512, NCOL - 1024]
        for blk in range(3):
            bank = pb[2 + blk]
            wd = widths[blk]
            for c in range(8):
                k.op("pe", lambda e, c=c, blk=blk, bank=bank, j=j, wd=wd: e.matmul(bank[:, 0:wd], hT[j][:, c * 128:(c + 1) * 128], win[:, c, blk * 512:blk * 512 + wd],
                                                                                start=(c == 0), stop=(c == 7)),
                     reads=[hT[j], R2_b], writes=[bank], same_ok=True)
        yield
        k.op("act", lambda e: e.copy(qk32[:, 0:512], pb[2][:]), reads=[pb[2]], writes=[qk32])
        k.op("act", lambda e: e.copy(qk32[:, 512:896], pb[3][:, 0:384]), reads=[pb[3]], writes=[qk32])
        k.op("act", lambda e, j=j: e.copy(qk16[j][:, 1280:1408], pb[3][:, 384:512]), reads=[pb[3]], writes=[qk16[j]])
        k.op("act", lambda e, tt=tt: e.copy(Vs[:, tt, :].rearrange("p (g d) -> p g d", g=2)[:, :, 0:64], pb[4][:, 0:128].rearrange("p (g d) -> p g d", g=2)),
             reads=[pb[4]], writes=[V_b[tt]])
        k.op("act", lambda e, tt=tt: e.copy(Vw[:, tt, :].rearrange("p (g d) -> p g d", g=2)[:, :, 0:64], pb[4][:, 128:256].rearrange("p (g d) -> p g d", g=2)),
             reads=[pb[4]], writes=[V_b[tt]])
        k.op("act", lambda e, tt=tt: e.copy(gates[:, tt, :], pb[4][:, 256:280]), reads=[pb[4]], writes=[gates])
        k.op("pool", lambda e, tt=tt: e.memset(Vs[:, tt, :].rearrange("p (g d) -> p g d", g=2)[:, :, 64:65], 1.0), writes=[V_b[tt]])
        k.op("pool", lambda e, tt=tt: e.memset(Vw[:, tt, :].rearrange("p (g d) -> p g d", g=2)[:, :, 64:65], 1.0), writes=[V_b[tt]])
        yield
        cB = cosT[:, tt, :].unsqueeze(1).to_broadcast([128, 14, 8])
        sB = sinT[:, tt, :].unsqueeze(1).to_broadcast([128, 14, 8])
        qv = qk32[:].rearrange("p (h d) -> p h d", h=14)
        t1 = qv[:, :, 0:8]; t2 = qv[:, :, 8:16]
        r = [rtmp[:, i, 0:112].rearrange("p (h d) -> p h d", h=14) for i in range(4)]
        RB = [qk32, cosT, sinT, rtmp]
        k.op("dve", lambda e: e.tensor_tensor(out=r[0], in0=t1, in1=cB, op=ALU.mult), reads=RB, writes=[rtmp])
        k.op("dve", lambda e: e.tensor_tensor(out=r[1], in0=t2, in1=sB, op=ALU.mult), reads=RB, writes=[rtmp])
        k.op("dve", lambda e: e.tensor_tensor(out=r[2], in0=t2, in1=cB, op=ALU.mult), reads=RB, writes=[rtmp])
        k.op("dve", lambda e: e.tensor_tensor(out=r[3], in0=t1, in1=sB, op=ALU.mult), reads=RB, writes=[rtmp])
        k.op("dve", lambda e: e.tensor_tensor(out=t1, in0=r[0], in1=r[1], op=ALU.subtract), reads=[rtmp, qk32], writes=[qk32])
        k.op("dve", lambda e: e.tensor_tensor(out=t2, in0=r[2], in1=r[3], op=ALU.add), reads=[rtmp, qk32], writes=[qk32])
        yield
        k.op("pool", lambda e, j=j: e.tensor_copy(qk16[j][:, 0:512], qk32[:, 0:512]), reads=[qk32], writes=[qk16[j]])
        kdst = qk16[j][:, 512:1280].rearrange("p (k two d) -> p k two d", k=6, two=2)
        ksrc = qk32[:, 512:896].rearrange("p (k d) -> p k d", k=6)
        k.op("pool", lambda e, kdst=kdst, ksrc=ksrc, j=j: e.tensor_copy(kdst[:, :, 0, :], ksrc), reads=[qk32], writes=[qk16[j]])
        k.op("dve", lambda e, kdst=kdst, ksrc=ksrc, j=j: e.tensor_copy(kdst[:, :, 1, :], ksrc), reads=[qk32], writes=[qk16[j]])
        yield
        pA = pb[5][:].bitcast(BF16); pB = pb[6][:].bitcast(BF16)
        for blk in range(8):
            k.op("pe", lambda e, blk=blk, j=j: e.transpose(pA[:, blk * 128:(blk + 1) * 128], qk16[j][:, blk * 128:(blk + 1) * 128], id16[:]),
                 reads=[qk16[j], id16], writes=[pb[5]], same_ok=True)
        for blk in range(2):
            k.op("pe", lambda e, blk=blk, j=j: e.transpose(pB[:, blk * 128:(blk + 1) * 128], qk16[j][:, 1024 + blk * 128:1024 + (blk + 1) * 128], id16[:]),
                 reads=[qk16[j], id16], writes=[pb[6]], same_ok=True)
        for g in range(2):
            k.op("pe", lambda e, g=g, j=j: e.transpose(pB[0:64, 256 + g * 128:256 + (g + 1) * 128], qk16[j][:, 1280 + g * 64:1280 + (g + 1) * 64], id16[:]),
                 reads=[qk16[j], id16], writes=[pb[6]], same_ok=True)
        yield
        tsl = slice(tt * 128, (tt + 1) * 128)
        k.op("dve", lambda e, tsl=tsl: e.tensor_copy(QT[:, :, tsl], pA[:, 0:512].rearrange("p (b t) -> p b t", b=4)), reads=[pb[5]], writes=[QT_b[tt]])
        k.op("dve", lambda e, tsl=tsl: e.tensor_copy(R1[0:64, 0:2, tsl], pA[0:64, 512:768].rearrange("p (b t) -> p b t", b=2)), reads=[pb[5]], writes=[R1_b])
        k.op("dve", lambda e, tsl=tsl: e.tensor_copy(KT[:, 0:2, tsl], pA[:, 768:1024].rearrange("p (b t) -> p b t", b=2)), reads=[pb[5]], writes=[KT_b[tt]])
        k.op("act", lambda e, tsl=tsl: e.copy(KT[:, 2:4, tsl], pB[:, 0:256].rearrange("p (b t) -> p b t", b=2)), reads=[pb[6]], writes=[KT_b[tt]])
        k.op("act", lambda e, tsl=tsl: e.copy(R1[0:64, 2:4, tsl], pB[0:64, 256:512].rearrange("p (b t) -> p b t", b=2)), reads=[pb[6]], writes=[R1_b])
        yield

    run_streams([tileC(tt) for tt in range(NT1)], int(os.environ.get('LAGC', 4)))
    k.op("act", lambda e: e.activation(out=gates[:, :, :], in_=gates[:, :, :], func=AF.Sigmoid), reads=[gates], writes=[gates])

    if PH < 2:
        k.dma("sp", o_d.t.ap()[0:128, :], QT[:, 0, 0:512], reads=QT_b, writes=[o_d])
        k.dma("sp", o_d.t.ap()[128:256, :], KT[:, 0, 0:512], reads=KT_b, writes=[o_d])
        k.dma("sp", o_d.t.ap()[256:384, :], KT[:, 2, 0:512], reads=KT_b, writes=[o_d])
        k.dma("sp", o_d.t.ap()[384:448, :], R1[0:64, 0, 0:512], reads=[R1_b], writes=[o_d])
        k.dma("sp", o_d.t.ap()[448:512, :], R1[0:64, 2, 0:512], reads=[R1_b], writes=[o_d])
        k.finish([o_d])
        return k

    for kind in range(2):
        k.dma("pool", w1s, w1_d.t.ap()[kind].rearrange("(l d) f -> d l f", d=64), reads=[], writes=[R2_b])
        for g in range(2):
            XT = R1[0:64, kind * 2 + g, :]
            for fc in range(2):
                bank = pb[fc]
                for l in range(32):
                    k.op("pe", lambda e, l=l, fc=fc, bank=bank, XT=XT: e.matmul(bank[:, 0:255], w1s[:, l, fc * 128:(fc + 1) * 128], XT[:, l:l + 16 * 254 + 1:16],
                                                                            start=(l == 0), stop=False, skip_group_check=True),
                         reads=[R2_b, R1_b], writes=[bank], same_ok=True)
                for l in range(32):
                    k.op("pe", lambda e, l=l, fc=fc, bank=bank: e.matmul(bank[:, 255:256], w1s[:, l, fc * 128:(fc + 1) * 128], posT16[:, kind * 32 + l:kind * 32 + l + 1],
                                                                     start=False, stop=(l == 31), skip_group_check=True),
                         reads=[R2_b, posT16], writes=[bank], same_ok=True)
                u, t2_, t3_ = gel[0], gel[1], gel[2]
                k.op("dve", lambda e, fc=fc, bank=bank: e.tensor_tensor(out=cb1[:, fc:fc + 1], in0=bank[:, 255:256], in1=b1c[:, kind * 2 + fc:kind * 2 + fc + 1], op=ALU.add),
                     reads=[bank, b1c], writes=[cb1])
                k.op("act", lambda e, fc=fc, bank=bank: e.activation(out=u[:, 0:255], in_=bank[:, 0:255], func=AF.Identity, bias=cb1[:, fc:fc + 1], scale=1.0),
                     reads=[bank, cb1], writes=[u])
                k.op("dve", lambda e: e.tensor_tensor(out=t2_[:, 0:255], in0=u[:, 0:255], in1=u[:, 0:255], op=ALU.mult), reads=[u], writes=[t2_])
                k.op("dve", lambda e: e.tensor_scalar(t2_[:, 0:255], t2_[:, 0:255], 0.044715, 1.0, op0=ALU.mult, op1=ALU.add), reads=[t2_], writes=[t2_])
                k.op("dve", lambda e: e.tensor_tensor(out=t2_[:, 0:255], in0=t2_[:, 0:255], in1=u[:, 0:255], op=ALU.mult), reads=[t2_, u], writes=[t2_])
                k.op("act", lambda e: e.activation(out=t3_[:, 0:255], in_=t2_[:, 0:255], func=AF.Sigmoid, scale=GC), reads=[t2_], writes=[t3_])
                k.op("dve", lambda e, fc=fc: e.tensor_tensor(out=hidT[:, fc, 0:255], in0=t3_[:, 0:255], in1=u[:, 0:255], op=ALU.mult), reads=[t3_, u], writes=[hidT])
            if kind == 0:
                for fc in range(2):
                    k.op("pe", lambda e, fc=fc: e.matmul(pb[2][:, 0:255], w2d[:, 0, fc, :], hidT[:, fc, 0:255], start=(fc == 0), stop=(fc == 1)),
                         reads=[w2d, hidT], writes=[pb[2]], same_ok=True)
                k.op("act", lambda e, g=g: e.activation(out=KCMP[:, g, 0:255], in_=pb[2][:, 0:255], func=AF.Identity, bias=b2c[:, 0:1], scale=1.0),
                     reads=[pb[2], b2c], writes=[KCMP])
            else:
                for c in range(2):
                    nn = 128
                    for fc in range(2):
                        k.op("pe", lambda e, fc=fc, c=c, nn=nn: e.matmul(pb[3][0:nn, c * 64:(c + 1) * 64], hidT[:, fc, c * 128:c * 128 + nn], w2d[:, 1, fc, 0:64],
                                                                        start=(fc == 0 and c == 0), stop=(fc == 1), skip_group_check=True),
                             reads=[w2d, hidT], writes=[pb[3]], same_ok=True)
                k.op("pool", lambda e, g=g: e.memset(VCX[:, 1, g, 0:64], 0.0), writes=[VCX])
                k.op("dve", lambda e, g=g: e.tensor_tensor(out=VCX[:, 0, g, 0:64], in0=pb[3][:, 0:64], in1=b2B[:], op=ALU.add), reads=[pb[3], b2B], writes=[VCX])
                k.op("dve", lambda e, g=g: e.tensor_tensor(out=VCX[0:127, 1, g, 0:64], in0=pb[3][0:127, 64:128], in1=b2B[0:127, :], op=ALU.add), reads=[pb[3], b2B], writes=[VCX])

    if PH < 3:
        k.dma("sp", o_d.t.ap()[0:128, 0:256], KCMP[:, 0, :], reads=[KCMP], writes=[o_d])
        k.dma("sp", o_d.t.ap()[128:256, 0:256], KCMP[:, 1, :], reads=[KCMP], writes=[o_d])
        k.dma("sp", o_d.t.ap()[256:384, 0:258], VCX[:, 0, :, :].rearrange("p b c -> p (b c)"), reads=[VCX], writes=[o_d])
        k.dma("sp", o_d.t.ap()[384:512, 0:258], VCX[:, 1, :, :].rearrange("p b c -> p (b c)"), reads=[VCX], writes=[o_d])
        k.finish([o_d])
        return k

    QZn = [k.sb([128, 512], BF16, f"QZn{i_}") for i_ in range(2)]
    for qz_ in QZn:
        k.op("pool", lambda e, qz_=qz_: e.memset(qz_[:], 0.0), writes=[qz_])

    def qz_pre(g, i, n):
        qz_ = QZn[n % 2]

        def pre():
            for hp in range(2):
                rows = slice(hp * 64, (hp + 1) * 64)
                dst = qz_[rows, :].rearrange("p (b h q) -> p b h q", b=2, h=2)[:, :, hp, :]
                k.op("pool", lambda e, dst=dst, rows=rows: e.tensor_copy(dst, QT[rows, 2 * g:2 * g + 2, i * 128:(i + 1) * 128]), reads=[QT_b[i]], writes=[qz_])
        return pre

    def qk_mms(ktype, g, c, i):
        n = g * NTT + i
        qz_ = QZn[n % 2]
        if ktype == "c":
            lh = KCMP[:, g, c * 128:(c + 1) * 128]; rd = [KCMP, qz_]
        else:
            kb = (0 if ktype == "s" else 2) + g
            lh = KT[:, kb, c * 128:(c + 1) * 128]; rd = [KT_b[c], qz_]
        return [(lh, qz_[:, :], rd, (lambda bank: bank[:, :]))]

    def full(bank):
        return bank[:, :].rearrange("p (r q) -> p r q", r=4)

    def bcast(ap2d, parts):
        return ap2d.unsqueeze(1).to_broadcast([parts, 4, 128])

    def addbias(mm, lh, rh2d, parts, rd):
        mm.append((lh, bcast(rh2d, parts), rd, full))

    steps = []

    def cmp_steps(g, i):
        n = g * NTT + i
        nch = 2 if i >= 16 else 1
        out = []
        for c in range(nch):
            mm = qk_mms("c", g, c, i)
            off = 128 * i - 2048 * c
            addbias(mm, id16[:], bc[:, off:off + 128], 128, [id16, bc])

            def fin(g=g, i=i, n=n):
                A, B = pb[2], pb[3]
                cnt = [0]
                lim = int(os.environ.get('FINOPS', 1000))
                def kop(*a_, **kw_):
                    cnt[0] += 1
                    if cnt[0] <= lim: k.op(*a_, **kw_)
                oc = [A[:, 0:129], A[:, 129:258], B[:, 0:129], B[:, 129:258]]
                bk = [A, A, B, B]
                F = [fsc]
                for r_ in range(4):
                    kop("dve", lambda e, r_=r_: e.tensor_scalar(fsc[:, r_:r_ + 1], oc[r_][:, 128:129], 1e-30, None, op0=ALU.add), reads=[bk[r_]] + F, writes=F)
                kop("dve", lambda e: e.reciprocal(fsc[:, 0:4], fsc[:, 0:4]), reads=F, writes=F)
                kop("dve", lambda e: e.tensor_scalar(imp[:], oc[0][:, 64:128], fsc[:, 0:1], None, op0=ALU.mult), reads=[A] + F, writes=[imp])
                for r_ in range(1, 4):
                    kop("dve", lambda e, r_=r_: e.scalar_tensor_tensor(out=imp[:], in0=oc[r_][:, 64:128], scalar=fsc[:, r_:r_ + 1], in1=imp[:], op0=ALU.mult, op1=ALU.add),
                         reads=[bk[r_], imp] + F, writes=[imp])
                gv = gates[:, i, g * 12:(g + 1) * 12].rearrange("p (r t) -> p r t", t=3)
                kop("dve", lambda e: e.tensor_tensor(out=fsc[:, 4:8], in0=fsc[:, 0:4], in1=gv[:, :, 0], op=ALU.mult), reads=F + [gates], writes=F)
                for r_ in range(4):
                    kop("act", lambda e, r_=r_: e.activation(out=oacc[n % 2][:, r_, :], in_=oc[r_][:, 0:64], func=AF.Copy, scale=fsc[:, 4 + r_:5 + r_]),
                         reads=[bk[r_]] + F, writes=[oacc[n % 2]])
                W = [selw, imp, tft]
                val = selw[:, 0:64]; m8a = selw[:, 64:72]; val2 = selw[:, 72:136]; m8b = selw[:, 136:144]
                kop("dve", lambda e: e.tensor_tensor(out=val, in0=imp[:], in1=tft[:, 64 - 2 * i:128 - 2 * i], op=ALU.max), reads=W, writes=[selw])
                kop("dve", lambda e: e.tensor_tensor(out=val, in0=val, in1=tft[:, 128 + 64 - 2 * i:128 + 128 - 2 * i], op=ALU.min), reads=W, writes=[selw])
                kop("dve", lambda e: e.memset(val[:, 0:1], 1e6), reads=W, writes=[selw])
                if 'NOSEL' in os.environ:
                    kop("dve", lambda e: e.memset(val2, 1.0), reads=W, writes=[selw])
                else:
                    kop("dve", lambda e: e.max(out=m8a, in_=val), reads=W, writes=[selw])
                    kop("dve", lambda e: e.match_replace(out=val2, in_to_replace=m8a, in_values=val, imm_value=-1e30), reads=W, writes=[selw])
                    kop("dve", lambda e: e.max(out=m8b, in_=val2), reads=W, writes=[selw])
                    kop("dve", lambda e: e.tensor_scalar(val2, val, m8b[:, 7:8], None, op0=ALU.is_ge), reads=W, writes=[selw])
                kop("dve", lambda e: e.tensor_scalar(val2, val2, -NEG, NEG, op0=ALU.mult, op1=ALU.add), reads=W, writes=[selw])
                def tail(n=n):
                    k.op("pe", lambda e: e.transpose(pb[3][0:64, 384:512], val2, id32[:]), reads=[selw, id32], writes=[pb[3]])
                    k.op("act", lambda e: e.copy(MST[n % 2][0:64, :], pb[3][0:64, 384:512]), reads=[pb[3]], writes=[MST[n % 2]])
                return (n, tail)

            ob = (pb[2], pb[3])
            out.append(dict(qk=mm, v=VCX[:, c, g, :], vr=[VCX], **({"pre": qz_pre(g, i, n)} if c == 0 else {}),
                            o=(lambda sub, ob=ob: (ob[sub // 2][:, (sub % 2) * 129:(sub % 2) * 129 + 129], ob[sub // 2])),
                            first=(c == 0), last=(c == nch - 1), fin=fin, sf=(lambda sub: sub % 2 == 0)))
        return out

    def sw_steps(g, i):
        n = g * NTT + i
        out = []
        for c in range(i + 1):
            mm = qk_mms("s", g, c, i)
            addbias(mm, oh[:, c * 128:(c + 1) * 128], MST[n % 2][:], 128, [oh, MST[n % 2]])
            if c == i:
                addbias(mm, id16[:], cbt[:, 0:128], 128, [id16, cbt])
            out.append(dict(qk=mm, v=Vs[:, c, g * 65:(g + 1) * 65], vr=[V_b[c]], **({"mst_job": n} if c == 0 else {}),
                            o=(lambda sub: (pb[4][:, sub * 65:(sub + 1) * 65], pb[4])),
                            first=(c == 0), last=(c == i), fin=(lambda: None), sf=(lambda sub: sub == 0)))
        c0 = max(0, i - 4)
        for c in range(c0, i + 1):
            mm = qk_mms("w", g, c, i)
            if c == i:
                addbias(mm, id16[:], cbt[:, 0:128], 128, [id16, cbt])
            if c == i - 4:
                addbias(mm, id16[:], cbt[:, 128:256], 128, [id16, cbt])

            def fin(g=g, i=i, n=n):
                F = [fsc]
                osv = pb[4][:, 0:260].rearrange("p (r d) -> p r d", r=4)
                owv = pb[5][:, 0:260].rearrange("p (r d) -> p r d", r=4)
                gv = gates[:, i, g * 12:(g + 1) * 12].rearrange("p (r t) -> p r t", t=3)
                k.op("dve", lambda e: e.tensor_scalar(fsc[:, 8:12], osv[:, :, 64], 1e-30, None, op0=ALU.add), reads=[pb[4]] + F, writes=F)
                k.op("dve", lambda e: e.tensor_scalar(fsc[:, 12:16], owv[:, :, 64], 1e-30, None, op0=ALU.add), reads=[pb[5]] + F, writes=F)
                k.op("dve", lambda e: e.reciprocal(fsc[:, 8:16], fsc[:, 8:16]), reads=F, writes=F)
                k.op("dve", lambda e: e.tensor_tensor(out=fsc[:, 8:12], in0=fsc[:, 8:12], in1=gv[:, :, 1], op=ALU.mult), reads=F + [gates], writes=F)
                k.op("dve", lambda e: e.tensor_tensor(out=fsc[:, 12:16], in0=fsc[:, 12:16], in1=gv[:, :, 2], op=ALU.mult), reads=F + [gates], writes=F)
                oa = oacc[n % 2]
                for r_ in range(4):
                    h = 4 * g + r_
                    k.op("dve", lambda e, r_=r_: e.scalar_tensor_tensor(out=oa[:, r_, :], in0=osv[:, r_, 0:64], scalar=fsc[:, 8 + r_:9 + r_], in1=oa[:, r_, :], op0=ALU.mult, op1=ALU.add),
                         reads=[pb[4], oa] + F, writes=[oa])
                    k.op("dve", lambda e, r_=r_, h=h: e.scalar_tensor_tensor(out=oo[:, i, h * 64:(h + 1) * 64], in0=owv[:, r_, 0:64], scalar=fsc[:, 12 + r_:13 + r_], in1=oa[:, r_, :], op0=ALU.mult, op1=ALU.add),
                         reads=[pb[5], oa] + F, writes=[oo_b[i], R1_b])

            out.append(dict(qk=mm, v=Vw[:, c, g * 65:(g + 1) * 65], vr=[V_b[c]],
                            o=(lambda sub: (pb[5][:, sub * 65:(sub + 1) * 65], pb[5])),
                            first=(c == c0), last=(c == i), fin=(fin if c == i else (lambda: None)), sf=(lambda sub: sub == 0)))
        return out

    jobs = [(g, i) for g in range(2) for i in range(NTT)][:int(os.environ.get('NJ', 64))]
    ONLY = os.environ.get('ONLY', '')
    if jobs: steps += cmp_steps(*jobs[0])
    for n in range(len(jobs)):
        if n + 1 < len(jobs):
            steps += cmp_steps(*jobs[n + 1])
        if ONLY != 'cmp': steps += sw_steps(*jobs[n])
    if steps: run_steps2(k, steps, pb, PT)

    for tt in range(NTT):
        k.dma("sp" if tt % 2 == 0 else "act", o_d(tt)[0], oo[:, tt, :], reads=[oo_b[tt], R1_b], writes=[o_d(tt)[1]])
    k.pop_scope()
    return


D = 1024
NT = 16
TOK = NT * 128
EPS = 1e-6


def emit_B(k, pb, l, last_layer, even, x_d, G_o, out_d, modc, gsc, selp):
    k.push_scope()
    n2c_d = k.dram_once("n2c_%d" % l, [128, 8], F32, "ExternalInput")
    wout_d = k.dram_once("wout_%d" % l, [D, D], F32, "ExternalInput")
    wr_d = k.dram_once("wr_%d" % l, [D, 20], F32, "ExternalInput")
    br_d = k.dram_once("br_%d" % l, [1, 20], F32, "ExternalInput")
    wgate_d = k.dram_once("wgate_%d" % l, [16, D, 256], F32, "ExternalInput")
    wup_d = k.dram_once("wup_%d" % l, [16, D, 256], F32, "ExternalInput")
    wdown_d = k.dram_once("wdown_%d" % l, [16, 256, D], F32, "ExternalInput")
    sel_d = k.dram_once("sel", [32, 16 * 128], BF16)
    id32_d = k.dram_once("id32", [128, 128], F32)
    id16_d = k.dram_once("id16", [128, 128], BF16)
    if last_layer:
        fg_d = k.dram_once("fg", [1, D], F32)

    xa = k.sb([128, NT, D], F32, "xa")
    xa_b = [Buf(f"xa{i}") for i in range(NT)]
    h2T = k.sb([128, 8, TOK], BF16, "h2T")
    h2T_b = [Buf(f"h2T{i}") for i in range(NT)]
    aT = k.sb([128, 4, TOK], BF16, "aT")
    aT_b = [Buf(f"aT{i}") for i in range(4)]
    wbig = k.sb([128, 8, D], BF16, "wbig")
    wbig_b = [Buf("wbigA"), Buf("wbigB")]
    wgu = [k.sb([128, 8, 512], BF16, f"wgu{i}") for i in range(2)]
    stage = [k.sb([128, D], F32, f"stage{i}") for i in range(2)]
    scr = k.sb([128, 24 * 1024 // 4], F32, "scr")
    gB = k.sb([128, D], F32, "gB")
    g1B = g2B = gB
    ocands = [[k.sb([128, D], BF16, f"ocand{jj}_{i}") for i in range(2)] for jj in range(2)]
    combT = k.sb([32, TOK], BF16, "combT")
    combT_b = [Buf(f"combT{i}") for i in range(4)]
    sel = k.sb([32, 16 * 128], BF16, "sel")
    id32 = k.sb([128, 128], F32, "id32")
    id16 = k.sb([128, 128], BF16, "id16")
    n2c = k.sb([128, 8], F32, "n2c")
    a2 = k.sb([128, 8], F32, "a2")
    wr = k.sb([128, 8, 20], F32, "wr")
    br = k.sb([1, 20], F32, "br")
    ones1 = k.sb([1, 128], F32, "ones1")
    smalls = [k.sb([128, 64], F32, f"small{i}") for i in range(2)]
    lgs = [k.sb([128, 20], F32, f"lg{i}") for i in range(2)]
    rts = [k.sb([128, 80], F32, f"rt{i}") for i in range(2)]
    chis = [k.sb([128, 16], BF16, f"chi{i}") for i in range(2)]


    k.dma("sp", sel[:], sel_d.t.ap(), writes=[sel])
    k.dma("sp", id32[:], id32_d.t.ap(), writes=[id32])
    k.dma("sp", id16[:], id16_d.t.ap(), writes=[id16])
    k.dma("sp", n2c[:], n2c_d.t.ap(), writes=[n2c])
    k.dma("sp", wr[:], wr_d.t.ap().rearrange("(c p) n -> p c n", p=128), writes=[wr])
    k.dma("sp", br[:], br_d.t.ap(), writes=[br])
    k.op("dve", lambda e: e.memset(ones1[:], 1.0), writes=[ones1])
    k.op("dve", lambda e: e.scalar_tensor_tensor(out=a2[:], in0=modc[:, 32:40], scalar=1.0, in1=n2c[:], op0=ALU.add, op1=ALU.mult),
         reads=[modc, n2c], writes=[a2])
    sh2 = modc[:, 24:32]

    k.dma("sp", gB[:], gsc.t.ap()[0], reads=[gsc], writes=[gB])
    for c in range(8):
        st = stage[c % 2]
        if even:
            r_, cc_ = c // 4, c % 4
            row0 = (256 * r_ + 128 * cc_) if cc_ < 2 else (512 + 256 * r_ + 128 * (cc_ - 2))
        else:
            row0 = c * 128
        k.dma("sp" if c % 2 == 0 else "act", st[:], wout_d.t.ap()[row0:row0 + 128, :], writes=[st])
        k.op("pool", lambda e, c=c, st=st: e.tensor_tensor(out=wbig[:, c, :], in0=st[:], in1=g1B[:], op=ALU.mult),
             reads=[st, g1B], writes=[wbig_b[0], wbig_b[1]])

    def scr_view(off_bytes, nbytes, dt, shape_tail):
        n32 = nbytes // 4
        ap = scr[:, off_bytes // 4: off_bytes // 4 + n32]
        if dt is not F32:
            ap = ap.bitcast(dt)
        return ap

    o_t = [scr_view(i * 2048, 2048, BF16, None) for i in range(2)]
    o_tb = [Buf("o0"), Buf("o1")]
    oT_t = [scr_view(4096 + i * 2048, 2048, BF16, None) for i in range(2)]
    oT_tb = [Buf("oT0"), Buf("oT1")]
    xn_ts = [scr_view(8192 + i * 4096, 4096, F32, None) for i in range(2)]
    xn_bs = [Buf("xn0"), Buf("xn1")]
    hT32_t = [scr_view(16384 + i * 4096, 4096, F32, None) for i in range(2)]
    hT32_b = [Buf("hT32_0"), Buf("hT32_1")]
    sg_t = [scr_view(i * 2048, 2048, F32, None) for i in range(2)]
    t1_t = [scr_view(4096 + i * 2048, 2048, F32, None) for i in range(2)]

    RBATCH = os.environ.get('RBATCH', '1') == '1'
    LGg = k.sb([128, NT, 4], F32, "LGg")
    LGe = k.sb([128, NT, 16], F32, "LGe")

    def load_B(tt):
        oc = ocands[tt % 2]
        k.dma("sp", xa[:, tt, :], x_d(tt)[0], reads=[x_d(tt)[1]], writes=[xa_b[tt]])
        for h_ in range(2):
            for r_ in range(2):
                row0 = r_ * 2048 + tt * 128
                k.dma("act" if r_ == 0 else "sp", oc[h_][:, r_ * 512:(r_ + 1) * 512], G_o[h_].t.ap()[row0:row0 + 128, :], reads=[G_o[h_]], writes=[oc[h_]])

    def tileB(tt):
        j = tt % 2
        small = smalls[j]; lg = lgs[j]; rt = rts[j]; chi = chis[j]
        xn_t = xn_ts[j]; xn_b = xn_bs[j]
        ocand = ocands[j]
        if tt == 0:
            load_B(0)
        if tt + 1 < NT:
            load_B(tt + 1)
        k.op("dve", lambda e, j=j: e.tensor_scalar(o_t[j], ocand[0][:], selp[:, 0:1], None, op0=ALU.mult), reads=[ocand[0], selp], writes=[o_tb[j]])
        k.op("dve", lambda e, j=j: e.scalar_tensor_tensor(out=o_t[j], in0=ocand[1][:], scalar=selp[:, 1:2], in1=o_t[j], op0=ALU.mult, op1=ALU.add),
             reads=[ocand[1], selp, o_tb[j]], writes=[o_tb[j]])
        yield
        pT = pb[j][:].bitcast(BF16)
        for c in range(8):
            k.op("pe", lambda e, c=c, pT=pT, j=j: e.transpose(pT[:, c * 128:(c + 1) * 128], o_t[j][:, c * 128:(c + 1) * 128], id16[:]),
                 reads=[o_tb[j], id16], writes=[pb[j]], same_ok=True)
        k.op("act", lambda e, pT=pT, j=j: e.copy(oT_t[j], pT), reads=[pb[j]], writes=[oT_tb[j]])
        yield
        for dh in range(2):
            for c in range(8):
                k.op("pe", lambda e, c=c, dh=dh, j=j: e.matmul(pb[2 + dh][:], oT_t[j][:, c * 128:(c + 1) * 128], wbig[:, c, dh * 512:(dh + 1) * 512],
                                                               start=(c == 0), stop=(c == 7)),
                     reads=[oT_tb[j], wbig_b[0], wbig_b[1]], writes=[pb[2 + dh]], same_ok=True)
            k.op("dve", lambda e, dh=dh, tt=tt: e.tensor_tensor(out=xa[:, tt, dh * 512:(dh + 1) * 512], in0=pb[2 + dh][:], in1=xa[:, tt, dh * 512:(dh + 1) * 512], op=ALU.add),
                 reads=[pb[2 + dh], xa_b[tt]], writes=[xa_b[tt]])
        yield
        k.op("dve", lambda e: e.memset(small[:, 0:2], 0.0), writes=[small])
        k.op("act", lambda e, tt=tt, j=j: e.activation(out=oT_t[j], in_=xa[:, tt, :], func=AF.Square, accum_out=small[:, 0:1]),
             reads=[xa_b[tt], small], writes=[oT_tb[j], small])
        k.op("act", lambda e: e.activation(out=small[:, 1:2], in_=small[:, 0:1], func=AF.Sqrt, scale=1.0 / D, bias=EPS),
             reads=[small], writes=[small])
        yield
        k.op("dve", lambda e: e.reciprocal(small[:, 2:3], small[:, 1:2]), reads=[small], writes=[small])
        k.op("dve", lambda e, tt=tt: e.tensor_scalar(xn_t, xa[:, tt, :], small[:, 2:3], None, op0=ALU.mult),
             reads=[xa_b[tt], small], writes=[xn_b])
        yield
        for c in range(8):
            bank = pb[4 + c // 4]
            k.op("pe", lambda e, c=c, bank=bank: e.transpose(bank[:, (c % 4) * 128:(c % 4 + 1) * 128], xn_t[:, c * 128:(c + 1) * 128], id32[:]),
                 reads=[xn_b, id32], writes=[bank], same_ok=True)
        yield
        for c in range(8):
            bank = pb[4 + c // 4]
            src = bank[:, (c % 4) * 128:(c % 4 + 1) * 128]
            k.op("act", lambda e, c=c, src=src, j=j: e.activation(out=hT32_t[j][:, c * 128:(c + 1) * 128], in_=src, func=AF.Identity,
                                                                 scale=a2[:, c:c + 1], bias=sh2[:, c:c + 1]),
                 reads=[bank, a2, modc], writes=[hT32_b[j]])
        yield
        k.op("dve", lambda e, tt=tt, j=j: e.tensor_copy(h2T[:, :, tt * 128:(tt + 1) * 128], hT32_t[j].rearrange("p (c t) -> p c t", c=8)),
             reads=[hT32_b[j]], writes=[h2T_b[tt]])
        for c in range(8):
            k.op("pe", lambda e, c=c, j=j: e.matmul(pb[6][:, 0:20], hT32_t[j][:, c * 128:(c + 1) * 128], wr[:, c, :], start=(c == 0), stop=False),
                 reads=[hT32_b[j], wr], writes=[pb[6]], same_ok=True)
        k.op("pe", lambda e: e.matmul(pb[6][:, 0:20], ones1[:], br[:], start=False, stop=True), reads=[ones1, br], writes=[pb[6]], same_ok=True)
        yield
        if RBATCH:
            k.op("dve", lambda e, tt=tt: e.tensor_copy(LGg[:, tt, :], pb[6][:, 0:4]), reads=[pb[6]], writes=[LGg])
            k.op("dve", lambda e, tt=tt: e.tensor_copy(LGe[:, tt, :], pb[6][:, 4:20]), reads=[pb[6]], writes=[LGe])
            yield
            return
        k.op("dve", lambda e: e.tensor_copy(lg[:], pb[6][:, 0:20]), reads=[pb[6]], writes=[lg])
        gmax = rt[:, 0:1]; ngmax = rt[:, 1:2]; gsum = rt[:, 2:3]; gw = rt[:, 3:4]
        goh = rt[:, 4:8]; pen = rt[:, 8:12]; gexp = rt[:, 12:16]
        elm = rt[:, 16:32]; eq1 = rt[:, 32:48]; eq2 = rt[:, 48:64]
        v1 = rt[:, 64:65]; v2 = rt[:, 65:66]; dd = rt[:, 66:67]; e2 = rt[:, 67:68]; w1 = rt[:, 68:69]; c1 = rt[:, 69:70]; c2 = rt[:, 70:71]
        comb = small[:, 16:32]; chi32 = small[:, 32:48]; c2x = small[:, 32:64]
        R = [lg, rt]
        k.op("dve", lambda e: e.reduce_max(gmax, lg[:, 0:4], axis=AX.X), reads=R, writes=[rt])
        k.op("dve", lambda e: e.tensor_scalar(ngmax, gmax, -1.0, None, op0=ALU.mult), reads=R, writes=[rt])
        k.op("dve", lambda e: e.memset(gsum, 0.0), reads=R, writes=[rt])
        k.op("act", lambda e: e.activation(out=gexp, in_=lg[:, 0:4], func=AF.Exp, bias=ngmax, scale=1.0, accum_out=gsum), reads=R, writes=[rt])
        k.op("dve", lambda e: e.tensor_scalar(goh, lg[:, 0:4], gmax, None, op0=ALU.is_ge), reads=R, writes=[rt])
        k.op("dve", lambda e: e.tensor_scalar(pen, goh, 1e30, -1e30, op0=ALU.mult, op1=ALU.add), reads=R, writes=[rt])
        k.op("dve", lambda e: e.tensor_tensor(out=elm.rearrange("p (g j) -> p g j", g=4), in0=lg[:, 4:20].rearrange("p (g j) -> p g j", g=4),
                                              in1=pen.unsqueeze(2).to_broadcast([128, 4, 4]), op=ALU.add), reads=R, writes=[rt])
        k.op("dve", lambda e: e.reduce_max(v1, elm, axis=AX.X), reads=R, writes=[rt])
        k.op("dve", lambda e: e.tensor_scalar(eq1, elm, v1, None, op0=ALU.is_ge), reads=R, writes=[rt])
        k.op("dve", lambda e: e.scalar_tensor_tensor(out=eq2, in0=eq1, scalar=-2e30, in1=elm, op0=ALU.mult, op1=ALU.add), reads=R, writes=[rt])
        k.op("dve", lambda e: e.reduce_max(v2, eq2, axis=AX.X), reads=R, writes=[rt])
        k.op("dve", lambda e: e.tensor_scalar(eq2, eq2, v2, None, op0=ALU.is_ge), reads=R, writes=[rt])
        k.op("dve", lambda e: e.tensor_tensor(out=dd, in0=v2, in1=v1, op=ALU.subtract), reads=R, writes=[rt])
        k.op("act", lambda e: e.activation(out=e2, in_=dd, func=AF.Exp), reads=R, writes=[rt])
        yield
        k.op("dve", lambda e: e.reciprocal(gw, gsum), reads=R, writes=[rt])
        k.op("dve", lambda e: e.tensor_scalar(w1, e2, 1.0, None, op0=ALU.add), reads=R, writes=[rt])
        k.op("dve", lambda e: e.reciprocal(w1, w1), reads=R, writes=[rt])
        k.op("dve", lambda e: e.tensor_tensor(out=c1, in0=w1, in1=gw, op=ALU.mult), reads=R, writes=[rt])
        k.op("dve", lambda e: e.tensor_tensor(out=c2, in0=c1, in1=e2, op=ALU.mult), reads=R, writes=[rt])
        k.op("dve", lambda e: e.tensor_scalar(comb, eq1, c1, None, op0=ALU.mult), reads=R + [small], writes=[small])
        k.op("dve", lambda e: e.scalar_tensor_tensor(out=comb, in0=eq2, scalar=c2, in1=comb, op0=ALU.mult, op1=ALU.add), reads=R + [small], writes=[small])
        k.op("dve", lambda e: e.tensor_copy(chi[:], comb), reads=[small], writes=[chi])
        k.op("dve", lambda e: e.tensor_copy(chi32, chi[:]), reads=[chi, small], writes=[small])
        k.op("dve", lambda e: e.tensor_tensor(out=small[:, 48:64], in0=comb, in1=chi32, op=ALU.subtract), reads=[small], writes=[small])
        k.op("dve", lambda e: e.tensor_copy(chi[:], small[:, 48:64]), reads=[small], writes=[chi])
        k.op("dve", lambda e: e.tensor_copy(small[:, 48:64], chi[:]), reads=[chi, small], writes=[small])
        k.op("pe", lambda e: e.transpose(pb[7][0:32, 0:128], c2x, id32[:]), reads=[small, id32], writes=[pb[7]])
        k.op("act", lambda e, tt=tt: e.copy(combT[:, tt * 128:(tt + 1) * 128], pb[7][0:32, 0:128]), reads=[pb[7]], writes=[combT_b[tt // 4]])
        yield

    run_streams([tileB(tt) for tt in range(NT)], int(os.environ.get('LAGB', 3)))

    if RBATCH:
        k.barrier()
        RS = Buf("RS")

        def rs(off, n):
            return scr[:, 2048 + off:2048 + off + n]
        gmax = rs(0, 16); gsum = rs(16, 16); gw = rs(32, 16); v1 = rs(48, 16); v2 = rs(64, 16); dd = rs(80, 16)
        e2 = rs(96, 16); w1 = rs(112, 16); c1 = rs(128, 16); c2 = rs(144, 16)
        Gs = rs(256, 64); goh = rs(320, 64); pen = rs(384, 64)
        elm = rs(512, 256); eq1 = rs(768, 256); eq2 = rs(1024, 256); comb = rs(1280, 256); tmp = rs(1536, 256)
        C2 = rs(2048, 512)
        chi = scr[:, 2048 + 2560:2048 + 2560 + 128].bitcast(BF16)

        def v3(ap, m):
            return ap.rearrange("p (a b) -> p a b", b=m)

        def bc(ap, n, m):
            return ap.unsqueeze(2).to_broadcast([128, n, m])

        def dv(fn, reads=()):
            k.op("dve", fn, reads=list(reads) + [RS], writes=[RS])
        dv(lambda e: e.tensor_reduce(out=gmax, in_=LGg[:, :, :], axis=AX.X, op=ALU.max), [LGg])
        dv(lambda e: e.tensor_tensor(out=v3(Gs, 4), in0=LGg[:, :, :], in1=bc(gmax, NT, 4), op=ALU.subtract), [LGg])
        k.op("act", lambda e: e.activation(out=Gs, in_=Gs, func=AF.Exp), reads=[RS], writes=[RS])
        dv(lambda e: e.tensor_reduce(out=gsum, in_=v3(Gs, 4), axis=AX.X, op=ALU.add))
        dv(lambda e: e.reciprocal(gw, gsum))
        dv(lambda e: e.tensor_tensor(out=v3(goh, 4), in0=LGg[:, :, :], in1=bc(gmax, NT, 4), op=ALU.is_ge), [LGg])
        dv(lambda e: e.tensor_scalar(pen, goh, 1e30, -1e30, op0=ALU.mult, op1=ALU.add))
        dv(lambda e: e.tensor_tensor(out=v3(elm, 4), in0=LGe[:, :, :].rearrange("p t (g j) -> p (t g) j", j=4), in1=bc(pen, NT * 4, 4), op=ALU.add), [LGe])
        dv(lambda e: e.tensor_reduce(out=v1, in_=v3(elm, 16), axis=AX.X, op=ALU.max))
        dv(lambda e: e.tensor_tensor(out=v3(eq1, 16), in0=v3(elm, 16), in1=bc(v1, NT, 16), op=ALU.is_ge))
        dv(lambda e: e.scalar_tensor_tensor(out=eq2, in0=eq1, scalar=-2e30, in1=elm, op0=ALU.mult, op1=ALU.add))
        dv(lambda e: e.tensor_reduce(out=v2, in_=v3(eq2, 16), axis=AX.X, op=ALU.max))
        dv(lambda e: e.tensor_tensor(out=v3(eq2, 16), in0=v3(eq2, 16), in1=bc(v2, NT, 16), op=ALU.is_ge))
        dv(lambda e: e.tensor_tensor(out=dd, in0=v2, in1=v1, op=ALU.subtract))
        k.op("act", lambda e: e.activation(out=e2, in_=dd, func=AF.Exp), reads=[RS], writes=[RS])
        dv(lambda e: e.tensor_scalar(w1, e2, 1.0, None, op0=ALU.add))
        dv(lambda e: e.reciprocal(w1, w1))
        dv(lambda e: e.tensor_tensor(out=c1, in0=w1, in1=gw, op=ALU.mult))
        dv(lambda e: e.tensor_tensor(out=c2, in0=c1, in1=e2, op=ALU.mult))
        dv(lambda e: e.tensor_tensor(out=v3(comb, 16), in0=v3(eq1, 16), in1=bc(c1, NT, 16), op=ALU.mult))
        dv(lambda e: e.tensor_tensor(out=v3(tmp, 16), in0=v3(eq2, 16), in1=bc(c2, NT, 16), op=ALU.mult))
        dv(lambda e: e.tensor_tensor(out=comb, in0=comb, in1=tmp, op=ALU.add))
        C3 = v3(C2, 32)
        dv(lambda e: e.tensor_copy(chi, comb))
        dv(lambda e: e.tensor_copy(C3[:, :, 0:16], v3(chi, 16)))
        dv(lambda e: e.tensor_tensor(out=v3(tmp, 16), in0=v3(comb, 16), in1=C3[:, :, 0:16], op=ALU.subtract))
        dv(lambda e: e.tensor_copy(chi, tmp))
        dv(lambda e: e.tensor_copy(C3[:, :, 16:32], v3(chi, 16)))
        for tt in range(NT):
            bank = pb[6 + tt % 2]
            k.op("pe", lambda e, tt=tt, bank=bank: e.transpose(bank[0:32, 0:128], C3[:, tt, :], id32[:]), reads=[RS, id32], writes=[bank])
            k.op("act", lambda e, tt=tt, bank=bank: e.copy(combT[:, tt * 128:(tt + 1) * 128], bank[0:32, 0:128]), reads=[bank], writes=[combT_b[tt // 4]])

    k.dma("sp", gB[:], gsc.t.ap()[1], reads=[gsc], writes=[gB])
    cbs = [k.sb([128, 512], F32, f"cbs{i}") for i in range(2)]
    for pr in range(8):
        wd_buf = wbig_b[pr % 2]
        wd_off = (pr % 2) * 4
        for el_ in range(2):
            ex = pr * 2 + el_
            for fc in range(2):
                st = stage[(el_ * 2 + fc) % 2]
                k.dma("sp" if fc == 0 else "act", st[:], wdown_d.t.ap()[ex, fc * 128:(fc + 1) * 128, :], writes=[st])
                k.op("pool", lambda e, st=st, el_=el_, fc=fc: e.tensor_tensor(out=wbig[:, wd_off + el_ * 2 + fc, :], in0=st[:], in1=g2B[:], op=ALU.mult),
                     reads=[st, g2B], writes=[wd_buf])
        for el_ in range(2):
            ex = pr * 2 + el_
            wb = wgu[ex % 2]
            k.dma("pool", wb[:, :, 0:256], wgate_d.t.ap()[ex].rearrange("(c p) f -> p c f", p=128), writes=[wb])
            k.dma("pool", wb[:, :, 256:512], wup_d.t.ap()[ex].rearrange("(c p) f -> p c f", p=128), writes=[wb])
            for tb in range(4):
                tsl = slice(tb * 512, (tb + 1) * 512)
                hb = h2T_b[tb * 4:(tb + 1) * 4]
                cb = cbs[(ex * 4 + tb) % 2]
                k.op("pe", lambda e, ex=ex, tsl=tsl: e.matmul(pb[4][:], sel[:, ex * 128:(ex + 1) * 128], combT[:, tsl], start=True, stop=True),
                     reads=[sel, combT_b[tb]], writes=[pb[4]], same_ok=True)
                k.op("act", lambda e, cb=cb: e.copy(cb[:], pb[4][:]), reads=[pb[4]], writes=[cb])
                for fc in range(2):
                    jj = (tb * 2 + fc) % 2
                    pg = pb[0 + jj]; pu = pb[2 + jj]
                    for c in range(8):
                        k.op("pe", lambda e, c=c, fc=fc, pg=pg, wb=wb, tsl=tsl: e.matmul(pg[:], wb[:, c, fc * 128:(fc + 1) * 128], h2T[:, c, tsl], start=(c == 0), stop=(c == 7)),
                             reads=[wb] + hb, writes=[pg], same_ok=True)
                    for c in range(8):
                        k.op("pe", lambda e, c=c, fc=fc, pu=pu, wb=wb, tsl=tsl: e.matmul(pu[:], wb[:, c, 256 + fc * 128:256 + (fc + 1) * 128], h2T[:, c, tsl], start=(c == 0), stop=(c == 7)),
                             reads=[wb] + hb, writes=[pu], same_ok=True)
                    k.op("act", lambda e, jj=jj, pg=pg: e.activation(out=sg_t[jj], in_=pg[:], func=AF.Silu), reads=[pg], writes=[o_tb[jj]])
                    k.op("dve", lambda e, jj=jj, pu=pu: e.tensor_tensor(out=t1_t[jj], in0=pu[:], in1=sg_t[jj], op=ALU.mult), reads=[pu, o_tb[jj]], writes=[oT_tb[jj]])
                    k.op("dve", lambda e, jj=jj, el_=el_, fc=fc, tsl=tsl, cb=cb: e.tensor_tensor(out=aT[:, el_ * 2 + fc, tsl], in0=cb[:], in1=t1_t[jj], op=ALU.mult),
                         reads=[cb, oT_tb[jj]], writes=[aT_b[tb]])
        for tt in range(NT):
            for dh in range(2):
                py = pb[5 + (tt * 2 + dh) % 3]
                for jx in range(4):
                    k.op("pe", lambda e, jx=jx, tt=tt, dh=dh, py=py: e.matmul(py[:], aT[:, jx, tt * 128:(tt + 1) * 128], wbig[:, wd_off + jx, dh * 512:(dh + 1) * 512],
                                                                         start=(jx == 0), stop=(jx == 3)),
                         reads=[aT_b[tt // 4], wd_buf], writes=[py], same_ok=True)
                k.op("dve", lambda e, tt=tt, dh=dh, py=py: e.tensor_tensor(out=xa[:, tt, dh * 512:(dh + 1) * 512], in0=py[:], in1=xa[:, tt, dh * 512:(dh + 1) * 512], op=ALU.add),
                     reads=[py, xa_b[tt]], writes=[xa_b[tt]])

    if last_layer:
        k.dma("sp", gB[:], fg_d.t.ap()[0:1, :].to_broadcast([128, D]), writes=[gB])
    for tt in range(NT):
        if last_layer:
            small = smalls[tt % 2]
            k.op("dve", lambda e, small=small: e.memset(small[:, 0:2], 0.0), writes=[small])
            k.op("act", lambda e, tt=tt, small=small: e.activation(out=oT_t[tt % 2], in_=xa[:, tt, :], func=AF.Square, accum_out=small[:, 0:1]),
                 reads=[xa_b[tt], small], writes=[oT_tb[tt % 2], small])
            k.op("act", lambda e, small=small: e.activation(out=small[:, 1:2], in_=small[:, 0:1], func=AF.Sqrt, scale=1.0 / D, bias=EPS), reads=[small], writes=[small])
            k.op("dve", lambda e, small=small: e.reciprocal(small[:, 2:3], small[:, 1:2]), reads=[small], writes=[small])
            k.op("dve", lambda e, tt=tt, small=small: e.scalar_tensor_tensor(out=xa[:, tt, :], in0=xa[:, tt, :], scalar=small[:, 2:3], in1=gB[:], op0=ALU.mult, op1=ALU.mult),
                 reads=[xa_b[tt], small, gB], writes=[xa_b[tt]])
        k.dma("sp" if tt % 2 == 0 else "act", out_d(tt)[0], xa[:, tt, :], reads=[xa_b[tt]], writes=[out_d(tt)[1]])
    k.pop_scope()


def emit_M(k, pb, l, cs, ones4, bselB, bselT, modc, gsc):
    k.push_scope()
    w_d = k.dram_once("adaw_%d" % l, [D, 6144], F32)
    b_d = k.dram_once("adab_%d" % l, [1, 6144], F32)
    wb = [k.sb([128, 3072], F32, f"mw{i}") for i in range(3)]
    bb = k.sb([1, 6144], F32, "mbb")
    modG = k.sb([4, 6144], F32, "modG")
    k.dma("sp", bb[:], b_d.t.ap(), writes=[bb])
    n = 0
    for rnd in range(2):
        for c in range(8):
            w = wb[n % 3]; n += 1
            k.dma("sp" if c % 2 == 0 else "act", w[:], w_d.t.ap()[c * 128:(c + 1) * 128, rnd * 3072:(rnd + 1) * 3072], writes=[w])
            for blk in range(6):
                k.op("pe", lambda e, c=c, blk=blk, w=w: e.matmul(pb[blk][0:4, :], cs[:, c * 4:(c + 1) * 4], w[:, blk * 512:(blk + 1) * 512], start=(c == 0), stop=False),
                     reads=[cs, w], writes=[pb[blk]], same_ok=True)
        for blk in range(6):
            c0 = rnd * 3072 + blk * 512
            k.op("pe", lambda e, blk=blk, c0=c0: e.matmul(pb[blk][0:4, :], ones4[:], bb[:, c0:c0 + 512], start=False, stop=True),
                 reads=[ones4, bb], writes=[pb[blk]], same_ok=True)
            k.op("act", lambda e, blk=blk, c0=c0: e.copy(modG[:, c0:c0 + 512], pb[blk][0:4, :]), reads=[pb[blk]], writes=[modG])
    for idx in range(48):
        j, c = idx // 8, idx % 8
        k.op("pe", lambda e, idx=idx, j=j, c=c: e.matmul(pb[6][:, idx:idx + 1], modG[:, j * 1024 + c * 128:j * 1024 + (c + 1) * 128], bselT[:, 0:1],
                                                       start=(idx == 0), stop=(idx == 47), skip_group_check=True),
             reads=[modG, bselT], writes=[pb[6]], same_ok=True)
    k.op("dve", lambda e: e.tensor_copy(modc[:], pb[6][:, 0:48]), reads=[pb[6]], writes=[modc])
    gt = k.sb([128, 2, D], F32, "gt")
    for (gi, j, b0) in [(0, 2, 0), (1, 5, 2)]:
        for h in range(2):
            bank = pb[b0 + h]
            k.op("pe", lambda e, j=j, h=h, bank=bank: e.matmul(bank[:], bselB[:], modG[:, j * 1024 + h * 512:j * 1024 + (h + 1) * 512], start=True, stop=True),
                 reads=[bselB, modG], writes=[bank])
            k.op("act", lambda e, gi=gi, h=h, bank=bank: e.copy(gt[:, gi, h * 512:(h + 1) * 512], bank[:]), reads=[bank], writes=[gt])
    k.dma("sp", gsc.t.ap().rearrange("g p d -> p g d"), gt[:], reads=[gt], writes=[gsc])
    k.pop_scope()


def build_fused():
    k = KB()
    pb = [k.ps([128, 512], F32, f"pb{i}") for i in range(8)]
    cT_d = k.dram("cT", [128, 32], F32, "ExternalInput")
    bselB_d = k.dram("bselB", [4, 128], F32, "ExternalInput")
    bselT_d = k.dram("bselT", [4, 1], F32, "ExternalInput")
    selp_d = k.dram("selp", [128, 2], F32, "ExternalInput")
    x_in = k.dram("x", [S, D], F32, "ExternalInput")
    xown_in = k.dram("x_own", [2048, D], F32, "ExternalInput")
    xo = k.dram("xo", [2048, D], F32, "ExternalOutput")
    src_o = [k.dram(f"src_o{h}", [2048, 512], BF16, "Internal") for h in range(2)]
    G_o = [k.dram(f"G_o{h}", [4096, 512], BF16, "Internal") for h in range(2)]
    src_x = [k.dram(f"src_x{q}", [512, D], F32, "Internal") for q in range(4)]
    G_x = [k.dram(f"G_x{q}", [1024, D], F32, "Internal") for q in range(4)]

    def x_first(tt):
        return (x_in.t.ap()[tt * 128:(tt + 1) * 128, :], x_in)

    def x_gath(tt):
        r_, q_, w_ = tt // 16, (tt % 16) // 4, (tt % 4) * 128
        return (G_x[q_].t.ap()[r_ * 512 + w_:r_ * 512 + w_ + 128, :], G_x[q_])

    def o_dst(tt):
        return (src_o[tt // 16].t.ap()[(tt % 16) * 128:(tt % 16 + 1) * 128, :], src_o[tt // 16])

    def xown_first(tt):
        return (xown_in.t.ap()[tt * 128:(tt + 1) * 128, :], xown_in)

    def xown_src(tt):
        return (src_x[tt // 4].t.ap()[(tt % 4) * 128:(tt % 4 + 1) * 128, :], src_x[tt // 4])

    def xo_dst(tt):
        return (xo.t.ap()[tt * 128:(tt + 1) * 128, :], xo)

    cT = k.sb([128, 32], F32, "cT"); cs = k.sb([128, 32], F32, "cs")
    ones4 = k.sb([1, 4], F32, "ones4")
    bselB = k.sb([4, 128], F32, "bselB"); bselT = k.sb([4, 1], F32, "bselT")
    selp = k.sb([128, 2], F32, "selp")
    modc = k.sb([128, 48], F32, "modc")
    gsc = k.dram("gsc", [2, 128, D], F32, "Internal")
    for (t, d) in [(cT, cT_d), (bselB, bselB_d), (bselT, bselT_d), (selp, selp_d)]:
        k.dma("sp", t[:], d.t.ap(), writes=[t])
    k.op("dve", lambda e: e.memset(ones4[:], 1.0), writes=[ones4])
    k.op("act", lambda e: e.activation(out=cs[:], in_=cT[:], func=AF.Silu), reads=[cT], writes=[cs])
    groups = [[0, 1], [2, 3], [4, 5], [6, 7]]
    NL = int(os.environ.get("NLAYERS", 4))
    for l in range(NL):
        last = (l == NL - 1)
        STOP = os.environ.get("STOP", "")
        emit_M(k, pb, l, cs, ones4, bselB, bselT, modc, gsc)
        if STOP == "M": break
        x_src = x_first if l == 0 else x_gath
        if l % 2 == 0:
            emit_A_even(k, pb, l, 0.8 - 0.6 * math.exp(-0.3 * l), x_src, o_dst, modc)
        else:
            emit_A_odd(k, pb, l, x_src, o_dst, modc)
        if STOP == "A": break
        for h in range(2):
            k.allgather(src_o[h], G_o[h], groups)
        if STOP == "AG": break
        emit_B(k, pb, l, last and NL == 4, l % 2 == 0, xown_first if l == 0 else xown_src, G_o, xo_dst if last else xown_src, modc, gsc, selp)
        if not last:
            for q in range(4):
                k.allgather(src_x[q], G_x[q], groups)
    k.finish([xo])
    return k


_bf = ml_dtypes.bfloat16
_KF = {}


def kernel(x, c, norm1_g, norm2_g, final_g, ada_w, ada_b, ev_w_in, ev_w_out, ev_lambda, ev_subln_g,
           od_w_in, od_w_out, od_cmp_pos, od_cmp_w1, od_cmp_b1, od_cmp_w2, od_cmp_b2,
           moe_wg, moe_bg, moe_we, moe_be, moe_w_gate, moe_w_up, moe_w_down):
    f32 = lambda a: np.ascontiguousarray(np.asarray(a, dtype=np.float32))
    x = f32(x); c = f32(c)
    NL = int(os.environ.get("NLAYERS", 4))
    if "k" not in _KF:
        _KF["k"] = build_fused()
    kf = _KF["k"]
    pos = np.arange(4096, dtype=np.float32)
    inv = (np.float32(500000.0) ** (-np.arange(0, 16, 2, dtype=np.float32) / np.float32(16))).astype(np.float32)
    ang = pos[:, None] * inv[None, :]
    cosT = np.cos(ang).astype(np.float32); sinT = np.sin(ang).astype(np.float32)
    cosT = np.ascontiguousarray(cosT.reshape(32, 128, 8).transpose(1, 0, 2).reshape(128, 256))
    sinT = np.ascontiguousarray(sinT.reshape(32, 128, 8).transpose(1, 0, 2).reshape(128, 256))
    eall = np.zeros((128, 4096), np.float32)
    for n in range(16):
        eall[n, n * 256:(n + 1) * 256] = 1
    kk = np.arange(128)[:, None]; qq = np.arange(512)[None, :]
    cb = np.concatenate([np.where(128 * d + kk > qq, NEG, 0.0) for d in range(4)], axis=1).astype(np.float32)
    ptab = np.zeros((128, 96), np.float32); ptab[:, 16:32] = -1e30; ptab[:, 32:48] = 1.0; ptab[:, 64 + 16] = 1.0
    cs_ = np.arange(255) * 16; ce = cs_ + 31; ss = np.arange(64) * 64
    overlap = ((cs_[:, None] <= ss[None, :] + 63) & (ce[:, None] >= ss[None, :])).astype(np.float32)
    ov = np.zeros((256, 64), np.float32); ov[:255] = overlap
    ovl = np.concatenate([ov[0:128], ov[128:256]], axis=1)
    npp = np.arange(128)[:, None]; u = np.arange(4096)[None, :]
    bc = np.where(16 * npp + 31 <= u, 0.0, NEG).astype(np.float32)
    oh = (np.arange(4096)[None, :] // 64 == np.arange(128)[:, None]).astype(np.float32)
    k2 = np.arange(128)[:, None]; q2 = np.arange(128)[None, :]
    cbt = np.concatenate([np.where(k2 > q2, NEG, 0.0), np.where(k2 <= q2, NEG, 0.0)], axis=1).astype(np.float32)
    qp = np.arange(128)[:, None]; xx = np.arange(128)[None, :] - 64; own = qp // 64
    TF = np.where((xx == own) | (xx == own - 1), 1e6, -1e30).astype(np.float32)
    TN = np.where(xx <= own, 1e30, -1e30).astype(np.float32)
    sel = np.zeros((32, 16, 128), np.float32)
    for e in range(16):
        sel[e, e, :] = 1.0
        sel[16 + e, e, :] = 1.0
    shared = dict(cosT=cosT, sinT=sinT, id32=np.eye(128, dtype=np.float32), id16=np.eye(128, dtype=np.float32).astype(_bf),
                  eall=eall.astype(_bf), cb=cb.astype(_bf), ptab=ptab, sel=sel.reshape(32, 2048).astype(_bf),
                  cT=np.ascontiguousarray(c.T.reshape(8, 128, 4).transpose(1, 0, 2).reshape(128, 32)))
    if NL > 1:
        shared.update(ovl=ovl.astype(_bf), bc=bc.astype(_bf), oh=oh.astype(_bf), cbt=cbt.astype(_bf), tft=np.concatenate([TF, TN], axis=1))
    if NL == 4:
        shared["fg"] = f32(final_g)[None, :].copy()
    for l in range(NL):
        i = l // 2
        shared["adaw_%d" % l] = f32(ada_w[l]); shared["adab_%d" % l] = f32(ada_b[l])[None, :].copy()
        shared["n1c_%d" % l] = np.ascontiguousarray(f32(norm1_g)[l].reshape(8, 128).T)
        shared["n2c_%d" % l] = np.ascontiguousarray(f32(norm2_g)[l].reshape(8, 128).T)
        shared["wout_%d" % l] = f32(ev_w_out[i]) if l % 2 == 0 else f32(od_w_out[i])
        shared["wr_%d" % l] = np.ascontiguousarray(np.concatenate([f32(moe_wg[l]), f32(moe_we[l])], axis=1))
        shared["br_%d" % l] = np.concatenate([f32(moe_bg[l]), f32(moe_be[l])])[None, :].copy()
        shared["wgate_%d" % l] = f32(moe_w_gate[l]); shared["wup_%d" % l] = f32(moe_w_up[l]); shared["wdown_%d" % l] = f32(moe_w_down[l])
        if l % 2 == 0:
            shared["lam_%d" % l] = f32(ev_lambda[i]).reshape(1, 256).copy()
            shared["subg_%d" % l] = f32(ev_subln_g[i]).reshape(1, 128).copy()
        else:
            pos_ = f32(od_cmp_pos[i]); b1 = f32(od_cmp_b1[i]); w2 = f32(od_cmp_w2[i]); b2 = f32(od_cmp_b2[i])
            shared["w1_%d" % l] = f32(od_cmp_w1[i])
            shared["posT_%d" % l] = np.ascontiguousarray(np.concatenate([pos_[0].T, pos_[1].T], axis=1))
            shared["b1c_%d" % l] = np.ascontiguousarray(b1.reshape(2, 2, 128).transpose(2, 0, 1).reshape(128, 4))
            shared["w2d_%d" % l] = np.ascontiguousarray(np.concatenate([w2, w2], axis=-1))
            shared["b2c_%d" % l] = np.concatenate([b2[0], b2[0]])[:, None].copy()
            shared["b2r_%d" % l] = b2[1][None, :].copy()
    in_maps = []
    for core in range(8):
        b, p = core // 2, core % 2
        d = dict(shared)
        d["x"] = np.ascontiguousarray(x[b]); d["x_own"] = np.ascontiguousarray(x[b, p * 2048:(p + 1) * 2048])
        selp = np.zeros((128, 2), np.float32); selp[:, p] = 1.0
        bselB = np.zeros((4, 128), np.float32); bselB[b, :] = 1.0
        d["selp"] = selp; d["bselB"] = bselB; d["bselT"] = np.ascontiguousarray(bselB[:, 0:1])
        for l in range(NL):
            i = l // 2
            if l % 2 == 0:
                w = f32(ev_w_in[i]); sl = slice(256 * p, 256 * p + 256)
                d["winA_%d" % l] = np.ascontiguousarray(np.concatenate([w[:, 0:512][:, sl], w[:, 512:1024][:, sl], w[:, 1536:2048][:, sl], w[:, 2048:2560][:, sl],
                                                                       w[:, 1024:1536][:, sl], w[:, 2560:3072][:, sl]], axis=1))
            else:
                w = f32(od_w_in[i]); g = slice(128 * p, 128 * p + 128)
                d["winC_%d" % l] = np.ascontiguousarray(np.concatenate([w[:, 512 * p:512 * p + 512], w[:, 1024:1280][:, g], w[:, 1536:1792][:, g], w[:, 2048:2304][:, g],
                                                                       w[:, 1280:1536][:, g], w[:, 1792:2048][:, g], w[:, 2304:2560][:, g],
                                                                       w[:, 2560 + 24 * p:2560 + 24 * p + 24]], axis=1))
        in_maps.append(d)
    res = run_bass_kernel_spmd(kf.nc, in_maps, core_ids=list(range(8))).results
    out = np.zeros_like(x)
    for core in range(8):
        b, p = core // 2, core % 2
        out[b, p * 2048:(p + 1) * 2048] = res[core]["xo"]
    return out
```

```python
import math, os
import numpy as np
import ml_dtypes
from contextlib import ExitStack
import concourse.bass as bass
import concourse.mybir as mybir
from concourse.bass_utils import run_bass_kernel_spmd


F32 = mybir.dt.float32
BF16 = mybir.dt.bfloat16
AF = mybir.ActivationFunctionType
ALU = mybir.AluOpType
AX = mybir.AxisListType

SEM_ROT = 8000


class Buf:
    __slots__ = ("w", "r", "name", "excl")

    def __init__(self, name="", excl=False):
        self.w = None
        self.r = []
        self.name = name
        self.excl = excl


class T:
    def __init__(self, t, name=""):
        self.t = t
        self.b = Buf(name)

    def __getitem__(self, idx):
        return self.t[idx]


class KB:
    def __init__(self):
        self.nc = bass.Bass("TRN2", target_bir_lowering=False)
        self.es = ExitStack()
        self.scopes = []
        self.drams = {}
        nc = self.nc
        self.eng = {"pe": nc.tensor, "act": nc.scalar, "dve": nc.vector, "pool": nc.gpsimd, "sp": nc.sync}
        self.sem = {}
        self.cnt = {}
        self.seen = {e: {} for e in self.eng}
        for e in self.eng:
            self._newsem(e)
        self.dsems = [self.es.enter_context(nc.semaphore(f"dq{i}")) for i in range(24)]
        self.dcnt = [0] * len(self.dsems)
        self.dnext = 0
        self.n_inst = 0
        self.n_wait = 0

    def _newsem(self, e):
        self.sem[e] = self.es.enter_context(self.nc.semaphore(f"s_{e}_{len(self.sem)}_{self.n_inst if hasattr(self,'n_inst') else 0}"))
        self.cnt[e] = 0

    def sb(self, shape, dt=F32, name=None):
        es = self.scopes[-1] if self.scopes else self.es
        p = self.nc.sbuf_base
        st = (p + 31) // 32 * 32
        padb = (-st) % 128
        if padb:
            self._npad = getattr(self, "_npad", 0) + 1
            es.enter_context(self.nc.sbuf_tensor(f"pad_{self._npad}", [128, padb // 2], BF16))
        self._nsb = getattr(self, "_nsb", 0) + 1
        t = es.enter_context(self.nc.sbuf_tensor(f"sb{self._nsb}_" + (name or "t"), list(shape), dt))
        return T(t, name or "")

    def push_scope(self):
        self.scopes.append(ExitStack())

    def pop_scope(self):
        self.barrier()
        self.scopes.pop().close()

    def dram_once(self, name, shape, dt, kind="ExternalInput"):
        if name not in self.drams:
            self.drams[name] = self.dram(name, shape, dt, kind)
        return self.drams[name]

    def barrier(self):
        toks = []
        for e in ("pe", "act", "dve", "pool"):
            if self.cnt[e] > 0:
                toks.append((self.sem[e], self.cnt[e]))
        for i, s_ in enumerate(self.dsems):
            if self.dcnt[i] > 0:
                toks.append((s_, self.dcnt[i]))
        for (s_, v) in getattr(self, "cc_toks", []):
            toks.append((s_, v))
        for e in ("pe", "act", "dve", "pool", "sp"):
            self._wait(e, toks)

    def allgather(self, src, dst, groups):
        toks = self._deps([src], [dst])
        self._wait("pool", toks)
        if not hasattr(self, "cc_sem"):
            self.cc_sem = self.es.enter_context(self.nc.semaphore("cc_sem"))
            self.cc_cnt = 0
            self.cc_toks = []
        self.nc.gpsimd.collective_compute("AllGather", mybir.AluOpType.bypass, replica_groups=groups,
                                          ins=[src.t.ap().opt()], outs=[dst.t.ap().opt()]).then_inc(self.cc_sem)
        self.cc_cnt += 1
        tok = (self.cc_sem, self.cc_cnt)
        self.cc_toks = [tok]
        self._commit(tok, [src], [dst])
        return tok

    def ps(self, shape, dt=F32, name=None):
        t = self.es.enter_context(self.nc.psum_tensor("ps_" + name, list(shape), dt) if name else self.nc.psum_tensor(list(shape), dt))
        r = T(t, name or "")
        r.b.excl = True
        return r

    def dram(self, name, shape, dt, kind):
        if kind == "Internal":
            t = self.nc.dram_tensor(name, list(shape), dt)
        else:
            t = self.nc.dram_tensor(name, list(shape), dt, kind=kind)
        return T(t, name)

    def _deps(self, reads, writes):
        toks = []
        for b in reads:
            b = b.b if isinstance(b, T) else b
            if b.w is not None:
                toks.append(b.w)
            if b.excl:
                toks.extend(b.r)
        for b in writes:
            b = b.b if isinstance(b, T) else b
            if b.w is not None:
                toks.append(b.w)
            toks.extend(b.r)
        return toks

    def _wait(self, e, toks, skip_sem=None):
        eng = self.eng[e]
        need = {}
        for (s, v) in toks:
            if skip_sem is not None and s is skip_sem:
                continue
            if self.seen[e].get(s, 0) >= v:
                continue
            if need.get(s, 0) < v:
                need[s] = v
        for s, v in need.items():
            eng.wait_ge(s, v)
            self.seen[e][s] = v
            self.n_wait += 1

    def _commit(self, tok, reads, writes):
        for b in writes:
            b = b.b if isinstance(b, T) else b
            b.w = tok
            b.r = []
        for b in reads:
            b = b.b if isinstance(b, T) else b
            b.r.append(tok)
            if len(b.r) > 64:
                m = {}
                for (s, v) in b.r:
                    if m.get(s, (None, 0))[1] < v:
                        m[s] = (s, v)
                b.r = list(m.values())

    def op(self, e, fn, reads=(), writes=(), same_ok=False):
        if self.cnt[e] >= SEM_ROT:
            self._newsem(e)
        toks = self._deps(reads, writes)
        self._wait(e, toks, skip_sem=self.sem[e] if same_ok else None)
        ins = fn(self.eng[e])
        self.cnt[e] += 1
        tok = (self.sem[e], self.cnt[e])
        ins.then_inc(self.sem[e], 1)
        self._commit(tok, reads, writes)
        self.n_inst += 1
        return tok

    def dma(self, q, out, in_, reads=(), writes=(), **kw):
        i = self.dnext
        self.dnext = (self.dnext + 1) % len(self.dsems)
        s = self.dsems[i]
        toks = self._deps(reads, writes)
        if self.dcnt[i] > 0:
            toks.append((s, self.dcnt[i]))
        self._wait(q, toks)
        self.dcnt[i] += 16
        self.eng[q].dma_start(out=out, in_=in_, **kw).then_inc(s, 16)
        tok = (s, self.dcnt[i])
        self._commit(tok, reads, writes)
        self.n_inst += 1
        return tok

    def finish(self, outs):
        toks = []
        for b in outs:
            b = b.b if isinstance(b, T) else b
            if b.w is not None:
                toks.append(b.w)
        self._wait("sp", toks)
        for i, s_ in enumerate(self.dsems):
            if self.dcnt[i] > 0:
                self._wait("sp", [(s_, self.dcnt[i])])
        self._wait("sp", getattr(self, "cc_toks", []))
        for e in ("pe", "act", "dve", "pool"):
            if self.cnt[e] > 0:
                self._wait("sp", [(self.sem[e], self.cnt[e])])

def run_streams(gens, lag):
    active = []
    nxt = 0
    g = 0
    while nxt < len(gens) or active:
        if nxt < len(gens) and g >= nxt * lag:
            active.append(gens[nxt]); nxt += 1
        for gen in list(active):
            try:
                next(gen)
            except StopIteration:
                active.remove(gen)
        g += 1

SKIP = set(os.environ.get('SKIP', '').split(','))

D = 1024
S = 4096
NTT = 32
EPS = 1e-6
NEG = -30000.0


def norm_to_hT(k, x_ap, x_buf, small, sq_t, sq_b, xn_t, xn_b, pbA, pbB, id32, a_col, sh_col, col_bufs, outs):
    k.op("dve", lambda e: e.memset(small[:, 0:2], 0.0), writes=[small])
    k.op("act", lambda e: e.activation(out=sq_t, in_=x_ap, func=AF.Square, accum_out=small[:, 0:1]),
         reads=[x_buf, small], writes=[sq_b, small])
    k.op("act", lambda e: e.activation(out=small[:, 1:2], in_=small[:, 0:1], func=AF.Sqrt, scale=1.0 / D, bias=EPS),
         reads=[small], writes=[small])
    yield
    k.op("dve", lambda e: e.reciprocal(small[:, 2:3], small[:, 1:2]), reads=[small], writes=[small])
    k.op("dve", lambda e: e.tensor_scalar(xn_t, x_ap, small[:, 2:3], None, op0=ALU.mult),
         reads=[x_buf, small], writes=[xn_b])
    yield
    for c in range(8):
        bank = pbA if c < 4 else pbB
        k.op("pe", lambda e, c=c, bank=bank: e.transpose(bank[:, (c % 4) * 128:(c % 4 + 1) * 128], xn_t[:, c * 128:(c + 1) * 128], id32[:]),
             reads=[xn_b, id32], writes=[bank], same_ok=True)
    yield
    out_ap, out_buf = outs
    for c in range(8):
        bank = pbA if c < 4 else pbB
        src = bank[:, (c % 4) * 128:(c % 4 + 1) * 128]
        k.op("act", lambda e, c=c, src=src: e.activation(out=out_ap[:, c * 128:(c + 1) * 128], in_=src, func=AF.Identity,
                                                         scale=a_col[:, c:c + 1], bias=sh_col[:, c:c + 1]),
             reads=[bank] + col_bufs, writes=[out_buf])
    yield


def emit_A_even(k, pb, l, lambda_init, x_d, o_d, modc, PH=3):
    k.push_scope()
    n1c_d = k.dram_once("n1c_%d" % l, [128, 8], F32, "ExternalInput")
    win_d = k.dram_once("winA_%d" % l, [D, 1536], F32, "ExternalInput")
    cos_d = k.dram_once("cosT", [128, NTT * 8], F32)
    sin_d = k.dram_once("sinT", [128, NTT * 8], F32)
    lam_d = k.dram_once("lam_%d" % l, [1, 256], F32, "ExternalInput")
    sg_d = k.dram_once("subg_%d" % l, [1, 128], F32, "ExternalInput")
    id32_d = k.dram_once("id32", [128, 128], F32)
    id16_d = k.dram_once("id16", [128, 128], BF16)
    eall_d = k.dram_once("eall", [128, S], BF16)
    cb_d = k.dram_once("cb", [128, 4 * 512], BF16)
    ptab_d = k.dram_once("ptab", [128, 96], F32)

    win = k.sb([128, 8, 1536], BF16, "win")
    QKT = k.sb([128, 8, S], BF16, "QKT")
    QKT_b = [[Buf(f"QKT{j}_{t}") for t in range(8)] for j in range(8)]
    Va = k.sb([128, NTT, 4 * 65], BF16, "Va")
    Vb = k.sb([128, NTT, 2 * 129], BF16, "Vb")
    V_b = [Buf(f"V{t}") for t in range(NTT)]
    oo = k.sb([128, NTT, 512], BF16, "oo")
    oo_b = [Buf(f"oo{t}") for t in range(NTT)]
    eall = k.sb([128, S], BF16, "eall")
    cb = k.sb([128, 4 * 512], BF16, "cb")
    ptab = k.sb([128, 96], F32, "ptab")
    id32 = k.sb([128, 128], F32, "id32")
    id16 = k.sb([128, 128], BF16, "id16")
    n1c = k.sb([128, 8], F32, "n1c")
    a1 = k.sb([128, 8], F32, "a1")
    cosT = k.sb([128, NTT, 8], F32, "cosT")
    sinT = k.sb([128, NTT, 8], F32, "sinT")
    kmT = k.sb([128, 2, 16], BF16, "kmT")
    kmT32 = k.sb([128, 2, 16], F32, "kmT32")
    lamr = k.sb([1, 256], F32, "lamr")
    lams = k.sb([1, 8], F32, "lams")
    ones1 = k.sb([1, 128], F32, "ones1")
    nlam = k.sb([128, 1], F32, "nlam")
    gsl = k.sb([128, 128], F32, "gsl")
    smalls = [k.sb([128, 16], F32, f"small{i}") for i in range(2)]
    xt = [k.sb([128, D], F32, f"xt{i}") for i in range(3)]
    sq = k.sb([128, 128], BF16, "sq")
    hT = [k.sb([128, 8 * 128], BF16, f"hT{i}") for i in range(2)]
    qk16 = [k.sb([128, 1024], BF16, f"qk16_{i}") for i in range(2)]
    rtmps = [k.sb([128, 4, 128], F32, f"rtmp{i}") for i in range(2)]
    PT = [k.sb([128, 512], BF16, f"PT{i}") for i in range(2)]
    gst = k.sb([128, 64], F32, "gst")
    fsc = k.sb([128, 8], F32, "fsc")
    ob32 = k.sb([128, 4, 128], F32, "ob32")
    ob32_b = [Buf(f"ob32_{i}") for i in range(4)]
    QZ = [k.sb([128, 512], BF16, f"QZ{i}") for i in range(2)]
    qzc = [0]
    fss = [k.sb([128, 4], F32, f"fss{i}") for i in range(4)]

    MBTv = [win[:, 0:3, :].rearrange("p a b -> p (a b)")[:, 0:S], win[:, 3:6, :].rearrange("p a b -> p (a b)")[:, 0:S]]
    MBT_b = [Buf("MBT0"), Buf("MBT1")]
    for (t, d) in [(eall, eall_d), (cb, cb_d), (ptab, ptab_d), (id32, id32_d), (id16, id16_d), (n1c, n1c_d), (lamr, lam_d)]:
        k.dma("sp", t[:], d.t.ap(), writes=[t])
    k.dma("sp", cosT[:], cos_d.t.ap().rearrange("p (t f) -> p t f", f=8), writes=[cosT])
    k.dma("sp", sinT[:], sin_d.t.ap().rearrange("p (t f) -> p t f", f=8), writes=[sinT])
    k.dma("sp", gsl[:], sg_d.t.ap()[0:1, :].to_broadcast([128, 128]), writes=[gsl])
    for c in range(8):
        k.dma("pool", win[:, c, :], win_d.t.ap()[c * 128:(c + 1) * 128, :], writes=[win])
    k.op("dve", lambda e: e.memset(ones1[:], 1.0), writes=[ones1])
    k.op("dve", lambda e: e.scalar_tensor_tensor(out=a1[:], in0=modc[:, 8:16], scalar=1.0, in1=n1c[:], op0=ALU.add, op1=ALU.mult),
         reads=[modc, n1c], writes=[a1])
    sh1 = modc[:, 0:8]
    k.op("dve", lambda e: e.tensor_scalar(gsl[:], gsl[:], 1.0 - lambda_init, None, op0=ALU.mult), reads=[gsl], writes=[gsl])
    if "lam" in SKIP: lamr = k.sb([1, 256], F32, "lamr2")
    k.op("dve", lambda e: e.memset(lams[:], 0.0), writes=[lams])
    k.op("dve", lambda e: e.tensor_tensor(out=lamr[:, 0:64], in0=lamr[:, 0:64], in1=lamr[:, 64:128], op=ALU.mult), reads=[lamr], writes=[lamr])
    k.op("dve", lambda e: e.tensor_tensor(out=lamr[:, 128:192], in0=lamr[:, 128:192], in1=lamr[:, 192:256], op=ALU.mult), reads=[lamr], writes=[lamr])
    k.op("dve", lambda e: e.reduce_sum(lams[:, 0:1], lamr[:, 0:64], axis=AX.X), reads=[lamr, lams], writes=[lams])
    k.op("dve", lambda e: e.reduce_sum(lams[:, 1:2], lamr[:, 128:192], axis=AX.X), reads=[lamr, lams], writes=[lams])
    k.op("act", lambda e: e.activation(out=lams[:, 2:4], in_=lams[:, 0:2], func=AF.Exp), reads=[lams], writes=[lams])
    k.op("dve", lambda e: e.tensor_tensor(out=lams[:, 4:5], in0=lams[:, 3:4], in1=lams[:, 2:3], op=ALU.subtract), reads=[lams], writes=[lams])
    k.op("dve", lambda e: e.tensor_scalar(lams[:, 5:6], lams[:, 4:5], -lambda_init, None, op0=ALU.add), reads=[lams], writes=[lams])
    if "nlam" not in SKIP:
        k.op("pe", lambda e: e.matmul(pb[7][:, 0:1], ones1[:], lams[:, 5:6], start=True, stop=True), reads=[ones1, lams], writes=[pb[7]])
        k.op("dve", lambda e: e.tensor_copy(nlam[:], pb[7][:, 0:1]), reads=[pb[7]], writes=[nlam])

    NT1e = int(os.environ.get('NT1', NTT))

    def load_x(tt):
        xb = xt[tt % 3]
        k.dma("sp" if tt % 2 == 0 else "act", xb[:], x_d(tt)[0], reads=[x_d(tt)[1]], writes=[xb])

    def tileA(tt):
        j = tt % 2
        xb = xt[tt % 3]
        small = smalls[j]
        rtmp = rtmps[j]
        if tt == 0:
            load_x(0)
        if tt + 1 < NT1e:
            load_x(tt + 1)
        yield from norm_to_hT(k, xb[:], xb, small, hT[j][:], hT[j], xb[:], xb, pb[0], pb[1], id32, a1, sh1, [a1, modc], (hT[j][:], hT[j]))
        for blk in range(3):
            bank = pb[2 + blk]
            for c in range(8):
                k.op("pe", lambda e, c=c, blk=blk, bank=bank, j=j: e.matmul(bank[:], hT[j][:, c * 128:(c + 1) * 128], win[:, c, blk * 512:(blk + 1) * 512],
                                                                         start=(c == 0), stop=(c == 7)),
                     reads=[hT[j], win], writes=[bank], same_ok=True)
        yield
        for blk in range(2):
            k.op("act", lambda e, blk=blk, j=j: e.copy(qk16[j][:, blk * 512:(blk + 1) * 512], pb[2 + blk][:]), reads=[pb[2 + blk]], writes=[qk16[j]])
        k.op("act", lambda e, tt=tt: e.copy(Va[:, tt, :].rearrange("p (h d) -> p h d", h=4)[:, :, 0:64], pb[4][:, 0:256].rearrange("p (h d) -> p h d", h=4)),
             reads=[pb[4]], writes=[V_b[tt]])
        k.op("act", lambda e, tt=tt: e.copy(Vb[:, tt, :].rearrange("p (h d) -> p h d", h=2)[:, :, 0:128], pb[4][:, 256:512].rearrange("p (h d) -> p h d", h=2)),
             reads=[pb[4]], writes=[V_b[tt]])
        k.op("pool", lambda e, tt=tt: e.memset(Va[:, tt, :].rearrange("p (h d) -> p h d", h=4)[:, :, 64:65], 1.0), writes=[V_b[tt]])
        k.op("pool", lambda e, tt=tt: e.memset(Vb[:, tt, :].rearrange("p (h d) -> p h d", h=2)[:, :, 128:129], 1.0), writes=[V_b[tt]])
        yield
        cB = cosT[:, tt, :].unsqueeze(1).to_broadcast([128, 8, 8])
        sB = sinT[:, tt, :].unsqueeze(1).to_broadcast([128, 8, 8])
        for blk in range(2):
            pv = pb[2 + blk][:].rearrange("p (h d) -> p h d", h=8)
            t1 = pv[:, :, 0:8]; t2 = pv[:, :, 8:16]
            ov = qk16[j][:, blk * 512:(blk + 1) * 512].rearrange("p (h d) -> p h d", h=8)
            r = [rtmp[:, i, 0:64].rearrange("p (h d) -> p h d", h=8) for i in range(4)]
            RB = [pb[2 + blk], cosT, sinT, rtmp, qk16[j]]
            k.op("dve", lambda e, t1=t1, r=r: e.tensor_tensor(out=r[0], in0=t1, in1=cB, op=ALU.mult), reads=RB, writes=[rtmp])
            k.op("dve", lambda e, t2=t2, r=r: e.tensor_tensor(out=r[1], in0=t2, in1=sB, op=ALU.mult), reads=RB, writes=[rtmp])
            k.op("dve", lambda e, t2=t2, r=r: e.tensor_tensor(out=r[2], in0=t2, in1=cB, op=ALU.mult), reads=RB, writes=[rtmp])
            k.op("dve", lambda e, t1=t1, r=r: e.tensor_tensor(out=r[3], in0=t1, in1=sB, op=ALU.mult), reads=RB, writes=[rtmp])
            k.op("dve", lambda e, ov=ov, r=r: e.tensor_tensor(out=ov[:, :, 0:8], in0=r[0], in1=r[1], op=ALU.subtract), reads=[rtmp, qk16[j]], writes=[qk16[j]])
            k.op("dve", lambda e, ov=ov, r=r: e.tensor_tensor(out=ov[:, :, 8:16], in0=r[2], in1=r[3], op=ALU.add), reads=[rtmp, qk16[j]], writes=[qk16[j]])
        yield
        pT = pb[5 + j][:].bitcast(BF16)
        for blk in range(8):
            k.op("pe", lambda e, blk=blk, pT=pT, j=j: e.transpose(pT[:, blk * 128:(blk + 1) * 128], qk16[j][:, blk * 128:(blk + 1) * 128], id16[:]),
                 reads=[qk16[j], id16], writes=[pb[5 + j]], same_ok=True)
        yield
        k.op("dve", lambda e, pT=pT, tt=tt: e.tensor_copy(QKT[:, :, tt * 128:(tt + 1) * 128], pT.rearrange("p (b t) -> p b t", b=8)),
             reads=[pb[5 + j]], writes=[QKT_b[jb][tt // 4] for jb in range(8)])
        yield

    run_streams([tileA(tt) for tt in range(NT1e)], int(os.environ.get('LAGA', 4)))

    for jb in range(0 if 'red' in SKIP else 2):
        k.op("dve", lambda e, jb=jb: e.tensor_reduce(out=kmT32[:, jb, :], in_=QKT[:, 2 + jb, :].rearrange("p (n l) -> p n l", l=256), axis=AX.X, op=ALU.add),
             reads=QKT_b[2 + jb], writes=[kmT32])
    k.op("dve", lambda e: e.tensor_scalar(kmT[:], kmT32[:], 1.0 / 256, None, op0=ALU.mult), reads=[kmT32], writes=[kmT])

    def run_attention(steps, bg=None):
        n = len(steps)

        def emit_qk(i):
            st = steps[i]
            if "pre" in st:
                st["pre"]()
            bank = pb[i % 2]
            mm = st["qk"]
            for ii, (lh, rh, rd) in enumerate(mm):
                k.op("pe", lambda e, lh=lh, rh=rh, ii=ii, bank=bank: e.matmul(bank[:], lh, rh, start=(ii == 0), stop=(ii == len(mm) - 1)),
                     reads=rd, writes=[bank], same_ok=True)

        pending = [None]
        emit_qk(0)
        for i in range(n):
            st = steps[i]
            if i + 1 < n:
                emit_qk(i + 1)
            if bg is not None and i % 2 == 1:
                next(bg, None)
            bank = pb[i % 2]
            pt = PT[i % 2]
            k.op("act", lambda e, bank=bank, pt=pt: e.activation(out=pt[:], in_=bank[:], func=AF.Exp, scale=0.125), reads=[bank], writes=[pt])
            if "oT" in st:
                obT, obT_buf = st["oT"]
                k.op("pe", lambda e, obT=obT, pt=pt, st=st: e.matmul(obT, st["v"], pt[:], start=st["first"], stop=st["last"], skip_group_check=True),
                     reads=[pt] + st["vr"], writes=[obT_buf], same_ok=True)
            else:
                for sub in range(4):
                    oap, obuf = st["o"](sub)
                    k.op("pe", lambda e, sub=sub, oap=oap, pt=pt, st=st: e.matmul(oap, pt[:, sub * 128:(sub + 1) * 128], st["v"], start=(st["first"] and st["sf"](sub)), stop=st["last"], skip_group_check=True),
                         reads=[pt] + st["vr"], writes=[obuf], same_ok=True)
            if pending[0] is not None:
                pending[0]()
                pending[0] = None
            if st["last"]:
                if "fin_a" in st:
                    st["fin_a"]()
                    pending[0] = st["fin"]
                else:
                    st["fin"]()
        if pending[0] is not None:
            pending[0]()
            pending[0] = None

    gsts = [gst, k.sb([128, 64], F32, "gst1")]

    def gate_gen(h):
        jq = h // 2
        r0 = (h % 2) * 64
        mbt = MBTv[h % 2]; mbt_b = MBT_b[h % 2]
        for tt in range(NTT):
            own = tt // 2
            gs_ = gsts[tt % 2]
            k.op("pe", lambda e, tt=tt: e.matmul(pb[6][:, 0:16], QKT[r0:r0 + 64, jq, tt * 128:(tt + 1) * 128], kmT[r0:r0 + 64, jq, :], start=True, stop=True),
                 reads=[QKT_b[jq][tt // 4], kmT], writes=[pb[6]])
            G = [gs_, ptab]
            gsm = gs_[:, 0:16]; m8 = gs_[:, 16:24]; okf = gs_[:, 24:40]; mb = gs_[:, 40:56]
            k.op("dve", lambda e, own=own, gsm=gsm: e.tensor_tensor(out=gsm, in0=pb[6][:, 0:16], in1=ptab[:, 16 - own:32 - own], op=ALU.add), reads=[pb[6]] + G, writes=[gs_])
            k.op("dve", lambda e, gsm=gsm, m8=m8: e.max(out=m8, in_=gsm), reads=G, writes=[gs_])
            k.op("dve", lambda e, gsm=gsm, m8=m8, okf=okf: e.tensor_scalar(okf, gsm, m8[:, 2:3], None, op0=ALU.is_ge), reads=G, writes=[gs_])
            k.op("dve", lambda e, own=own, okf=okf: e.tensor_tensor(out=okf, in0=okf, in1=ptab[:, 32 + 16 - own:32 + 32 - own], op=ALU.mult), reads=G, writes=[gs_])
            k.op("dve", lambda e, own=own, okf=okf: e.tensor_tensor(out=okf, in0=okf, in1=ptab[:, 64 + 16 - own:64 + 32 - own], op=ALU.add), reads=G, writes=[gs_])
            k.op("dve", lambda e, okf=okf, mb=mb: e.tensor_scalar(mb, okf, -NEG, NEG, op0=ALU.mult, op1=ALU.add), reads=G, writes=[gs_])
            yield
            k.op("pe", lambda e, mb=mb: e.transpose(pb[7][0:16, 0:128], mb, id32[:]), reads=[gs_, id32], writes=[pb[7]])
            k.op("act", lambda e, tt=tt, mbt=mbt: e.copy(mbt[0:16, tt * 128:(tt + 1) * 128], pb[7][0:16, 0:128]), reads=[pb[7]], writes=[mbt_b, win])
            yield

    NH_A = 4 if PH >= 2 else 0
    bg0 = gate_gen(0) if NH_A else iter(())
    bgc = [0]
    for dh in range(2 if PH >= 3 else 0):
        for Q in range(8):
            nch = 4 * Q + 4
            steps = []
            for m in range(2):
                r0 = m * 64
                ob = (pb[2 + 2 * m], pb[3 + 2 * m])
                qz = QZ[qzc[0] % 2]; qzc[0] += 1

                def pre(qz=qz, r0=r0, dh=dh, Q=Q):
                    k.op("pool", lambda e: e.memset(qz[64 - r0:128 - r0, :], 0.0), writes=[qz])
                    k.op("pool", lambda e: e.tensor_copy(qz[r0:r0 + 64, :], QKT[r0:r0 + 64, 4 + dh, Q * 512:(Q + 1) * 512]), reads=[QKT_b[4 + dh][Q]], writes=[qz])
                for c in range(nch):
                    mm = [(QKT[:, 6 + dh, c * 128:(c + 1) * 128], qz[:, :], [QKT_b[6 + dh][c // 4], qz])]
                    if c >= 4 * Q:
                        d = c - 4 * Q
                        mm.append((id16[:], cb[:, d * 512:(d + 1) * 512], [id16, cb]))

                    def fin(Q=Q, dh=dh, m=m):
                        if m == 0:
                            return
                        for sub in range(4):
                            o0 = pb[2 + sub // 2][:, (sub % 2) * 129:(sub % 2) * 129 + 129]
                            o1 = pb[4 + sub // 2][:, (sub % 2) * 129:(sub % 2) * 129 + 129]
                            B0 = pb[2 + sub // 2]; B1 = pb[4 + sub // 2]
                            f = fss[sub]
                            F = [f]
                            k.op("dve", lambda e, f=f, o0=o0: e.tensor_scalar(f[:, 0:1], o0[:, 128:129], 1e-30, None, op0=ALU.add), reads=[B0] + F, writes=F)
                            k.op("dve", lambda e, f=f, o1=o1: e.tensor_scalar(f[:, 1:2], o1[:, 128:129], 1e-30, None, op0=ALU.add), reads=[B1] + F, writes=F)
                            k.op("dve", lambda e, f=f: e.reciprocal(f[:, 0:2], f[:, 0:2]), reads=F, writes=F)
                            k.op("dve", lambda e, f=f: e.tensor_tensor(out=f[:, 1:2], in0=f[:, 1:2], in1=nlam[:], op=ALU.mult), reads=F + [nlam], writes=F)
                            k.op("act", lambda e, sub=sub, f=f, o0=o0: e.activation(out=ob32[:, sub, :], in_=o0[:, 0:128], func=AF.Copy, scale=f[:, 0:1]), reads=[B0] + F, writes=[ob32_b[sub]])
                            k.op("dve", lambda e, sub=sub, f=f, o1=o1: e.scalar_tensor_tensor(out=ob32[:, sub, :], in0=o1[:, 0:128], scalar=f[:, 1:2], in1=ob32[:, sub, :], op0=ALU.mult, op1=ALU.add),
                                 reads=[B1, ob32_b[sub]] + F, writes=[ob32_b[sub]])
                        for sub in range(4):
                            tt = Q * 4 + sub
                            f = fss[sub]
                            F = [f]
                            k.op("dve", lambda e, f=f: e.memset(f[:, 2:4], 0.0), reads=F, writes=F)
                            k.op("act", lambda e, sub=sub, f=f: e.activation(out=sq[:, 0:128], in_=ob32[:, sub, :], func=AF.Square, accum_out=f[:, 2:3]), reads=[ob32_b[sub]] + F, writes=[sq] + F)
                            k.op("act", lambda e, f=f: e.activation(out=f[:, 3:4], in_=f[:, 2:3], func=AF.Sqrt, scale=1.0 / 128, bias=EPS), reads=F, writes=F)
                            k.op("dve", lambda e, f=f: e.reciprocal(f[:, 3:4], f[:, 3:4]), reads=F, writes=F)
                            k.op("dve", lambda e, sub=sub, tt=tt, f=f: e.scalar_tensor_tensor(out=oo[:, tt, 256 + dh * 128:256 + (dh + 1) * 128], in0=ob32[:, sub, :], scalar=f[:, 3:4], in1=gsl[:],
                                                                                  op0=ALU.mult, op1=ALU.mult), reads=[ob32_b[sub], gsl] + F, writes=[oo_b[tt]])
                    steps.append(dict(qk=mm, v=Vb[:, c, dh * 129:(dh + 1) * 129], vr=[V_b[c]],
                                      o=(lambda sub, ob=ob: (ob[sub // 2][:, (sub % 2) * 129:(sub % 2) * 129 + 129], ob[sub // 2])),
                                      first=(c == 0), last=(c == nch - 1), fin=fin, sf=(lambda sub: sub % 2 == 0)))
                    if c == 0:
                        steps[-1]["pre"] = pre
            run_attention_diff(k, steps, pb, PT, bg0, bgc)

    if NH_A:
        for _ in bg0:
            pass
    for h in range(NH_A):
        jq = h // 2
        r0 = (h % 2) * 64
        mbt = MBTv[h % 2]; mbt_b = MBT_b[h % 2]
        bg = gate_gen(h + 1) if h + 1 < NH_A else None
        steps = []
        for Q in range(8):
            nch = 4 * Q + 4
            qz = QZ[qzc[0] % 2]; qzc[0] += 1

            def pre(qz=qz, r0=r0, jq=jq, Q=Q):
                k.op("pool", lambda e: e.memset(qz[64 - r0:128 - r0, :], 0.0), writes=[qz])
                k.op("pool", lambda e: e.tensor_copy(qz[r0:r0 + 64, :], QKT[r0:r0 + 64, jq, Q * 512:(Q + 1) * 512]), reads=[QKT_b[jq][Q]], writes=[qz])
            for c in range(nch):
                mm = [(QKT[:, 2 + jq, c * 128:(c + 1) * 128], qz[:, :], [QKT_b[2 + jq][c // 4], qz]),
                      (eall[:, c * 128:(c + 1) * 128], mbt[:, Q * 512:(Q + 1) * 512], [eall, mbt_b])]
                if c >= 4 * Q:
                    d = c - 4 * Q
                    mm.append((id16[:], cb[:, d * 512:(d + 1) * 512], [id16, cb]))
                obank = pb[2 + Q % 2]

                def fin(Q=Q, h=h, obank=obank):
                    ov = obank[:, 0:260].rearrange("p (s d) -> p s d", s=4)
                    k.op("dve", lambda e: e.tensor_scalar(fsc[:, 0:4], ov[:, :, 64], 1e-30, None, op0=ALU.add), reads=[obank, fsc], writes=[fsc])
                    k.op("dve", lambda e: e.reciprocal(fsc[:, 0:4], fsc[:, 0:4]), reads=[fsc], writes=[fsc])
                    for sub in range(4):
                        tt = Q * 4 + sub
                        k.op("act", lambda e, sub=sub, tt=tt: e.activation(out=oo[:, tt, h * 64:(h + 1) * 64], in_=ov[:, sub, 0:64], func=AF.Copy, scale=fsc[:, sub:sub + 1]),
                             reads=[obank, fsc], writes=[oo_b[tt]])
                steps.append(dict(qk=mm, v=Va[:, c, h * 65:(h + 1) * 65], vr=[V_b[c]],
                                  o=(lambda sub, obank=obank: (obank[:, sub * 65:(sub + 1) * 65], obank)),
                                  first=(c == 0), last=(c == nch - 1), fin=fin, sf=(lambda sub: sub == 0)))
                if c == 0:
                    steps[-1]["pre"] = pre
        run_attention(steps, bg)
        if bg is not None:
            for _ in bg:
                pass

    for tt in range(NTT):
        k.dma("sp", o_d(tt)[0], oo[:, tt, :], reads=[oo_b[tt]], writes=[o_d(tt)[1]])
    k.pop_scope()


def run_attention_diff(k, steps, pb, PT, bg=None, bgc=None):
    n = len(steps)

    def emit_qk(i):
        st = steps[i]
        if "pre" in st:
            st["pre"]()
        bank = pb[i % 2]
        mm = st["qk"]
        for ii, q in enumerate(mm):
            lh, rh, rd = q[0], q[1], q[2]
            outv = q[3](bank) if len(q) > 3 else bank[:]
            k.op("pe", lambda e, lh=lh, rh=rh, ii=ii, outv=outv: e.matmul(outv, lh, rh, start=(ii == 0), stop=(ii == len(mm) - 1), skip_group_check=True),
                 reads=rd, writes=[bank], same_ok=True)

    emit_qk(0)
    for i in range(n):
        st = steps[i]
        if i + 1 < n:
            emit_qk(i + 1)
        if bg is not None:
            bgc[0] += 1
            if bgc[0] % 4 == 0:
                next(bg, None)
        bank = pb[i % 2]
        pt = PT[i % 2]
        SM = os.environ.get('STEPMODE', 'all')
        if SM == 'qk': continue
        k.op("act", lambda e, bank=bank, pt=pt: e.activation(out=pt[:], in_=bank[:], func=AF.Exp, scale=0.125), reads=[bank], writes=[pt])
        for sub in range(0 if os.environ.get('STEPMODE', 'all') == 'qkexp' else 4):
            oap, obuf = st["o"](sub)
            k.op("pe", lambda e, sub=sub, oap=oap, pt=pt, st=st: e.matmul(oap, pt[:, sub * 128:(sub + 1) * 128], st["v"], start=(st["first"] and st["sf"](sub)), stop=st["last"], skip_group_check=True),
                 reads=[pt] + st["vr"], writes=[obuf], same_ok=True)
        if st["last"]:
            st["fin"]()


D = 1024
S = 4096
NTT = 32
EPS = 1e-6
NEG = -30000.0
NCOL = 1304
GC = 2.0 * math.sqrt(2.0 / math.pi)


def run_steps2(k, steps, pb, PT):
    n = len(steps)
    first_sw = {st["mst_job"]: idx for idx, st in enumerate(steps) if "mst_job" in st}
    deferred = []

    def emit_qk(i):
        st = steps[i]
        if "pre" in st:
            st["pre"]()
        bank = pb[i % 2]
        first = True
        for (lh, rh, rd, ofn) in st["qk"]:
            k.op("pe", lambda e, lh=lh, rh=rh, bank=bank, ofn=ofn, s0=first: e.matmul(ofn(bank), lh, rh, start=s0, stop=False, skip_group_check=True),
                 reads=rd, writes=[bank], same_ok=True)
            first = False

    emit_qk(0)
    for i in range(n):
        st = steps[i]
        if i + 1 < n:
            emit_qk(i + 1)
        bank = pb[i % 2]
        pt = PT[i % 2]
        k.op("act", lambda e, bank=bank, pt=pt: e.activation(out=pt[:], in_=bank[:], func=AF.Exp, scale=0.125), reads=[bank], writes=[pt])
        for sub in range(4):
            oap, obuf = st["o"](sub)
            k.op("pe", lambda e, sub=sub, oap=oap, pt=pt, st=st: e.matmul(oap, pt[:, sub * 128:(sub + 1) * 128], st["v"], start=(st["first"] and st["sf"](sub)), stop=st["last"], skip_group_check=True),
                 reads=[pt] + st["vr"], writes=[obuf], same_ok=True)
        if st["last"]:
            r = st["fin"]()
            if r is not None:
                job, tail = r
                due = min(i + 3, first_sw.get(job, i) - 2)
                if due <= i:
                    tail()
                else:
                    deferred.append((due, tail))
        while deferred and deferred[0][0] <= i:
            deferred.pop(0)[1]()
    for _, tail in deferred:
        tail()


def emit_A_odd(k, pb, l, x_d, o_d, modc, NT1=NTT, PH=3):
    k.push_scope()
    n1c_d = k.dram_once("n1c_%d" % l, [128, 8], F32, "ExternalInput")
    win_d = k.dram_once("winC_%d" % l, [D, NCOL], F32, "ExternalInput")
    cos_d = k.dram_once("cosT", [128, NTT * 8], F32)
    sin_d = k.dram_once("sinT", [128, NTT * 8], F32)
    id32_d = k.dram_once("id32", [128, 128], F32)
    id16_d = k.dram_once("id16", [128, 128], BF16)
    w1_d = k.dram_once("w1_%d" % l, [2, 2048, 256], F32, "ExternalInput")
    posT_d = k.dram_once("posT_%d" % l, [64, 2 * 32], F32, "ExternalInput")
    b1c_d = k.dram_once("b1c_%d" % l, [128, 4], F32, "ExternalInput")
    w2d_d = k.dram_once("w2d_%d" % l, [2, 256, 128], F32, "ExternalInput")
    b2c_d = k.dram_once("b2c_%d" % l, [128, 1], F32, "ExternalInput")
    b2r_d = k.dram_once("b2r_%d" % l, [1, 64], F32, "ExternalInput")
    ovl_d = k.dram_once("ovl", [128, 2 * 64], BF16)
    bc_d = k.dram_once("bc", [128, S], BF16)
    oh_d = k.dram_once("oh", [128, S], BF16)
    cbt_d = k.dram_once("cbt", [128, 256], BF16)
    tft_d = k.dram_once("tft", [128, 256], F32)

    QT = k.sb([128, 4, S], BF16, "QT")
    QT_b = [Buf(f"QT{t}") for t in range(NTT)]
    KT = k.sb([128, 4, S], BF16, "KT")
    KT_b = [Buf(f"KT{t}") for t in range(NTT)]
    R1 = k.sb([128, 4, S], BF16, "R1")
    R1_b = Buf("R1")
    oo = R1[:, :, :].rearrange("p a b -> p (a b)").rearrange("p (t c) -> p t c", c=512)
    oo_b = [Buf(f"oo{t}") for t in range(NTT)]
    Vs = k.sb([128, NTT, 2 * 65], BF16, "Vs")
    Vw = k.sb([128, NTT, 2 * 65], BF16, "Vw")
    V_b = [Buf(f"V{t}") for t in range(NTT)]
    gates = k.sb([128, NTT, 24], F32, "gates")
    R2 = k.sb([128, 8 * NCOL], BF16, "R2")
    R2_b = Buf("R2")
    win = R2[:, :].rearrange("p (c n) -> p c n", c=8)
    w1s = R2[0:64, 0:32 * 256].rearrange("p (l f) -> p l f", l=32)
    oh = k.sb([128, S], BF16, "oh")
    bc = k.sb([128, S], BF16, "bc")
    cbt = k.sb([128, 256], BF16, "cbt")
    tft = k.sb([128, 256], F32, "tft")
    id32 = k.sb([128, 128], F32, "id32")
    id16 = k.sb([128, 128], BF16, "id16")
    n1c = k.sb([128, 8], F32, "n1c")
    a1 = k.sb([128, 8], F32, "a1")
    cosT = k.sb([128, NTT, 8], F32, "cosT")
    sinT = k.sb([128, NTT, 8], F32, "sinT")
    ones1 = k.sb([1, 128], F32, "ones1")
    smalls = [k.sb([128, 16], F32, f"small{i}") for i in range(2)]
    xt = [k.sb([128, D], F32, f"xt{i}") for i in range(3)]
    hT = [k.sb([128, 8 * 128], BF16, f"hT{i}") for i in range(2)]
    qk32s = [k.sb([128, 896], F32, f"qk32_{i}") for i in range(2)]
    qk16 = [k.sb([128, 1408], BF16, f"qk16_{i}") for i in range(2)]
    rtmps = [k.sb([128, 4, 128], F32, f"rtmp{i}") for i in range(2)]
    PT = [k.sb([128, 512], BF16, f"PT{i}") for i in range(2)]
    posT = k.sb([64, 64], F32, "posT")
    posT16 = k.sb([64, 64], BF16, "posT16")
    b1c = k.sb([128, 4], F32, "b1c")
    w2d = k.sb([128, 2, 2, 128], BF16, "w2d")
    b2c = k.sb([128, 1], F32, "b2c")
    b2r = k.sb([1, 64], F32, "b2r")
    b2B = k.sb([128, 64], F32, "b2B")
    cb1 = k.sb([128, 2], F32, "cb1")
    gel = [k.sb([128, 256], F32, f"gel{i}") for i in range(3)]
    hidT = k.sb([128, 2, 256], BF16, "hidT")
    KCMP = k.sb([128, 2, 256], BF16, "KCMP")
    VCX = k.sb([128, 2, 2, 129], BF16, "VCX")
    imp = k.sb([128, 64], F32, "imp")
    selw = k.sb([128, 160], F32, "selw")
    MST = [k.sb([128, 128], BF16, f"MST{i}") for i in range(2)]
    oacc = [k.sb([128, 4, 64], F32, f"oacc{i}") for i in range(2)]
    fsc = k.sb([128, 32], F32, "fsc")

    for (t, d) in [(oh, oh_d), (bc, bc_d), (cbt, cbt_d), (tft, tft_d), (id32, id32_d), (id16, id16_d), (n1c, n1c_d),
                   (posT, posT_d), (b1c, b1c_d), (b2c, b2c_d), (b2r, b2r_d)]:
        k.dma("sp", t[:], d.t.ap(), writes=[t])
    k.dma("sp", b2B[:], b2r_d.t.ap()[0:1, :].to_broadcast([128, 64]), writes=[b2B])
    k.dma("sp", cosT[:], cos_d.t.ap().rearrange("p (t f) -> p t f", f=8), writes=[cosT])
    k.dma("sp", sinT[:], sin_d.t.ap().rearrange("p (t f) -> p t f", f=8), writes=[sinT])
    for c in range(8):
        k.dma("pool", win[:, c, :], win_d.t.ap()[c * 128:(c + 1) * 128, :], writes=[R2_b])
    for kind in range(2):
        k.dma("pool", w2d[:, kind, :, :], w2d_d.t.ap()[kind].rearrange("(c p) e -> p c e", p=128), writes=[w2d])
    for c in range(2):
        for g in range(2):
            k.dma("sp", VCX[:, c, g, 64:128], ovl_d.t.ap()[:, c * 64:(c + 1) * 64], writes=[VCX])
    k.op("pool", lambda e: e.memset(VCX[:, :, :, 128:129], 1.0), writes=[VCX])
    k.op("pool", lambda e: e.memset(KCMP[:], 0.0), writes=[KCMP])
    for m_ in MST:
        k.op("pool", lambda e, m_=m_: e.memset(m_[:], 0.0), writes=[m_])
    k.op("pool", lambda e: e.memset(hidT[:], 0.0), writes=[hidT])
    k.op("dve", lambda e: e.memset(ones1[:], 1.0), writes=[ones1])
    k.op("dve", lambda e: e.tensor_copy(posT16[:], posT[:]), reads=[posT], writes=[posT16])
    k.op("dve", lambda e: e.scalar_tensor_tensor(out=a1[:], in0=modc[:, 8:16], scalar=1.0, in1=n1c[:], op0=ALU.add, op1=ALU.mult),
         reads=[modc, n1c], writes=[a1])
    sh1 = modc[:, 0:8]

    def load_x(tt):
        xb = xt[tt % 3]
        k.dma("sp" if tt % 2 == 0 else "act", xb[:], x_d(tt)[0], reads=[x_d(tt)[1]], writes=[xb])

    def tileC(tt):
        j = tt % 2
        xb = xt[tt % 3]
        small = smalls[j]
        rtmp = rtmps[j]
        qk32 = qk32s[j]
        if tt == 0:
            load_x(0)
        if tt + 1 < NT1:
            load_x(tt + 1)
        yield from norm_to_hT(k, xb[:], xb, small, hT[j][:], hT[j], xb[:], xb, pb[0], pb[1], id32, a1, sh1, [a1, modc], (hT[j][:], hT[j]))
        widths = [512, 512, NCOL - 1024]
        for blk in range(3):
            bank = pb[2 + blk]
            wd = widths[blk]
            for c in range(8):
                k.op("pe", lambda e, c=c, blk=blk, bank=bank, j=j, wd=wd: e.matmul(bank[:, 0:wd], hT[j][:, c * 128:(c + 1) * 128], win[:, c, blk * 512:blk * 512 + wd],
                                                                                start=(c == 0), stop=(c == 7)),
                     reads=[hT[j], R2_b], writes=[bank], same_ok=True)
        yield
        k.op("act", lambda e: e.copy(qk32[:, 0:512], pb[2][:]), reads=[pb[2]], writes=[qk32])
        k.op("act", lambda e: e.copy(qk32[:, 512:896], pb[3][:, 0:384]), reads=[pb[3]], writes=[qk32])
        k.op("act", lambda e, j=j: e.copy(qk16[j][:, 1280:1408], pb[3][:, 384:512]), reads=[pb[3]], writes=[qk16[j]])
        k.op("act", lambda e, tt=tt: e.copy(Vs[:, tt, :].rearrange("p (g d) -> p g d", g=2)[:, :, 0:64], pb[4][:, 0:128].rearrange("p (g d) -> p g d", g=2)),
             reads=[pb[4]], writes=[V_b[tt]])
        k.op("act", lambda e, tt=tt: e.copy(Vw[:, tt, :].rearrange("p (g d) -> p g d", g=2)[:, :, 0:64], pb[4][:, 128:256].rearrange("p (g d) -> p g d", g=2)),
             reads=[pb[4]], writes=[V_b[tt]])
        k.op("act", lambda e, tt=tt: e.copy(gates[:, tt, :], pb[4][:, 256:280]), reads=[pb[4]], writes=[gates])
        k.op("pool", lambda e, tt=tt: e.memset(Vs[:, tt, :].rearrange("p (g d) -> p g d", g=2)[:, :, 64:65], 1.0), writes=[V_b[tt]])
        k.op("pool", lambda e, tt=tt: e.memset(Vw[:, tt, :].rearrange("p (g d) -> p g d", g=2)[:, :, 64:65], 1.0), writes=[V_b[tt]])
        yield
        cB = cosT[:, tt, :].unsqueeze(1).to_broadcast([128, 14, 8])
        sB = sinT[:, tt, :].unsqueeze(1).to_broadcast([128, 14, 8])
        qv = qk32[:].rearrange("p (h d) -> p h d", h=14)
        t1 = qv[:, :, 0:8]; t2 = qv[:, :, 8:16]
        r = [rtmp[:, i, 0:112].rearrange("p (h d) -> p h d", h=14) for i in range(4)]
        RB = [qk32, cosT, sinT, rtmp]
        k.op("dve", lambda e: e.tensor_tensor(out=r[0], in0=t1, in1=cB, op=ALU.mult), reads=RB, writes=[rtmp])
        k.op("dve", lambda e: e.tensor_tensor(out=r[1], in0=t2, in1=sB, op=ALU.mult), reads=RB, writes=[rtmp])
        k.op("dve", lambda e: e.tensor_tensor(out=r[2], in0=t2, in1=cB, op=ALU.mult), reads=RB, writes=[rtmp])
        k.op("dve", lambda e: e.tensor_tensor(out=r[3], in0=t1, in1=sB, op=ALU.mult), reads=RB, writes=[rtmp])
        k.op("dve", lambda e: e.tensor_tensor(out=t1, in0=r[0], in1=r[1], op=ALU.subtract), reads=[rtmp, qk32], writes=[qk32])
        k.op("dve", lambda e: e.tensor_tensor(out=t2, in0=r[2], in1=r[3], op=ALU.add), reads=[rtmp, qk32], writes=[qk32])
        yield
        k.op("pool", lambda e, j=j: e.tensor_copy(qk16[j][:, 0:512], qk32[:, 0:512]), reads=[qk32], writes=[qk16[j]])
        kdst = qk16[j][:, 512:1280].rearrange("p (k two d) -> p k two d", k=6, two=2)
        ksrc = qk32[:, 512:896].rearrange("p (k d) -> p k d", k=6)
        k.op("pool", lambda e, kdst=kdst, ksrc=ksrc, j=j: e.tensor_copy(kdst[:, :, 0, :], ksrc), reads=[qk32], writes=[qk16[j]])
        k.op("dve", lambda e, kdst=kdst, ksrc=ksrc, j=j: e.tensor_copy(kdst[:, :, 1, :], ksrc), reads=[qk32], writes=[qk16[j]])
        yield
        pA = pb[5][:].bitcast(BF16); pB = pb[6][:].bitcast(BF16)
        for blk in range(8):
            k.op("pe", lambda e, blk=blk, j=j: e.transpose(pA[:, blk * 128:(blk + 1) * 128], qk16[j][:, blk * 128:(blk + 1) * 128], id16[:]),
                 reads=[qk16[j], id16], writes=[pb[5]], same_ok=True)
        for blk in range(2):
            k.op("pe", lambda e, blk=blk, j=j: e.transpose(pB[:, blk * 128:(blk + 1) * 128], qk16[j][:, 1024 + blk * 128:1024 + (blk + 1) * 128], id16[:]),
                 reads=[qk16[j], id16], writes=[pb[6]], same_ok=True)
        for g in range(2):
            k.op("pe", lambda e, g=g, j=j: e.transpose(pB[0:64, 256 + g * 128:256 + (g + 1) * 128], qk16[j][:, 1280 + g * 64:1280 + (g + 1) * 64], id16[:]),
                 reads=[qk16[j], id16], writes=[pb[6]], same_ok=True)
        yield
        tsl = slice(tt * 128, (tt + 1) * 128)
        k.op("dve", lambda e, tsl=tsl: e.tensor_copy(QT[:, :, tsl], pA[:, 0:512].rearrange("p (b t) -> p b t", b=4)), reads=[pb[5]], writes=[QT_b[tt]])
        k.op("dve", lambda e, tsl=tsl: e.tensor_copy(R1[0:64, 0:2, tsl], pA[0:64, 512:768].rearrange("p (b t) -> p b t", b=2)), reads=[pb[5]], writes=[R1_b])
        k.op("dve", lambda e, tsl=tsl: e.tensor_copy(KT[:, 0:2, tsl], pA[:, 768:1024].rearrange("p (b t) -> p b t", b=2)), reads=[pb[5]], writes=[KT_b[tt]])
        k.op("act", lambda e, tsl=tsl: e.copy(KT[:, 2:4, tsl], pB[:, 0:256].rearrange("p (b t) -> p b t", b=2)), reads=[pb[6]], writes=[KT_b[tt]])
        k.op("act", lambda e, tsl=tsl: e.copy(R1[0:64, 2:4, tsl], pB[0:64, 256:512].rearrange("p (b t) -> p b t", b=2)), reads=[pb[6]], writes=[R1_b])
        yield

    run_streams([tileC(tt) for tt in range(NT1)], int(os.environ.get('LAGC', 4)))
    k.op("act", lambda e: e.activation(out=gates[:, :, :], in_=gates[:, :, :], func=AF.Sigmoid), reads=[gates], writes=[gates])

    if PH < 2:
        k.dma("sp", o_d.t.ap()[0:128, :], QT[:, 0, 0:512], reads=QT_b, writes=[o_d])
        k.dma("sp", o_d.t.ap()[128:256, :], KT[:, 0, 0:512], reads=KT_b, writes=[o_d])
        k.dma("sp", o_d.t.ap()[256:384, :], KT[:, 2, 0:512], reads=KT_b, writes=[o_d])
        k.dma("sp", o_d.t.ap()[384:448, :], R1[0:64, 0, 0:512], reads=[R1_b], writes=[o_d])
        k.dma("sp", o_d.t.ap()[448:512, :], R1[0:64, 2, 0:512], reads=[R1_b], writes=[o_d])
        k.finish([o_d])
        return k

    for kind in range(2):
        k.dma("pool", w1s, w1_d.t.ap()[kind].rearrange("(l d) f -> d l f", d=64), reads=[], writes=[R2_b])
        for g in range(2):
            XT = R1[0:64, kind * 2 + g, :]
            for fc in range(2):
                bank = pb[fc]
                for l in range(32):
                    k.op("pe", lambda e, l=l, fc=fc, bank=bank, XT=XT: e.matmul(bank[:, 0:255], w1s[:, l, fc * 128:(fc + 1) * 128], XT[:, l:l + 16 * 254 + 1:16],
                                                                            start=(l == 0), stop=False, skip_group_check=True),
                         reads=[R2_b, R1_b], writes=[bank], same_ok=True)
                for l in range(32):
                    k.op("pe", lambda e, l=l, fc=fc, bank=bank: e.matmul(bank[:, 255:256], w1s[:, l, fc * 128:(fc + 1) * 128], posT16[:, kind * 32 + l:kind * 32 + l + 1],
                                                                     start=False, stop=(l == 31), skip_group_check=True),
                         reads=[R2_b, posT16], writes=[bank], same_ok=True)
                u, t2_, t3_ = gel[0], gel[1], gel[2]
                k.op("dve", lambda e, fc=fc, bank=bank: e.tensor_tensor(out=cb1[:, fc:fc + 1], in0=bank[:, 255:256], in1=b1c[:, kind * 2 + fc:kind * 2 + fc + 1], op=ALU.add),
                     reads=[bank, b1c], writes=[cb1])
                k.op("act", lambda e, fc=fc, bank=bank: e.activation(out=u[:, 0:255], in_=bank[:, 0:255], func=AF.Identity, bias=cb1[:, fc:fc + 1], scale=1.0),
                     reads=[bank, cb1], writes=[u])
                k.op("dve", lambda e: e.tensor_tensor(out=t2_[:, 0:255], in0=u[:, 0:255], in1=u[:, 0:255], op=ALU.mult), reads=[u], writes=[t2_])
                k.op("dve", lambda e: e.tensor_scalar(t2_[:, 0:255], t2_[:, 0:255], 0.044715, 1.0, op0=ALU.mult, op1=ALU.add), reads=[t2_], writes=[t2_])
                k.op("dve", lambda e: e.tensor_tensor(out=t2_[:, 0:255], in0=t2_[:, 0:255], in1=u[:, 0:255], op=ALU.mult), reads=[t2_, u], writes=[t2_])
                k.op("act", lambda e: e.activation(out=t3_[:, 0:255], in_=t2_[:, 0:255], func=AF.Sigmoid, scale=GC), reads=[t2_], writes=[t3_])
                k.op("dve", lambda e, fc=fc: e.tensor_tensor(out=hidT[:, fc, 0:255], in0=t3_[:, 0:255], in1=u[:, 0:255], op=ALU.mult), reads=[t3_, u], writes=[hidT])
            if kind == 0:
                for fc in range(2):
                    k.op("pe", lambda e, fc=fc: e.matmul(pb[2][:, 0:255], w2d[:, 0, fc, :], hidT[:, fc, 0:255], start=(fc == 0), stop=(fc == 1)),
                         reads=[w2d, hidT], writes=[pb[2]], same_ok=True)
                k.op("act", lambda e, g=g: e.activation(out=KCMP[:, g, 0:255], in_=pb[2][:, 0:255], func=AF.Identity, bias=b2c[:, 0:1], scale=1.0),
                     reads=[pb[2], b2c], writes=[KCMP])
            else:
                for c in range(2):
                    nn = 128
                    for fc in range(2):
                        k.op("pe", lambda e, fc=fc, c=c, nn=nn: e.matmul(pb[3][0:nn, c * 64:(c + 1) * 64], hidT[:, fc, c * 128:c * 128 + nn], w2d[:, 1, fc, 0:64],
                                                                        start=(fc == 0 and c == 0), stop=(fc == 1), skip_group_check=True),
                             reads=[w2d, hidT], writes=[pb[3]], same_ok=True)
                k.op("pool", lambda e, g=g: e.memset(VCX[:, 1, g, 0:64], 0.0), writes=[VCX])
                k.op("dve", lambda e, g=g: e.tensor_tensor(out=VCX[:, 0, g, 0:64], in0=pb[3][:, 0:64], in1=b2B[:], op=ALU.add), reads=[pb[3], b2B], writes=[VCX])
                k.op("dve", lambda e, g=g: e.tensor_tensor(out=VCX[0:127, 1, g, 0:64], in0=pb[3][0:127, 64:128], in1=b2B[0:127, :], op=ALU.add), reads=[pb[3], b2B], writes=[VCX])

    if PH < 3:
        k.dma("sp", o_d.t.ap()[0:128, 0:256], KCMP[:, 0, :], reads=[KCMP], writes=[o_d])
        k.dma("sp", o_d.t.ap()[128:256, 0:256], KCMP[:, 1, :], reads=[KCMP], writes=[o_d])
        k.dma("sp", o_d.t.ap()[256:384, 0:258], VCX[:, 0, :, :].rearrange("p b c -> p (b c)"), reads=[VCX], writes=[o_d])
        k.dma("sp", o_d.t.ap()[384:512, 0:258], VCX[:, 1, :, :].rearrange("p b c -> p (b c)"), reads=[VCX], writes=[o_d])
        k.finish([o_d])
        return k

    QZn = [k.sb([128, 512], BF16, f"QZn{i_}") for i_ in range(2)]
    for qz_ in QZn:
        k.op("pool", lambda e, qz_=qz_: e.memset(qz_[:], 0.0), writes=[qz_])

    def qz_pre(g, i, n):
        qz_ = QZn[n % 2]

        def pre():
            for hp in range(2):
                rows = slice(hp * 64, (hp + 1) * 64)
                dst = qz_[rows, :].rearrange("p (b h q) -> p b h q", b=2, h=2)[:, :, hp, :]
                k.op("pool", lambda e, dst=dst, rows=rows: e.tensor_copy(dst, QT[rows, 2 * g:2 * g + 2, i * 128:(i + 1) * 128]), reads=[QT_b[i]], writes=[qz_])
        return pre

    def qk_mms(ktype, g, c, i):
        n = g * NTT + i
        qz_ = QZn[n % 2]
        if ktype == "c":
            lh = KCMP[:, g, c * 128:(c + 1) * 128]; rd = [KCMP, qz_]
        else:
            kb = (0 if ktype == "s" else 2) + g
            lh = KT[:, kb, c * 128:(c + 1) * 128]; rd = [KT_b[c], qz_]
        return [(lh, qz_[:, :], rd, (lambda bank: bank[:, :]))]

    def full(bank):
        return bank[:, :].rearrange("p (r q) -> p r q", r=4)

    def bcast(ap2d, parts):
        return ap2d.unsqueeze(1).to_broadcast([parts, 4, 128])

    def addbias(mm, lh, rh2d, parts, rd):
        mm.append((lh, bcast(rh2d, parts), rd, full))

    steps = []

    def cmp_steps(g, i):
        n = g * NTT + i
        nch = 2 if i >= 16 else 1
        out = []
        for c in range(nch):
            mm = qk_mms("c", g, c, i)
            off = 128 * i - 2048 * c
            addbias(mm, id16[:], bc[:, off:off + 128], 128, [id16, bc])

            def fin(g=g, i=i, n=n):
                A, B = pb[2], pb[3]
                cnt = [0]
                lim = int(os.environ.get('FINOPS', 1000))
                def kop(*a_, **kw_):
                    cnt[0] += 1
                    if cnt[0] <= lim: k.op(*a_, **kw_)
                oc = [A[:, 0:129], A[:, 129:258], B[:, 0:129], B[:, 129:258]]
                bk = [A, A, B, B]
                F = [fsc]
                for r_ in range(4):
                    kop("dve", lambda e, r_=r_: e.tensor_scalar(fsc[:, r_:r_ + 1], oc[r_][:, 128:129], 1e-30, None, op0=ALU.add), reads=[bk[r_]] + F, writes=F)
                kop("dve", lambda e: e.reciprocal(fsc[:, 0:4], fsc[:, 0:4]), reads=F, writes=F)
                kop("dve", lambda e: e.tensor_scalar(imp[:], oc[0][:, 64:128], fsc[:, 0:1], None, op0=ALU.mult), reads=[A] + F, writes=[imp])
                for r_ in range(1, 4):
                    kop("dve", lambda e, r_=r_: e.scalar_tensor_tensor(out=imp[:], in0=oc[r_][:, 64:128], scalar=fsc[:, r_:r_ + 1], in1=imp[:], op0=ALU.mult, op1=ALU.add),
                         reads=[bk[r_], imp] + F, writes=[imp])
                gv = gates[:, i, g * 12:(g + 1) * 12].rearrange("p (r t) -> p r t", t=3)
                kop("dve", lambda e: e.tensor_tensor(out=fsc[:, 4:8], in0=fsc[:, 0:4], in1=gv[:, :, 0], op=ALU.mult), reads=F + [gates], writes=F)
                for r_ in range(4):
                    kop("act", lambda e, r_=r_: e.activation(out=oacc[n % 2][:, r_, :], in_=oc[r_][:, 0:64], func=AF.Copy, scale=fsc[:, 4 + r_:5 + r_]),
                         reads=[bk[r_]] + F, writes=[oacc[n % 2]])
                W = [selw, imp, tft]
                val = selw[:, 0:64]; m8a = selw[:, 64:72]; val2 = selw[:, 72:136]; m8b = selw[:, 136:144]
                kop("dve", lambda e: e.tensor_tensor(out=val, in0=imp[:], in1=tft[:, 64 - 2 * i:128 - 2 * i], op=ALU.max), reads=W, writes=[selw])
                kop("dve", lambda e: e.tensor_tensor(out=val, in0=val, in1=tft[:, 128 + 64 - 2 * i:128 + 128 - 2 * i], op=ALU.min), reads=W, writes=[selw])
                kop("dve", lambda e: e.memset(val[:, 0:1], 1e6), reads=W, writes=[selw])
                if 'NOSEL' in os.environ:
                    kop("dve", lambda e: e.memset(val2, 1.0), reads=W, writes=[selw])
                else:
                    kop("dve", lambda e: e.max(out=m8a, in_=val), reads=W, writes=[selw])
                    kop("dve", lambda e: e.match_replace(out=val2, in_to_replace=m8a, in_values=val, imm_value=-1e30), reads=W, writes=[selw])
                    kop("dve", lambda e: e.max(out=m8b, in_=val2), reads=W, writes=[selw])
                    kop("dve", lambda e: e.tensor_scalar(val2, val, m8b[:, 7:8], None, op0=ALU.is_ge), reads=W, writes=[selw])
                kop("dve", lambda e: e.tensor_scalar(val2, val2, -NEG, NEG, op0=ALU.mult, op1=ALU.add), reads=W, writes=[selw])
                def tail(n=n):
                    k.op("pe", lambda e: e.transpose(pb[3][0:64, 384:512], val2, id32[:]), reads=[selw, id32], writes=[pb[3]])
                    k.op("act", lambda e: e.copy(MST[n % 2][0:64, :], pb[3][0:64, 384:512]), reads=[pb[3]], writes=[MST[n % 2]])
                return (n, tail)

            ob = (pb[2], pb[3])
            out.append(dict(qk=mm, v=VCX[:, c, g, :], vr=[VCX], **({"pre": qz_pre(g, i, n)} if c == 0 else {}),
                            o=(lambda sub, ob=ob: (ob[sub // 2][:, (sub % 2) * 129:(sub % 2) * 129 + 129], ob[sub // 2])),
                            first=(c == 0), last=(c == nch - 1), fin=fin, sf=(lambda sub: sub % 2 == 0)))
        return out

    def sw_steps(g, i):
        n = g * NTT + i
        out = []
        for c in range(i + 1):
            mm = qk_mms("s", g, c, i)
            addbias(mm, oh[:, c * 128:(c + 1) * 128], MST[n % 2][:], 128, [oh, MST[n % 2]])
            if c == i:
                addbias(mm, id16[:], cbt[:, 0:128], 128, [id16, cbt])
            out.append(dict(qk=mm, v=Vs[:, c, g * 65:(g + 1) * 65], vr=[V_b[c]], **({"mst_job": n} if c == 0 else {}),
                            o=(lambda sub: (pb[4][:, sub * 65:(sub + 1) * 65], pb[4])),
                            first=(c == 0), last=(c == i), fin=(lambda: None), sf=(lambda sub: sub == 0)))
        c0 = max(0, i - 4)
        for c in range(c0, i + 1):
            mm = qk_mms("w", g, c, i)
            if c == i:
                addbias(mm, id16[:], cbt[:, 0:128], 128, [id16, cbt])
            if c == i - 4:
                addbias(mm, id16[:], cbt[:, 128:256], 128, [id16, cbt])

            def fin(g=g, i=i, n=n):
                F = [fsc]
                osv = pb[4][:, 0:260].rearrange("p (r d) -> p r d", r=4)
                owv = pb[5][:, 0:260].rearrange("p (r d) -> p r d", r=4)
                gv = gates[:, i, g * 12:(g + 1) * 12].rearrange("p (r t) -> p r t", t=3)
                k.op("dve", lambda e: e.tensor_scalar(fsc[:, 8:12], osv[:, :, 64], 1e-30, None, op0=ALU.add), reads=[pb[4]] + F, writes=F)
                k.op("dve", lambda e: e.tensor_scalar(fsc[:, 12:16], owv[:, :, 64], 1e-30, None, op0=ALU.add), reads=[pb[5]] + F, writes=F)
                k.op("dve", lambda e: e.reciprocal(fsc[:, 8:16], fsc[:, 8:16]), reads=F, writes=F)
                k.op("dve", lambda e: e.tensor_tensor(out=fsc[:, 8:12], in0=fsc[:, 8:12], in1=gv[:, :, 1], op=ALU.mult), reads=F + [gates], writes=F)
                k.op("dve", lambda e: e.tensor_tensor(out=fsc[:, 12:16], in0=fsc[:, 12:16], in1=gv[:, :, 2], op=ALU.mult), reads=F + [gates], writes=F)
                oa = oacc[n % 2]
                for r_ in range(4):
                    h = 4 * g + r_
                    k.op("dve", lambda e, r_=r_: e.scalar_tensor_tensor(out=oa[:, r_, :], in0=osv[:, r_, 0:64], scalar=fsc[:, 8 + r_:9 + r_], in1=oa[:, r_, :], op0=ALU.mult, op1=ALU.add),
                         reads=[pb[4], oa] + F, writes=[oa])
                    k.op("dve", lambda e, r_=r_, h=h: e.scalar_tensor_tensor(out=oo[:, i, h * 64:(h + 1) * 64], in0=owv[:, r_, 0:64], scalar=fsc[:, 12 + r_:13 + r_], in1=oa[:, r_, :], op0=ALU.mult, op1=ALU.add),
                         reads=[pb[5], oa] + F, writes=[oo_b[i], R1_b])

            out.append(dict(qk=mm, v=Vw[:, c, g * 65:(g + 1) * 65], vr=[V_b[c]],
                            o=(lambda sub: (pb[5][:, sub * 65:(sub + 1) * 65], pb[5])),
                            first=(c == c0), last=(c == i), fin=(fin if c == i else (lambda: None)), sf=(lambda sub: sub == 0)))
        return out

    jobs = [(g, i) for g in range(2) for i in range(NTT)][:int(os.environ.get('NJ', 64))]
    ONLY = os.environ.get('ONLY', '')
    if jobs: steps += cmp_steps(*jobs[0])
    for n in range(len(jobs)):
        if n + 1 < len(jobs):
            steps += cmp_steps(*jobs[n + 1])
        if ONLY != 'cmp': steps += sw_steps(*jobs[n])
    if steps: run_steps2(k, steps, pb, PT)

    for tt in range(NTT):
        k.dma("sp", o_d(tt)[0], oo[:, tt, :], reads=[oo_b[tt], R1_b], writes=[o_d(tt)[1]])
    k.pop_scope()
    return


D = 1024
NT = 16
TOK = NT * 128
EPS = 1e-6


def emit_B(k, pb, l, last_layer, even, x_d, G_o, out_d, modc, gsc, selp):
    k.push_scope()
    n2c_d = k.dram_once("n2c_%d" % l, [128, 8], F32, "ExternalInput")
    wout_d = k.dram_once("wout_%d" % l, [D, D], F32, "ExternalInput")
    wr_d = k.dram_once("wr_%d" % l, [D, 20], F32, "ExternalInput")
    br_d = k.dram_once("br_%d" % l, [1, 20], F32, "ExternalInput")
    wgate_d = k.dram_once("wgate_%d" % l, [16, D, 256], F32, "ExternalInput")
    wup_d = k.dram_once("wup_%d" % l, [16, D, 256], F32, "ExternalInput")
    wdown_d = k.dram_once("wdown_%d" % l, [16, 256, D], F32, "ExternalInput")
    sel_d = k.dram_once("sel", [32, 16 * 128], BF16)
    id32_d = k.dram_once("id32", [128, 128], F32)
    id16_d = k.dram_once("id16", [128, 128], BF16)
    if last_layer:
        fg_d = k.dram_once("fg", [1, D], F32)

    xa = k.sb([128, NT, D], F32, "xa")
    xa_b = [Buf(f"xa{i}") for i in range(NT)]
    h2T = k.sb([128, 8, TOK], BF16, "h2T")
    h2T_b = [Buf(f"h2T{i}") for i in range(NT)]
    aT = k.sb([128, 4, TOK], BF16, "aT")
    aT_b = [Buf(f"aT{i}") for i in range(4)]
    wbig = k.sb([128, 8, D], BF16, "wbig")
    wbig_b = [Buf("wbigA"), Buf("wbigB")]
    wgu = [k.sb([128, 8, 512], BF16, f"wgu{i}") for i in range(2)]
    stage = [k.sb([128, D], F32, f"stage{i}") for i in range(2)]
    scr = k.sb([128, 24 * 1024 // 4], F32, "scr")
    gB = k.sb([128, D], F32, "gB")
    g1B = g2B = gB
    ocands = [[k.sb([128, D], BF16, f"ocand{jj}_{i}") for i in range(2)] for jj in range(2)]
    combT = k.sb([32, TOK], BF16, "combT")
    combT_b = [Buf(f"combT{i}") for i in range(4)]
    sel = k.sb([32, 16 * 128], BF16, "sel")
    id32 = k.sb([128, 128], F32, "id32")
    id16 = k.sb([128, 128], BF16, "id16")
    n2c = k.sb([128, 8], F32, "n2c")
    a2 = k.sb([128, 8], F32, "a2")
    wr = k.sb([128, 8, 20], F32, "wr")
    br = k.sb([1, 20], F32, "br")
    ones1 = k.sb([1, 128], F32, "ones1")
    smalls = [k.sb([128, 64], F32, f"small{i}") for i in range(2)]
    lgs = [k.sb([128, 20], F32, f"lg{i}") for i in range(2)]
    rts = [k.sb([128, 80], F32, f"rt{i}") for i in range(2)]
    chis = [k.sb([128, 16], BF16, f"chi{i}") for i in range(2)]


    k.dma("sp", sel[:], sel_d.t.ap(), writes=[sel])
    k.dma("sp", id32[:], id32_d.t.ap(), writes=[id32])
    k.dma("sp", id16[:], id16_d.t.ap(), writes=[id16])
    k.dma("sp", n2c[:], n2c_d.t.ap(), writes=[n2c])
    k.dma("sp", wr[:], wr_d.t.ap().rearrange("(c p) n -> p c n", p=128), writes=[wr])
    k.dma("sp", br[:], br_d.t.ap(), writes=[br])
    k.op("dve", lambda e: e.memset(ones1[:], 1.0), writes=[ones1])
    k.op("dve", lambda e: e.scalar_tensor_tensor(out=a2[:], in0=modc[:, 32:40], scalar=1.0, in1=n2c[:], op0=ALU.add, op1=ALU.mult),
         reads=[modc, n2c], writes=[a2])
    sh2 = modc[:, 24:32]

    k.dma("sp", gB[:], gsc.t.ap()[0], reads=[gsc], writes=[gB])
    for c in range(8):
        st = stage[c % 2]
        if even:
            r_, cc_ = c // 4, c % 4
            row0 = (256 * r_ + 128 * cc_) if cc_ < 2 else (512 + 256 * r_ + 128 * (cc_ - 2))
        else:
            row0 = c * 128
        k.dma("sp" if c % 2 == 0 else "act", st[:], wout_d.t.ap()[row0:row0 + 128, :], writes=[st])
        k.op("pool", lambda e, c=c, st=st: e.tensor_tensor(out=wbig[:, c, :], in0=st[:], in1=g1B[:], op=ALU.mult),
             reads=[st, g1B], writes=[wbig_b[0], wbig_b[1]])

    def scr_view(off_bytes, nbytes, dt, shape_tail):
        n32 = nbytes // 4
        ap = scr[:, off_bytes // 4: off_bytes // 4 + n32]
        if dt is not F32:
            ap = ap.bitcast(dt)
        return ap

    o_t = [scr_view(i * 2048, 2048, BF16, None) for i in range(2)]
    o_tb = [Buf("o0"), Buf("o1")]
    oT_t = [scr_view(4096 + i * 2048, 2048, BF16, None) for i in range(2)]
    oT_tb = [Buf("oT0"), Buf("oT1")]
    xn_ts = [scr_view(8192 + i * 4096, 4096, F32, None) for i in range(2)]
    xn_bs = [Buf("xn0"), Buf("xn1")]
    hT32_t = [scr_view(16384 + i * 4096, 4096, F32, None) for i in range(2)]
    hT32_b = [Buf("hT32_0"), Buf("hT32_1")]
    sg_t = [scr_view(i * 2048, 2048, F32, None) for i in range(2)]
    t1_t = [scr_view(4096 + i * 2048, 2048, F32, None) for i in range(2)]

    RBATCH = os.environ.get('RBATCH', '1') == '1'
    LGg = k.sb([128, NT, 4], F32, "LGg")
    LGe = k.sb([128, NT, 16], F32, "LGe")

    def load_B(tt):
        oc = ocands[tt % 2]
        k.dma("sp", xa[:, tt, :], x_d(tt)[0], reads=[x_d(tt)[1]], writes=[xa_b[tt]])
        for h_ in range(2):
            for r_ in range(2):
                row0 = r_ * 2048 + tt * 128
                k.dma("act" if r_ == 0 else "sp", oc[h_][:, r_ * 512:(r_ + 1) * 512], G_o[h_].t.ap()[row0:row0 + 128, :], reads=[G_o[h_]], writes=[oc[h_]])

    def tileB(tt):
        j = tt % 2
        small = smalls[j]; lg = lgs[j]; rt = rts[j]; chi = chis[j]
        xn_t = xn_ts[j]; xn_b = xn_bs[j]
        ocand = ocands[j]
        if tt == 0:
            load_B(0)
        if tt + 1 < NT:
            load_B(tt + 1)
        k.op("dve", lambda e, j=j: e.tensor_scalar(o_t[j], ocand[0][:], selp[:, 0:1], None, op0=ALU.mult), reads=[ocand[0], selp], writes=[o_tb[j]])
        k.op("dve", lambda e, j=j: e.scalar_tensor_tensor(out=o_t[j], in0=ocand[1][:], scalar=selp[:, 1:2], in1=o_t[j], op0=ALU.mult, op1=ALU.add),
             reads=[ocand[1], selp, o_tb[j]], writes=[o_tb[j]])
        yield
        pT = pb[j][:].bitcast(BF16)
        for c in range(8):
            k.op("pe", lambda e, c=c, pT=pT, j=j: e.transpose(pT[:, c * 128:(c + 1) * 128], o_t[j][:, c * 128:(c + 1) * 128], id16[:]),
                 reads=[o_tb[j], id16], writes=[pb[j]], same_ok=True)
        k.op("act", lambda e, pT=pT, j=j: e.copy(oT_t[j], pT), reads=[pb[j]], writes=[oT_tb[j]])
        yield
        for dh in range(2):
            for c in range(8):
                k.op("pe", lambda e, c=c, dh=dh, j=j: e.matmul(pb[2 + dh][:], oT_t[j][:, c * 128:(c + 1) * 128], wbig[:, c, dh * 512:(dh + 1) * 512],
                                                               start=(c == 0), stop=(c == 7)),
                     reads=[oT_tb[j], wbig_b[0], wbig_b[1]], writes=[pb[2 + dh]], same_ok=True)
            k.op("dve", lambda e, dh=dh, tt=tt: e.tensor_tensor(out=xa[:, tt, dh * 512:(dh + 1) * 512], in0=pb[2 + dh][:], in1=xa[:, tt, dh * 512:(dh + 1) * 512], op=ALU.add),
                 reads=[pb[2 + dh], xa_b[tt]], writes=[xa_b[tt]])
        yield
        k.op("dve", lambda e: e.memset(small[:, 0:2], 0.0), writes=[small])
        k.op("act", lambda e, tt=tt, j=j: e.activation(out=oT_t[j], in_=xa[:, tt, :], func=AF.Square, accum_out=small[:, 0:1]),
             reads=[xa_b[tt], small], writes=[oT_tb[j], small])
        k.op("act", lambda e: e.activation(out=small[:, 1:2], in_=small[:, 0:1], func=AF.Sqrt, scale=1.0 / D, bias=EPS),
             reads=[small], writes=[small])
        yield
        k.op("dve", lambda e: e.reciprocal(small[:, 2:3], small[:, 1:2]), reads=[small], writes=[small])
        k.op("dve", lambda e, tt=tt: e.tensor_scalar(xn_t, xa[:, tt, :], small[:, 2:3], None, op0=ALU.mult),
             reads=[xa_b[tt], small], writes=[xn_b])
        yield
        for c in range(8):
            bank = pb[4 + c // 4]
            k.op("pe", lambda e, c=c, bank=bank: e.transpose(bank[:, (c % 4) * 128:(c % 4 + 1) * 128], xn_t[:, c * 128:(c + 1) * 128], id32[:]),
                 reads=[xn_b, id32], writes=[bank], same_ok=True)
        yield
        for c in range(8):
            bank = pb[4 + c // 4]
            src = bank[:, (c % 4) * 128:(c % 4 + 1) * 128]
            k.op("act", lambda e, c=c, src=src, j=j: e.activation(out=hT32_t[j][:, c * 128:(c + 1) * 128], in_=src, func=AF.Identity,
                                                                 scale=a2[:, c:c + 1], bias=sh2[:, c:c + 1]),
                 reads=[bank, a2, modc], writes=[hT32_b[j]])
        yield
        k.op("dve", lambda e, tt=tt, j=j: e.tensor_copy(h2T[:, :, tt * 128:(tt + 1) * 128], hT32_t[j].rearrange("p (c t) -> p c t", c=8)),
             reads=[hT32_b[j]], writes=[h2T_b[tt]])
        for c in range(8):
            k.op("pe", lambda e, c=c, j=j: e.matmul(pb[6][:, 0:20], hT32_t[j][:, c * 128:(c + 1) * 128], wr[:, c, :], start=(c == 0), stop=False),
                 reads=[hT32_b[j], wr], writes=[pb[6]], same_ok=True)
        k.op("pe", lambda e: e.matmul(pb[6][:, 0:20], ones1[:], br[:], start=False, stop=True), reads=[ones1, br], writes=[pb[6]], same_ok=True)
        yield
        if RBATCH:
            k.op("dve", lambda e, tt=tt: e.tensor_copy(LGg[:, tt, :], pb[6][:, 0:4]), reads=[pb[6]], writes=[LGg])
            k.op("dve", lambda e, tt=tt: e.tensor_copy(LGe[:, tt, :], pb[6][:, 4:20]), reads=[pb[6]], writes=[LGe])
            yield
            return
        k.op("dve", lambda e: e.tensor_copy(lg[:], pb[6][:, 0:20]), reads=[pb[6]], writes=[lg])
        gmax = rt[:, 0:1]; ngmax = rt[:, 1:2]; gsum = rt[:, 2:3]; gw = rt[:, 3:4]
        goh = rt[:, 4:8]; pen = rt[:, 8:12]; gexp = rt[:, 12:16]
        elm = rt[:, 16:32]; eq1 = rt[:, 32:48]; eq2 = rt[:, 48:64]
        v1 = rt[:, 64:65]; v2 = rt[:, 65:66]; dd = rt[:, 66:67]; e2 = rt[:, 67:68]; w1 = rt[:, 68:69]; c1 = rt[:, 69:70]; c2 = rt[:, 70:71]
        comb = small[:, 16:32]; chi32 = small[:, 32:48]; c2x = small[:, 32:64]
        R = [lg, rt]
        k.op("dve", lambda e: e.reduce_max(gmax, lg[:, 0:4], axis=AX.X), reads=R, writes=[rt])
        k.op("dve", lambda e: e.tensor_scalar(ngmax, gmax, -1.0, None, op0=ALU.mult), reads=R, writes=[rt])
        k.op("dve", lambda e: e.memset(gsum, 0.0), reads=R, writes=[rt])
        k.op("act", lambda e: e.activation(out=gexp, in_=lg[:, 0:4], func=AF.Exp, bias=ngmax, scale=1.0, accum_out=gsum), reads=R, writes=[rt])
        k.op("dve", lambda e: e.tensor_scalar(goh, lg[:, 0:4], gmax, None, op0=ALU.is_ge), reads=R, writes=[rt])
        k.op("dve", lambda e: e.tensor_scalar(pen, goh, 1e30, -1e30, op0=ALU.mult, op1=ALU.add), reads=R, writes=[rt])
        k.op("dve", lambda e: e.tensor_tensor(out=elm.rearrange("p (g j) -> p g j", g=4), in0=lg[:, 4:20].rearrange("p (g j) -> p g j", g=4),
                                              in1=pen.unsqueeze(2).to_broadcast([128, 4, 4]), op=ALU.add), reads=R, writes=[rt])
        k.op("dve", lambda e: e.reduce_max(v1, elm, axis=AX.X), reads=R, writes=[rt])
        k.op("dve", lambda e: e.tensor_scalar(eq1, elm, v1, None, op0=ALU.is_ge), reads=R, writes=[rt])
        k.op("dve", lambda e: e.scalar_tensor_tensor(out=eq2, in0=eq1, scalar=-2e30, in1=elm, op0=ALU.mult, op1=ALU.add), reads=R, writes=[rt])
        k.op("dve", lambda e: e.reduce_max(v2, eq2, axis=AX.X), reads=R, writes=[rt])
        k.op("dve", lambda e: e.tensor_scalar(eq2, eq2, v2, None, op0=ALU.is_ge), reads=R, writes=[rt])
        k.op("dve", lambda e: e.tensor_tensor(out=dd, in0=v2, in1=v1, op=ALU.subtract), reads=R, writes=[rt])
        k.op("act", lambda e: e.activation(out=e2, in_=dd, func=AF.Exp), reads=R, writes=[rt])
        yield
        k.op("dve", lambda e: e.reciprocal(gw, gsum), reads=R, writes=[rt])
        k.op("dve", lambda e: e.tensor_scalar(w1, e2, 1.0, None, op0=ALU.add), reads=R, writes=[rt])
        k.op("dve", lambda e: e.reciprocal(w1, w1), reads=R, writes=[rt])
        k.op("dve", lambda e: e.tensor_tensor(out=c1, in0=w1, in1=gw, op=ALU.mult), reads=R, writes=[rt])
        k.op("dve", lambda e: e.tensor_tensor(out=c2, in0=c1, in1=e2, op=ALU.mult), reads=R, writes=[rt])
        k.op("dve", lambda e: e.tensor_scalar(comb, eq1, c1, None, op0=ALU.mult), reads=R + [small], writes=[small])
        k.op("dve", lambda e: e.scalar_tensor_tensor(out=comb, in0=eq2, scalar=c2, in1=comb, op0=ALU.mult, op1=ALU.add), reads=R + [small], writes=[small])
        k.op("dve", lambda e: e.tensor_copy(chi[:], comb), reads=[small], writes=[chi])
        k.op("dve", lambda e: e.tensor_copy(chi32, chi[:]), reads=[chi, small], writes=[small])
        k.op("dve", lambda e: e.tensor_tensor(out=small[:, 48:64], in0=comb, in1=chi32, op=ALU.subtract), reads=[small], writes=[small])
        k.op("dve", lambda e: e.tensor_copy(chi[:], small[:, 48:64]), reads=[small], writes=[chi])
        k.op("dve", lambda e: e.tensor_copy(small[:, 48:64], chi[:]), reads=[chi, small], writes=[small])
        k.op("pe", lambda e: e.transpose(pb[7][0:32, 0:128], c2x, id32[:]), reads=[small, id32], writes=[pb[7]])
        k.op("act", lambda e, tt=tt: e.copy(combT[:, tt * 128:(tt + 1) * 128], pb[7][0:32, 0:128]), reads=[pb[7]], writes=[combT_b[tt // 4]])
        yield

    run_streams([tileB(tt) for tt in range(NT)], int(os.environ.get('LAGB', 3)))

    if RBATCH:
        k.barrier()
        RS = Buf("RS")

        def rs(off, n):
            return scr[:, 2048 + off:2048 + off + n]
        gmax = rs(0, 16); gsum = rs(16, 16); gw = rs(32, 16); v1 = rs(48, 16); v2 = rs(64, 16); dd = rs(80, 16)
        e2 = rs(96, 16); w1 = rs(112, 16); c1 = rs(128, 16); c2 = rs(144, 16)
        Gs = rs(256, 64); goh = rs(320, 64); pen = rs(384, 64)
        elm = rs(512, 256); eq1 = rs(768, 256); eq2 = rs(1024, 256); comb = rs(1280, 256); tmp = rs(1536, 256)
        C2 = rs(2048, 512)
        chi = scr[:, 2048 + 2560:2048 + 2560 + 128].bitcast(BF16)

        def v3(ap, m):
            return ap.rearrange("p (a b) -> p a b", b=m)

        def bc(ap, n, m):
            return ap.unsqueeze(2).to_broadcast([128, n, m])

        def dv(fn, reads=()):
            k.op("dve", fn, reads=list(reads) + [RS], writes=[RS])
        dv(lambda e: e.tensor_reduce(out=gmax, in_=LGg[:, :, :], axis=AX.X, op=ALU.max), [LGg])
        dv(lambda e: e.tensor_tensor(out=v3(Gs, 4), in0=LGg[:, :, :], in1=bc(gmax, NT, 4), op=ALU.subtract), [LGg])
        k.op("act", lambda e: e.activation(out=Gs, in_=Gs, func=AF.Exp), reads=[RS], writes=[RS])
        dv(lambda e: e.tensor_reduce(out=gsum, in_=v3(Gs, 4), axis=AX.X, op=ALU.add))
        dv(lambda e: e.reciprocal(gw, gsum))
        dv(lambda e: e.tensor_tensor(out=v3(goh, 4), in0=LGg[:, :, :], in1=bc(gmax, NT, 4), op=ALU.is_ge), [LGg])
        dv(lambda e: e.tensor_scalar(pen, goh, 1e30, -1e30, op0=ALU.mult, op1=ALU.add))
        dv(lambda e: e.tensor_tensor(out=v3(elm, 4), in0=LGe[:, :, :].rearrange("p t (g j) -> p (t g) j", j=4), in1=bc(pen, NT * 4, 4), op=ALU.add), [LGe])
        dv(lambda e: e.tensor_reduce(out=v1, in_=v3(elm, 16), axis=AX.X, op=ALU.max))
        dv(lambda e: e.tensor_tensor(out=v3(eq1, 16), in0=v3(elm, 16), in1=bc(v1, NT, 16), op=ALU.is_ge))
        dv(lambda e: e.scalar_tensor_tensor(out=eq2, in0=eq1, scalar=-2e30, in1=elm, op0=ALU.mult, op1=ALU.add))
        dv(lambda e: e.tensor_reduce(out=v2, in_=v3(eq2, 16), axis=AX.X, op=ALU.max))
        dv(lambda e: e.tensor_tensor(out=v3(eq2, 16), in0=v3(eq2, 16), in1=bc(v2, NT, 16), op=ALU.is_ge))
        dv(lambda e: e.tensor_tensor(out=dd, in0=v2, in1=v1, op=ALU.subtract))
        k.op("act", lambda e: e.activation(out=e2, in_=dd, func=AF.Exp), reads=[RS], writes=[RS])
        dv(lambda e: e.tensor_scalar(w1, e2, 1.0, None, op0=ALU.add))
        dv(lambda e: e.reciprocal(w1, w1))
        dv(lambda e: e.tensor_tensor(out=c1, in0=w1, in1=gw, op=ALU.mult))
        dv(lambda e: e.tensor_tensor(out=c2, in0=c1, in1=e2, op=ALU.mult))
        dv(lambda e: e.tensor_tensor(out=v3(comb, 16), in0=v3(eq1, 16), in1=bc(c1, NT, 16), op=ALU.mult))
        dv(lambda e: e.tensor_tensor(out=v3(tmp, 16), in0=v3(eq2, 16), in1=bc(c2, NT, 16), op=ALU.mult))
        dv(lambda e: e.tensor_tensor(out=comb, in0=comb, in1=tmp, op=ALU.add))
        C3 = v3(C2, 32)
        dv(lambda e: e.tensor_copy(chi, comb))
        dv(lambda e: e.tensor_copy(C3[:, :, 0:16], v3(chi, 16)))
        dv(lambda e: e.tensor_tensor(out=v3(tmp, 16), in0=v3(comb, 16), in1=C3[:, :, 0:16], op=ALU.subtract))
        dv(lambda e: e.tensor_copy(chi, tmp))
        dv(lambda e: e.tensor_copy(C3[:, :, 16:32], v3(chi, 16)))
        for tt in range(NT):
            bank = pb[6 + tt % 2]
            k.op("pe", lambda e, tt=tt, bank=bank: e.transpose(bank[0:32, 0:128], C3[:, tt, :], id32[:]), reads=[RS, id32], writes=[bank])
            k.op("act", lambda e, tt=tt, bank=bank: e.copy(combT[:, tt * 128:(tt + 1) * 128], bank[0:32, 0:128]), reads=[bank], writes=[combT_b[tt // 4]])

    k.dma("sp", gB[:], gsc.t.ap()[1], reads=[gsc], writes=[gB])
    cbs = [k.sb([128, 512], F32, f"cbs{i}") for i in range(2)]
    for pr in range(8):
        wd_buf = wbig_b[pr % 2]
        wd_off = (pr % 2) * 4
        for el_ in range(2):
            ex = pr * 2 + el_
            for fc in range(2):
                st = stage[(el_ * 2 + fc) % 2]
                k.dma("sp" if fc == 0 else "act", st[:], wdown_d.t.ap()[ex, fc * 128:(fc + 1) * 128, :], writes=[st])
                k.op("pool", lambda e, st=st, el_=el_, fc=fc: e.tensor_tensor(out=wbig[:, wd_off + el_ * 2 + fc, :], in0=st[:], in1=g2B[:], op=ALU.mult),
                     reads=[st, g2B], writes=[wd_buf])
        for el_ in range(2):
            ex = pr * 2 + el_
            wb = wgu[ex % 2]
            k.dma("pool", wb[:, :, 0:256], wgate_d.t.ap()[ex].rearrange("(c p) f -> p c f", p=128), writes=[wb])
            k.dma("pool", wb[:, :, 256:512], wup_d.t.ap()[ex].rearrange("(c p) f -> p c f", p=128), writes=[wb])
            for tb in range(4):
                tsl = slice(tb * 512, (tb + 1) * 512)
                hb = h2T_b[tb * 4:(tb + 1) * 4]
                cb = cbs[(ex * 4 + tb) % 2]
                k.op("pe", lambda e, ex=ex, tsl=tsl: e.matmul(pb[4][:], sel[:, ex * 128:(ex + 1) * 128], combT[:, tsl], start=True, stop=True),
                     reads=[sel, combT_b[tb]], writes=[pb[4]], same_ok=True)
                k.op("act", lambda e, cb=cb: e.copy(cb[:], pb[4][:]), reads=[pb[4]], writes=[cb])
                for fc in range(2):
                    jj = (tb * 2 + fc) % 2
                    pg = pb[0 + jj]; pu = pb[2 + jj]
                    for c in range(8):
                        k.op("pe", lambda e, c=c, fc=fc, pg=pg, wb=wb, tsl=tsl: e.matmul(pg[:], wb[:, c, fc * 128:(fc + 1) * 128], h2T[:, c, tsl], start=(c == 0), stop=(c == 7)),
                             reads=[wb] + hb, writes=[pg], same_ok=True)
                    for c in range(8):
                        k.op("pe", lambda e, c=c, fc=fc, pu=pu, wb=wb, tsl=tsl: e.matmul(pu[:], wb[:, c, 256 + fc * 128:256 + (fc + 1) * 128], h2T[:, c, tsl], start=(c == 0), stop=(c == 7)),
                             reads=[wb] + hb, writes=[pu], same_ok=True)
                    k.op("act", lambda e, jj=jj, pg=pg: e.activation(out=sg_t[jj], in_=pg[:], func=AF.Silu), reads=[pg], writes=[o_tb[jj]])
                    k.op("dve", lambda e, jj=jj, pu=pu: e.tensor_tensor(out=t1_t[jj], in0=pu[:], in1=sg_t[jj], op=ALU.mult), reads=[pu, o_tb[jj]], writes=[oT_tb[jj]])
                    k.op("dve", lambda e, jj=jj, el_=el_, fc=fc, tsl=tsl, cb=cb: e.tensor_tensor(out=aT[:, el_ * 2 + fc, tsl], in0=cb[:], in1=t1_t[jj], op=ALU.mult),
                         reads=[cb, oT_tb[jj]], writes=[aT_b[tb]])
        for tt in range(NT):
            for dh in range(2):
                py = pb[5 + (tt * 2 + dh) % 3]
                for jx in range(4):
                    k.op("pe", lambda e, jx=jx, tt=tt, dh=dh, py=py: e.matmul(py[:], aT[:, jx, tt * 128:(tt + 1) * 128], wbig[:, wd_off + jx, dh * 512:(dh + 1) * 512],
                                                                         start=(jx == 0), stop=(jx == 3)),
                         reads=[aT_b[tt // 4], wd_buf], writes=[py], same_ok=True)
                k.op("dve", lambda e, tt=tt, dh=dh, py=py: e.tensor_tensor(out=xa[:, tt, dh * 512:(dh + 1) * 512], in0=py[:], in1=xa[:, tt, dh * 512:(dh + 1) * 512], op=ALU.add),
                     reads=[py, xa_b[tt]], writes=[xa_b[tt]])

    if last_layer:
        k.dma("sp", gB[:], fg_d.t.ap()[0:1, :].to_broadcast([128, D]), writes=[gB])
    for tt in range(NT):
        if last_layer:
            small = smalls[tt % 2]
            k.op("dve", lambda e, small=small: e.memset(small[:, 0:2], 0.0), writes=[small])
            k.op("act", lambda e, tt=tt, small=small: e.activation(out=oT_t[tt % 2], in_=xa[:, tt, :], func=AF.Square, accum_out=small[:, 0:1]),
                 reads=[xa_b[tt], small], writes=[oT_tb[tt % 2], small])
            k.op("act", lambda e, small=small: e.activation(out=small[:, 1:2], in_=small[:, 0:1], func=AF.Sqrt, scale=1.0 / D, bias=EPS), reads=[small], writes=[small])
            k.op("dve", lambda e, small=small: e.reciprocal(small[:, 2:3], small[:, 1:2]), reads=[small], writes=[small])
            k.op("dve", lambda e, tt=tt, small=small: e.scalar_tensor_tensor(out=xa[:, tt, :], in0=xa[:, tt, :], scalar=small[:, 2:3], in1=gB[:], op0=ALU.mult, op1=ALU.mult),
                 reads=[xa_b[tt], small, gB], writes=[xa_b[tt]])
        k.dma("sp" if tt % 2 == 0 else "act", out_d(tt)[0], xa[:, tt, :], reads=[xa_b[tt]], writes=[out_d(tt)[1]])
    k.pop_scope()


def emit_M(k, pb, l, cs, ones4, bselB, bselT, modc, gsc):
    k.push_scope()
    w_d = k.dram_once("adaw_%d" % l, [D, 6144], F32)
    b_d = k.dram_once("adab_%d" % l, [1, 6144], F32)
    wb = [k.sb([128, 3072], F32, f"mw{i}") for i in range(3)]
    bb = k.sb([1, 6144], F32, "mbb")
    modG = k.sb([4, 6144], F32, "modG")
    k.dma("sp", bb[:], b_d.t.ap(), writes=[bb])
    n = 0
    for rnd in range(2):
        for c in range(8):
            w = wb[n % 3]; n += 1
            k.dma("sp" if c % 2 == 0 else "act", w[:], w_d.t.ap()[c * 128:(c + 1) * 128, rnd * 3072:(rnd + 1) * 3072], writes=[w])
            for blk in range(6):
                k.op("pe", lambda e, c=c, blk=blk, w=w: e.matmul(pb[blk][0:4, :], cs[:, c * 4:(c + 1) * 4], w[:, blk * 512:(blk + 1) * 512], start=(c == 0), stop=False),
                     reads=[cs, w], writes=[pb[blk]], same_ok=True)
        for blk in range(6):
            c0 = rnd * 3072 + blk * 512
            k.op("pe", lambda e, blk=blk, c0=c0: e.matmul(pb[blk][0:4, :], ones4[:], bb[:, c0:c0 + 512], start=False, stop=True),
                 reads=[ones4, bb], writes=[pb[blk]], same_ok=True)
            k.op("act", lambda e, blk=blk, c0=c0: e.copy(modG[:, c0:c0 + 512], pb[blk][0:4, :]), reads=[pb[blk]], writes=[modG])
    for idx in range(48):
        j, c = idx // 8, idx % 8
        k.op("pe", lambda e, idx=idx, j=j, c=c: e.matmul(pb[6][:, idx:idx + 1], modG[:, j * 1024 + c * 128:j * 1024 + (c + 1) * 128], bselT[:, 0:1],
                                                       start=(idx == 0), stop=(idx == 47), skip_group_check=True),
             reads=[modG, bselT], writes=[pb[6]], same_ok=True)
    k.op("dve", lambda e: e.tensor_copy(modc[:], pb[6][:, 0:48]), reads=[pb[6]], writes=[modc])
    gt = k.sb([128, 2, D], F32, "gt")
    for (gi, j, b0) in [(0, 2, 0), (1, 5, 2)]:
        for h in range(2):
            bank = pb[b0 + h]
            k.op("pe", lambda e, j=j, h=h, bank=bank: e.matmul(bank[:], bselB[:], modG[:, j * 1024 + h * 512:j * 1024 + (h + 1) * 512], start=True, stop=True),
                 reads=[bselB, modG], writes=[bank])
            k.op("act", lambda e, gi=gi, h=h, bank=bank: e.copy(gt[:, gi, h * 512:(h + 1) * 512], bank[:]), reads=[bank], writes=[gt])
    k.dma("sp", gsc.t.ap().rearrange("g p d -> p g d"), gt[:], reads=[gt], writes=[gsc])
    k.pop_scope()


def build_fused():
    k = KB()
    pb = [k.ps([128, 512], F32, f"pb{i}") for i in range(8)]
    cT_d = k.dram("cT", [128, 32], F32, "ExternalInput")
    bselB_d = k.dram("bselB", [4, 128], F32, "ExternalInput")
    bselT_d = k.dram("bselT", [4, 1], F32, "ExternalInput")
    selp_d = k.dram("selp", [128, 2], F32, "ExternalInput")
    x_in = k.dram("x", [S, D], F32, "ExternalInput")
    xown_in = k.dram("x_own", [2048, D], F32, "ExternalInput")
    xo = k.dram("xo", [2048, D], F32, "ExternalOutput")
    src_o = [k.dram(f"src_o{h}", [2048, 512], BF16, "Internal") for h in range(2)]
    G_o = [k.dram(f"G_o{h}", [4096, 512], BF16, "Internal") for h in range(2)]
    src_x = [k.dram(f"src_x{q}", [512, D], F32, "Internal") for q in range(4)]
    G_x = [k.dram(f"G_x{q}", [1024, D], F32, "Internal") for q in range(4)]

    def x_first(tt):
        return (x_in.t.ap()[tt * 128:(tt + 1) * 128, :], x_in)

    def x_gath(tt):
        r_, q_, w_ = tt // 16, (tt % 16) // 4, (tt % 4) * 128
        return (G_x[q_].t.ap()[r_ * 512 + w_:r_ * 512 + w_ + 128, :], G_x[q_])

    def o_dst(tt):
        return (src_o[tt // 16].t.ap()[(tt % 16) * 128:(tt % 16 + 1) * 128, :], src_o[tt // 16])

    def xown_first(tt):
        return (xown_in.t.ap()[tt * 128:(tt + 1) * 128, :], xown_in)

    def xown_src(tt):
        return (src_x[tt // 4].t.ap()[(tt % 4) * 128:(tt % 4 + 1) * 128, :], src_x[tt // 4])

    def xo_dst(tt):
        return (xo.t.ap()[tt * 128:(tt + 1) * 128, :], xo)

    cT = k.sb([128, 32], F32, "cT"); cs = k.sb([128, 32], F32, "cs")
    ones4 = k.sb([1, 4], F32, "ones4")
    bselB = k.sb([4, 128], F32, "bselB"); bselT = k.sb([4, 1], F32, "bselT")
    selp = k.sb([128, 2], F32, "selp")
    modc = k.sb([128, 48], F32, "modc")
    gsc = k.dram("gsc", [2, 128, D], F32, "Internal")
    for (t, d) in [(cT, cT_d), (bselB, bselB_d), (bselT, bselT_d), (selp, selp_d)]:
        k.dma("sp", t[:], d.t.ap(), writes=[t])
    k.op("dve", lambda e: e.memset(ones4[:], 1.0), writes=[ones4])
    k.op("act", lambda e: e.activation(out=cs[:], in_=cT[:], func=AF.Silu), reads=[cT], writes=[cs])
    groups = [[0, 1], [2, 3], [4, 5], [6, 7]]
    NL = int(os.environ.get("NLAYERS", 4))
    for l in range(NL):
        last = (l == NL - 1)
        STOP = os.environ.get("STOP", "")
        emit_M(k, pb, l, cs, ones4, bselB, bselT, modc, gsc)
        if STOP == "M": break
        x_src = x_first if l == 0 else x_gath
        if l % 2 == 0:
            emit_A_even(k, pb, l, 0.8 - 0.6 * math.exp(-0.3 * l), x_src, o_dst, modc)
        else:
            emit_A_odd(k, pb, l, x_src, o_dst, modc)
        if STOP == "A": break
        for h in range(2):
            k.allgather(src_o[h], G_o[h], groups)
        if STOP == "AG": break
        emit_B(k, pb, l, last and NL == 4, l % 2 == 0, xown_first if l == 0 else xown_src, G_o, xo_dst if last else xown_src, modc, gsc, selp)
        if not last:
            for q in range(4):
                k.allgather(src_x[q], G_x[q], groups)
    k.finish([xo])
    return k


_bf = ml_dtypes.bfloat16
_KF = {}


def kernel(x, c, norm1_g, norm2_g, final_g, ada_w, ada_b, ev_w_in, ev_w_out, ev_lambda, ev_subln_g,
           od_w_in, od_w_out, od_cmp_pos, od_cmp_w1, od_cmp_b1, od_cmp_w2, od_cmp_b2,
           moe_wg, moe_bg, moe_we, moe_be, moe_w_gate, moe_w_up, moe_w_down):
    f32 = lambda a: np.ascontiguousarray(np.asarray(a, dtype=np.float32))
    x = f32(x); c = f32(c)
    NL = int(os.environ.get("NLAYERS", 4))
    if "k" not in _KF:
        _KF["k"] = build_fused()
    kf = _KF["k"]
    pos = np.arange(4096, dtype=np.float32)
    inv = (np.float32(500000.0) ** (-np.arange(0, 16, 2, dtype=np.float32) / np.float32(16))).astype(np.float32)
    ang = pos[:, None] * inv[None, :]
    cosT = np.cos(ang).astype(np.float32); sinT = np.sin(ang).astype(np.float32)
    cosT = np.ascontiguousarray(cosT.reshape(32, 128, 8).transpose(1, 0, 2).reshape(128, 256))
    sinT = np.ascontiguousarray(sinT.reshape(32, 128, 8).transpose(1, 0, 2).reshape(128, 256))
    eall = np.zeros((128, 4096), np.float32)
    for n in range(16):
        eall[n, n * 256:(n + 1) * 256] = 1
    kk = np.arange(128)[:, None]; qq = np.arange(512)[None, :]
    cb = np.concatenate([np.where(128 * d + kk > qq, NEG, 0.0) for d in range(4)], axis=1).astype(np.float32)
    ptab = np.zeros((128, 96), np.float32); ptab[:, 16:32] = -1e30; ptab[:, 32:48] = 1.0; ptab[:, 64 + 16] = 1.0
    cs_ = np.arange(255) * 16; ce = cs_ + 31; ss = np.arange(64) * 64
    overlap = ((cs_[:, None] <= ss[None, :] + 63) & (ce[:, None] >= ss[None, :])).astype(np.float32)
    ov = np.zeros((256, 64), np.float32); ov[:255] = overlap
    ovl = np.concatenate([ov[0:128], ov[128:256]], axis=1)
    npp = np.arange(128)[:, None]; u = np.arange(4096)[None, :]
    bc = np.where(16 * npp + 31 <= u, 0.0, NEG).astype(np.float32)
    oh = (np.arange(4096)[None, :] // 64 == np.arange(128)[:, None]).astype(np.float32)
    k2 = np.arange(128)[:, None]; q2 = np.arange(128)[None, :]
    cbt = np.concatenate([np.where(k2 > q2, NEG, 0.0), np.where(k2 <= q2, NEG, 0.0)], axis=1).astype(np.float32)
    qp = np.arange(128)[:, None]; xx = np.arange(128)[None, :] - 64; own = qp // 64
    TF = np.where((xx == own) | (xx == own - 1), 1e6, -1e30).astype(np.float32)
    TN = np.where(xx <= own, 1e30, -1e30).astype(np.float32)
    sel = np.zeros((32, 16, 128), np.float32)
    for e in range(16):
        sel[e, e, :] = 1.0
        sel[16 + e, e, :] = 1.0
    shared = dict(cosT=cosT, sinT=sinT, id32=np.eye(128, dtype=np.float32), id16=np.eye(128, dtype=np.float32).astype(_bf),
                  eall=eall.astype(_bf), cb=cb.astype(_bf), ptab=ptab, sel=sel.reshape(32, 2048).astype(_bf),
                  cT=np.ascontiguousarray(c.T.reshape(8, 128, 4).transpose(1, 0, 2).reshape(128, 32)))
    if NL > 1:
        shared.update(ovl=ovl.astype(_bf), bc=bc.astype(_bf), oh=oh.astype(_bf), cbt=cbt.astype(_bf), tft=np.concatenate([TF, TN], axis=1))
    if NL == 4:
        shared["fg"] = f32(final_g)[None, :].copy()
    for l in range(NL):
        i = l // 2
        shared["adaw_%d" % l] = f32(ada_w[l]); shared["adab_%d" % l] = f32(ada_b[l])[None, :].copy()
        shared["n1c_%d" % l] = np.ascontiguousarray(f32(norm1_g)[l].reshape(8, 128).T)
        shared["n2c_%d" % l] = np.ascontiguousarray(f32(norm2_g)[l].reshape(8, 128).T)
        shared["wout_%d" % l] = f32(ev_w_out[i]) if l % 2 == 0 else f32(od_w_out[i])
        shared["wr_%d" % l] = np.ascontiguousarray(np.concatenate([f32(moe_wg[l]), f32(moe_we[l])], axis=1))
        shared["br_%d" % l] = np.concatenate([f32(moe_bg[l]), f32(moe_be[l])])[None, :].copy()
        shared["wgate_%d" % l] = f32(moe_w_gate[l]); shared["wup_%d" % l] = f32(moe_w_up[l]); shared["wdown_%d" % l] = f32(moe_w_down[l])
        if l % 2 == 0:
            shared["lam_%d" % l] = f32(ev_lambda[i]).reshape(1, 256).copy()
            shared["subg_%d" % l] = f32(ev_subln_g[i]).reshape(1, 128).copy()
        else:
            pos_ = f32(od_cmp_pos[i]); b1 = f32(od_cmp_b1[i]); w2 = f32(od_cmp_w2[i]); b2 = f32(od_cmp_b2[i])
            shared["w1_%d" % l] = f32(od_cmp_w1[i])
            shared["posT_%d" % l] = np.ascontiguousarray(np.concatenate([pos_[0].T, pos_[1].T], axis=1))
            shared["b1c_%d" % l] = np.ascontiguousarray(b1.reshape(2, 2, 128).transpose(2, 0, 1).reshape(128, 4))
            shared["w2d_%d" % l] = np.ascontiguousarray(np.concatenate([w2, w2], axis=-1))
            shared["b2c_%d" % l] = np.concatenate([b2[0], b2[0]])[:, None].copy()
            shared["b2r_%d" % l] = b2[1][None, :].copy()
    in_maps = []
    for core in range(8):
        b, p = core // 2, core % 2
        d = dict(shared)
        d["x"] = np.ascontiguousarray(x[b]); d["x_own"] = np.ascontiguousarray(x[b, p * 2048:(p + 1) * 2048])
        selp = np.zeros((128, 2), np.float32); selp[:, p] = 1.0
        bselB = np.zeros((4, 128), np.float32); bselB[b, :] = 1.0
        d["selp"] = selp; d["bselB"] = bselB; d["bselT"] = np.ascontiguousarray(bselB[:, 0:1])
        for l in range(NL):
            i = l // 2
            if l % 2 == 0:
                w = f32(ev_w_in[i]); sl = slice(256 * p, 256 * p + 256)
                d["winA_%d" % l] = np.ascontiguousarray(np.concatenate([w[:, 0:512][:, sl], w[:, 512:1024][:, sl], w[:, 1536:2048][:, sl], w[:, 2048:2560][:, sl],
                                                                       w[:, 1024:1536][:, sl], w[:, 2560:3072][:, sl]], axis=1))
            else:
                w = f32(od_w_in[i]); g = slice(128 * p, 128 * p + 128)
                d["winC_%d" % l] = np.ascontiguousarray(np.concatenate([w[:, 512 * p:512 * p + 512], w[:, 1024:1280][:, g], w[:, 1536:1792][:, g], w[:, 2048:2304][:, g],
                                                                       w[:, 1280:1536][:, g], w[:, 1792:2048][:, g], w[:, 2304:2560][:, g],
                                                                       w[:, 2560 + 24 * p:2560 + 24 * p + 24]], axis=1))
        in_maps.append(d)
    res = run_bass_kernel_spmd(kf.nc, in_maps, core_ids=list(range(8))).results
    out = np.zeros_like(x)
    for core in range(8):
        b, p = core // 2, core % 2
        out[b, p * 2048:(p + 1) * 2048] = res[core]["xo"]
    return out
```
